# Optimizing a Trainium2 kernel written in Bass

```python
import jax, jax.numpy as jnp
from jax import lax
import numpy as np

D_MODEL = 1024
BATCH = 4
SEQ = 4096
DEPTH = 2

CHUNK = 128
EPS = 1e-6
A_GROUPS = 4
A_GROUP_DIM = 128
A_WIDTH = A_GROUPS * A_GROUP_DIM
B_HEADS = 4
B_KEY_DIM = 64
B_VAL_DIM = 128
B_QK_WIDTH = B_HEADS * B_KEY_DIM
B_V_WIDTH = B_HEADS * B_VAL_DIM
B_GATE_RANK = 16
B_GATE_NORMALIZER = 16.0
EVEN_SPLITS = (A_WIDTH, A_WIDTH, B_QK_WIDTH, B_QK_WIDTH, B_GATE_RANK, B_V_WIDTH, B_V_WIDTH)
EVEN_IN = sum(EVEN_SPLITS)
EVEN_MIX_WIDTH = A_WIDTH + B_V_WIDTH
C_HEADS = 16
C_HEAD_DIM = 64
C_WIDTH = C_HEADS * C_HEAD_DIM
ODD_SPLITS = (C_WIDTH, C_WIDTH, C_WIDTH, C_WIDTH, C_HEADS)
ODD_IN = sum(ODD_SPLITS)
D_FF_DENSE = 2816
N_EXPERTS = 8
TOP_K = 2
D_FF_EXPERT = 3584
N_EVEN = (DEPTH + 1) // 2
N_ODD = DEPTH // 2

kernel_name = "hybrid_gmlp_gla_fox_moe_trunk"


def _split(z, sizes):
    idx = np.cumsum(np.array(sizes))[:-1].tolist()
    return jnp.split(z, idx, axis=-1)


def rmsnorm(x, g):
    xf = x.astype(jnp.float32)
    y = xf * lax.rsqrt(jnp.mean(xf * xf, axis=-1, keepdims=True) + EPS)
    return (y * g.astype(jnp.float32)).astype(x.dtype)


def layernorm(x, g, b):
    xf = x.astype(jnp.float32)
    mu = jnp.mean(xf, axis=-1, keepdims=True)
    xc = xf - mu
    y = xc * lax.rsqrt(jnp.mean(xc * xc, axis=-1, keepdims=True) + EPS)
    return (y * g.astype(jnp.float32) + b.astype(jnp.float32)).astype(x.dtype)


def swiglu(h, w1, w3, w2):
    return (jax.nn.silu(h @ w1) * (h @ w3)) @ w2


def chunk_spatial_gating(u, v, w_s, b_s):
    bsz, seq, _ = u.shape
    n = seq // CHUNK
    mask = jnp.tril(jnp.ones((CHUNK, CHUNK), dtype=bool))
    w = jnp.where(mask[None], w_s, jnp.zeros_like(w_s))
    v5 = v.reshape(bsz, n, CHUNK, A_GROUPS, A_GROUP_DIM)
    mixed = jnp.einsum('gts,bnsgd->bntgd', w, v5) + b_s.T[None, None, :, :, None]
    return (u.reshape(bsz, n, CHUNK, A_GROUPS, A_GROUP_DIM) * mixed).reshape(bsz, seq, A_WIDTH)


def gla_chunked(q, k, v, log_a):
    bsz, seq, nh, dk = q.shape
    dv = v.shape[-1]
    n = seq // CHUNK
    def to_chunks(t):
        return jnp.swapaxes(t.reshape(bsz, n, CHUNK, nh, t.shape[-1]), 0, 1)
    g = jnp.cumsum(log_a.astype(jnp.float32).reshape(bsz, n, CHUNK, nh, dk), axis=2)
    g = jnp.swapaxes(g, 0, 1)
    causal = jnp.tril(jnp.ones((CHUNK, CHUNK), dtype=bool))

    def step(state, inp):
        qc, kc, vc, gc = inp
        qf = qc.astype(jnp.float32)
        kf = kc.astype(jnp.float32)
        vf = vc.astype(jnp.float32)
        o_inter = jnp.einsum('bthk,bhkv->bthv', qf * jnp.exp(gc), state)
        diff = gc[:, :, None] - gc[:, None, :]
        diff = jnp.where(causal[None, :, :, None, None], diff, -jnp.inf)
        attn = jnp.einsum('bthk,bshk,btshk->btsh', qf, kf, jnp.exp(diff))
        o_intra = jnp.einsum('btsh,bshv->bthv', attn, vf)
        g_last = gc[:, -1]
        k_dec = kf * jnp.exp(g_last[:, None] - gc)
        new_state = jnp.exp(g_last)[..., None] * state + jnp.einsum('bshk,bshv->bhkv', k_dec, vf)
        return new_state, o_inter + o_intra

    state0 = jnp.zeros((bsz, nh, dk, dv), jnp.float32)
    _, out = lax.scan(step, state0, (to_chunks(q), to_chunks(k), to_chunks(v), g))
    return jnp.swapaxes(out, 0, 1).reshape(bsz, seq, nh, dv).astype(v.dtype)


def forgetting_attention(q, k, v, log_f):
    bsz, seq, nh, hd = q.shape
    n = seq // CHUNK
    scale = hd ** -0.5
    c = jnp.cumsum(log_f, axis=1)
    c_key = jnp.swapaxes(c, 1, 2)
    q_blocks = jnp.swapaxes(q.reshape(bsz, n, CHUNK, nh, hd), 0, 1)
    c_blocks = jnp.swapaxes(c.reshape(bsz, n, CHUNK, nh), 0, 1)
    s_pos = jnp.arange(seq)

    def block(args):
        qb, cb, bi = args
        logits = jnp.einsum('bthd,bshd->bhts', qb, k).astype(jnp.float32) * scale
        logits = logits + jnp.swapaxes(cb, 1, 2)[..., None] - c_key[:, :, None, :]
        t_pos = bi * CHUNK + jnp.arange(CHUNK)
        mask = s_pos[None, :] <= t_pos[:, None]
        logits = jnp.where(mask[None, None], logits, -jnp.inf)
        p = jax.nn.softmax(logits, axis=-1)
        return jnp.einsum('bhts,bshd->bthd', p.astype(v.dtype), v)

    out = lax.map(block, (q_blocks, c_blocks, jnp.arange(n)))
    return jnp.swapaxes(out, 0, 1).reshape(bsz, seq, nh * hd)


def mixer_even(h, w_in, gate_up, gate_bias, w_s, b_s, ln_g, ln_b, head_g, w_o):
    bsz, seq, _ = h.shape
    z = h @ w_in
    u, v, q, k, g_lr, vb, og = _split(z, EVEN_SPLITS)
    u = jax.nn.gelu(u)
    v = layernorm(jax.nn.gelu(v), ln_g, ln_b)
    a_out = chunk_spatial_gating(u, v, w_s, b_s)
    gate_logit = (g_lr @ gate_up + gate_bias).astype(jnp.float32)
    log_a = jax.nn.log_sigmoid(gate_logit) / B_GATE_NORMALIZER
    qh = q.reshape(bsz, seq, B_HEADS, B_KEY_DIM) * (B_KEY_DIM ** -0.5)
    kh = k.reshape(bsz, seq, B_HEADS, B_KEY_DIM)
    vh = vb.reshape(bsz, seq, B_HEADS, B_VAL_DIM)
    o = gla_chunked(qh, kh, vh, log_a.reshape(bsz, seq, B_HEADS, B_KEY_DIM))
    o = rmsnorm(o, head_g) * jax.nn.silu(og.reshape(bsz, seq, B_HEADS, B_VAL_DIM))
    b_out = o.reshape(bsz, seq, B_V_WIDTH)
    return jnp.concatenate([a_out, b_out], axis=-1) @ w_o


def mixer_odd(h, w_in, forget_bias, q_g, k_g, w_o):
    bsz, seq, _ = h.shape
    z = h @ w_in
    q, k, v, og, f = _split(z, ODD_SPLITS)
    q = rmsnorm(q.reshape(bsz, seq, C_HEADS, C_HEAD_DIM), q_g)
    k = rmsnorm(k.reshape(bsz, seq, C_HEADS, C_HEAD_DIM), k_g)
    v = v.reshape(bsz, seq, C_HEADS, C_HEAD_DIM)
    log_f = jax.nn.log_sigmoid(f.astype(jnp.float32) + forget_bias.astype(jnp.float32))
    o = forgetting_attention(q, k, v, log_f)
    o = o * jax.nn.sigmoid(og)
    return o @ w_o


def moe_swiglu(h, router, w1, w3, w2):
    logits = (h @ router).astype(jnp.float32)
    top_vals, top_idx = lax.top_k(logits, TOP_K)
    top_w = jax.nn.softmax(top_vals, axis=-1)
    gates = jnp.sum(jax.nn.one_hot(top_idx, N_EXPERTS, dtype=jnp.float32) * top_w[..., None], axis=-2)
    gates = gates.astype(h.dtype)
    out = jnp.zeros_like(h)
    for e in range(N_EXPERTS):
        out = out + gates[..., e:e + 1] * swiglu(h, w1[e], w3[e], w2[e])
    return out


def setup_inputs(seed: int = 0) -> dict:
    key = jax.random.key(seed)
    ks = iter(jax.random.split(key, 32))
    f32 = jnp.float32
    def nrm(shape, scale):
        return jax.random.normal(next(ks), shape, f32) * scale
    def gain(shape):
        return 1.0 + 0.05 * jax.random.normal(next(ks), shape, f32)
    d = D_MODEL
    return {
        "x": jax.random.normal(next(ks), (BATCH, SEQ, d), f32),
        "even_norm_mix": gain((N_EVEN, d)),
        "even_w_in": nrm((N_EVEN, d, EVEN_IN), d ** -0.5),
        "even_gate_up": nrm((N_EVEN, B_GATE_RANK, B_QK_WIDTH), B_GATE_RANK ** -0.5),
        "even_gate_bias": nrm((N_EVEN, B_QK_WIDTH), 0.1),
        "even_w_s": nrm((N_EVEN, A_GROUPS, CHUNK, CHUNK), CHUNK ** -0.5),
        "even_b_s": 1.0 + 0.1 * jax.random.normal(next(ks), (N_EVEN, A_GROUPS, CHUNK), f32),
        "even_ln_g": gain((N_EVEN, A_WIDTH)),
        "even_ln_b": nrm((N_EVEN, A_WIDTH), 0.02),
        "even_head_g": gain((N_EVEN, B_HEADS, B_VAL_DIM)),
        "even_w_o": nrm((N_EVEN, EVEN_MIX_WIDTH, d), EVEN_MIX_WIDTH ** -0.5),
        "even_norm_ffn": gain((N_EVEN, d)),
        "even_ffn_w1": nrm((N_EVEN, d, D_FF_DENSE), d ** -0.5),
        "even_ffn_w3": nrm((N_EVEN, d, D_FF_DENSE), d ** -0.5),
        "even_ffn_w2": nrm((N_EVEN, D_FF_DENSE, d), D_FF_DENSE ** -0.5),
        "odd_norm_mix": gain((N_ODD, d)),
        "odd_w_in": nrm((N_ODD, d, ODD_IN), d ** -0.5),
        "odd_forget_bias": 2.0 + 0.5 * jax.random.normal(next(ks), (N_ODD, C_HEADS), f32),
        "odd_q_g": gain((N_ODD, C_HEAD_DIM)),
        "odd_k_g": gain((N_ODD, C_HEAD_DIM)),
        "odd_w_o": nrm((N_ODD, C_WIDTH, d), C_WIDTH ** -0.5),
        "odd_norm_ffn": gain((N_ODD, d)),
        "odd_router": nrm((N_ODD, d, N_EXPERTS), d ** -0.5),
        "odd_exp_w1": nrm((N_ODD, N_EXPERTS, d, D_FF_EXPERT), d ** -0.5),
        "odd_exp_w3": nrm((N_ODD, N_EXPERTS, d, D_FF_EXPERT), d ** -0.5),
        "odd_exp_w2": nrm((N_ODD, N_EXPERTS, D_FF_EXPERT, d), D_FF_EXPERT ** -0.5),
        "final_norm": gain((d,)),
    }


def reference(x, even_norm_mix, even_w_in, even_gate_up, even_gate_bias, even_w_s, even_b_s,
              even_ln_g, even_ln_b, even_head_g, even_w_o, even_norm_ffn, even_ffn_w1,
              even_ffn_w3, even_ffn_w2, odd_norm_mix, odd_w_in, odd_forget_bias, odd_q_g,
              odd_k_g, odd_w_o, odd_norm_ffn, odd_router, odd_exp_w1, odd_exp_w3,
              odd_exp_w2, final_norm):
    for i in range(DEPTH):
        j = i // 2
        if i % 2 == 0:
            h = rmsnorm(x, even_norm_mix[j])
            x = x + mixer_even(h, even_w_in[j], even_gate_up[j], even_gate_bias[j], even_w_s[j],
                               even_b_s[j], even_ln_g[j], even_ln_b[j], even_head_g[j], even_w_o[j])
            h = rmsnorm(x, even_norm_ffn[j])
            x = x + swiglu(h, even_ffn_w1[j], even_ffn_w3[j], even_ffn_w2[j])
        else:
            h = rmsnorm(x, odd_norm_mix[j])
            x = x + mixer_odd(h, odd_w_in[j], odd_forget_bias[j], odd_q_g[j], odd_k_g[j], odd_w_o[j])
            h = rmsnorm(x, odd_norm_ffn[j])
            x = x + moe_swiglu(h, odd_router[j], odd_exp_w1[j], odd_exp_w3[j], odd_exp_w2[j])
    return rmsnorm(x, final_norm)
```

```python
import numpy as np
from contextlib import ExitStack, contextmanager
import concourse.bass as bass
import concourse.mybir as mybir
from concourse.bass_utils import run_bass_kernel_spmd

F32 = mybir.dt.float32
BF16 = mybir.dt.bfloat16
I32 = mybir.dt.int32
AF = mybir.ActivationFunctionType
ALU = mybir.AluOpType
AX = mybir.AxisListType


class T:
    __slots__ = ("name", "w", "r", "dsem", "dcnt")

    def __init__(self, name):
        self.name = name
        self.w = {}
        self.r = {}
        self.dsem = None
        self.dcnt = 0


class Prog:
    ENGS = ("pe", "act", "dve", "pool", "sp")

    def __init__(self, nc, es):
        self.nc = nc
        self.es = es
        self.es_outer = es
        self.ops = {e: [] for e in self.ENGS}
        self.seen = {e: {} for e in self.ENGS}
        self.ndsem = 0
        self.dsems = []

    @contextmanager
    def stage(self):
        with ExitStack() as es:
            old = self.es
            self.es = es
            yield
            self.emit()
            self.es = old

    def sb(self, name, shape, dtype):
        nb = 1
        for d_ in shape[1:]:
            nb *= d_
        nb *= 2 if dtype == BF16 else 4
        self.sb_bytes = getattr(self, "sb_bytes", 0) + ((nb + 31) // 32) * 32
        self.sb_log = getattr(self, "sb_log", [])
        self.sb_log.append((self.es, ((nb + 31) // 32) * 32))
        live = sum(b for (es_, b) in self.sb_log if es_ is self.es or es_ is self.es_outer)
        assert live <= 196 * 1024, "SBUF budget exceeded: %d" % live
        return self.es.enter_context(self.nc.sbuf_tensor(name, list(shape), dtype))

    def ps(self, name, shape, dtype):
        return self.es.enter_context(self.nc.psum_tensor(name, list(shape), dtype))

    def _dsem(self, t):
        if t.dsem is None:
            t.dsem = self.ndsem
            self.ndsem += 1
        return t.dsem

    def _deps(self, eng, rd, wr, is_dma_group_sem=None, merge=False):
        deps = []
        for t in rd:
            for tok in t.w.values():
                deps.append((tok, True))
        for t in wr:
            if not merge:
                for tok in t.w.values():
                    if not (is_dma_group_sem is not None and tok[0] == "D" and tok[1] == is_dma_group_sem):
                        deps.append((tok, False))
            for tok in t.r.values():
                deps.append((tok, False))
        waits = []
        seen = self.seen[eng]
        for tok, raw in deps:
            key = (tok[0], tok[1])
            if tok[0] == "E" and tok[1] == eng and not raw:
                continue
            if seen.get(key, -1) >= tok[2]:
                continue
            seen[key] = tok[2]
            waits.append(tok)
        best = {}
        for tok in waits:
            key = (tok[0], tok[1])
            if key not in best or best[key][2] < tok[2]:
                best[key] = tok
        return list(best.values())

    def op(self, eng, fn, rd=(), wr=()):
        rd = [t for t in rd if t is not None]
        wr = [t for t in wr if t is not None]
        waits = self._deps(eng, rd, wr)
        idx = len(self.ops[eng])
        tok = ("E", eng, idx)
        self.ops[eng].append({"fn": fn, "waits": waits, "tok": tok, "need_inc": False})
        for t in rd:
            t.r[("E", eng)] = tok
        for t in wr:
            t.w = {("E", eng): tok}
            t.r = {}
        return tok

    def dma(self, q, out, in_, rd=(), wr=(), **kw):
        rd = [t for t in rd if t is not None]
        wr = [t for t in wr if t is not None]
        store = kw.pop("store", False)
        owner = rd[0] if store else wr[0]
        ds = self._dsem(owner)
        waits = self._deps(q, rd, wr, is_dma_group_sem=ds, merge=store)
        owner.dcnt += 16
        tok = ("D", ds, owner.dcnt)
        self.ops[q].append({"dma": (out, in_, kw), "waits": waits, "tok": tok})
        for t in rd:
            t.r[("D", ds)] = tok
        for t in wr:
            if store:
                t.w[("D", ds)] = tok
            else:
                t.w = {("D", ds): tok}
            t.r = {}
        return tok

    def wait_all(self, eng, tiles):
        waits = self._deps(eng, tiles, [])
        self.ops[eng].append({"fn": None, "waits": waits, "tok": None, "need_inc": False})

    def emit(self):
        nc = self.nc
        if not hasattr(self, "_start"):
            self._start = {e: 0 for e in self.ENGS}
            self._rank = {e: 0 for e in self.ENGS}
            self._esem = {e: self.es_outer.enter_context(nc.semaphore("s_" + e)) for e in self.ENGS}
            self._dsl = []
        start = self._start
        for e in self.ENGS:
            for o in self.ops[e][start[e]:]:
                for tok in o["waits"]:
                    if tok[0] == "E":
                        assert tok[2] >= start[tok[1]], "wait on op from an earlier block"
                        self.ops[tok[1]][tok[2]]["need_inc"] = True
        rank = {}
        for e in self.ENGS:
            c = self._rank[e]
            for i in range(start[e], len(self.ops[e])):
                if self.ops[e][i].get("need_inc"):
                    c += 1
                    rank[(e, i)] = c
            self._rank[e] = c
        while len(self._dsl) < self.ndsem:
            self._dsl.append(self.es_outer.enter_context(nc.semaphore("d%d" % len(self._dsl))))
        esem, dsem = self._esem, self._dsl
        engobj = {"pe": "tensor", "act": "scalar", "dve": "vector", "pool": "gpsimd", "sp": "sync"}

        def body(e):
            def run(engine):
                for i in range(start[e], len(self.ops[e])):
                    o = self.ops[e][i]
                    for tok in o["waits"]:
                        if tok[0] == "E":
                            engine.wait_ge(esem[tok[1]], rank[(tok[1], tok[2])])
                        else:
                            engine.wait_ge(dsem[tok[1]], tok[2])
                    if "dma" in o:
                        out, in_, kw = o["dma"]
                        engine.dma_start(out=out, in_=in_, **kw).then_inc(dsem[o["tok"][1]], 16)
                    elif o["fn"] is not None:
                        ins = o["fn"](engine)
                        if o["need_inc"]:
                            ins.then_inc(esem[e], 1)
            return run

        with nc.Block() as block:
            for e in self.ENGS:
                if len(self.ops[e]) > start[e]:
                    getattr(block, engobj[e])(body(e))
        for e in self.ENGS:
            start[e] = len(self.ops[e])
        for e in self.ENGS:
            for e2 in self.ENGS:
                if self.ops[e2]:
                    self.seen[e][("E", e2)] = len(self.ops[e2]) - 1


import os
DBG = os.environ.get("KDBG", "")

D = 1024
EPS = 1e-6


class Ctx:
    def __init__(self, P):
        self.P = P
        nc = P.nc
        self.bank = [P.ps("bank%d" % i, [128, 512], F32) for i in range(8)]
        self.Tbank = [T("bank%d" % i) for i in range(8)]
        self.ident = P.sb("ident", [128, 128], F32)
        self.Tident = T("ident")
        self.identb = P.sb("identb", [128, 128], BF16)
        self.Tidentb = T("identb")
        self.ones = P.sb("ones", [128, 128], F32)
        self.Tones = T("ones")
        P.op("pool", lambda e: e.memset(self.ones[:], 1.0), wr=[self.Tones])
        P.op("pool", lambda e: e.memset(self.ident[:], 1.0), wr=[self.Tident])
        P.op("pool", lambda e: e.affine_select(out=self.ident[:], in_=self.ident[:], pattern=[[-1, 128]],
                                               compare_op=ALU.is_equal, fill=0.0, base=0,
                                               channel_multiplier=1), rd=[self.Tident], wr=[self.Tident])
        P.op("pool", lambda e: e.tensor_copy(out=self.identb[:], in_=self.ident[:]),
             rd=[self.Tident], wr=[self.Tidentb])


def load_gexp(P, C, name, g_dram):
    gT = P.sb(name + "_gT", [128, 8], F32)
    TgT = T(name + "_gT")
    gexp = P.sb(name + "_gexp", [128, 8, 128], F32)
    Tg = T(name + "_gexp")
    P.dma("sp", gT[:], g_dram.rearrange("(c p) -> p c", p=128), wr=[TgT], allow_slow_non_contiguous=True)
    for c in range(8):
        P.op("dve", lambda e, c=c: e.tensor_scalar(out=gexp[:, c, :], in0=C.ones[:], scalar1=gT[:, c:c + 1],
                                                   scalar2=None, op0=ALU.mult),
             rd=[C.Tones, TgT], wr=[Tg])
    return gexp, Tg


def swiglu_stage(P, C, name, x_in, x_out, ntok, g_norm, w1, w3, w2, dff, router=None, nexp=1,
                 g_final=None):
    nc = P.nc
    x_in_ap, Tx_in = x_in
    x_out_ap, Tx_out = x_out
    NB = 16
    npass = ntok // (NB * 128)
    nff = dff // 128
    groups = []
    f0 = 0
    while f0 < nff:
        gsz = min(4, nff - f0)
        groups.append((f0, gsz))
        f0 += gsz
    moe = router is not None

    gexp, Tgexp = load_gexp(P, C, name + "_gn", g_norm)
    xacc = P.sb(name + "_xacc", [128, NB, D], F32)
    Txacc = [T(name + "_xacc%d" % i) for i in range(NB)]
    hT = P.sb(name + "_hT", [128, 8, NB * 128], BF16)
    ThT = [T(name + "_hT%d" % i) for i in range(NB)]
    hid = P.sb(name + "_hid", [128, 4, NB * 128], BF16)
    Thid = [[T(name + "_hid%d_%d" % (f, t)) for t in range(4)] for f in range(4)]
    junk = P.sb(name + "_junk", [128, D], BF16)
    Tjunk = T(name + "_junk")
    xn = P.sb(name + "_xn", [128, D], F32)
    Txn = T(name + "_xn")
    ss = P.sb(name + "_ss", [128, NB], F32)
    Tss = [T(name + "_ss%d" % i) for i in range(NB)]
    rstd = P.sb(name + "_rstd", [128, NB], F32)
    Trstd = [T(name + "_rstd%d" % i) for i in range(NB)]
    sa = [P.sb(name + "_sa%d" % i, [128, 512], F32) for i in range(2)]
    Tsa = [T(name + "_sa%d" % i) for i in range(2)]
    wbuf = {}
    for par in range(2):
        wbuf[("w1", par)] = (P.sb(name + "_w1g%d" % par, [128, 8, 512], BF16), [T("w1g") for _ in range(8)])
        wbuf[("w3", par)] = (P.sb(name + "_w3g%d" % par, [128, 8, 512], BF16), [T("w3g") for _ in range(8)])
        wbuf[("w2", par)] = (P.sb(name + "_w2g%d" % par, [128, 4, 1024], BF16), [T("w2g") for _ in range(4)])
    NSTG = 2
    stg = [P.sb(name + "_stg%d" % i, [128, 1024], F32) for i in range(NSTG)]
    Tstg = [T(name + "_stg%d" % i) for i in range(NSTG)]
    stg_i = [0]
    if moe:
        rw = P.sb(name + "_rw", [128, 8, nexp], F32)
        Trw = T(name + "_rw")
        P.dma("sp", rw[:], router.rearrange("(c p) e -> p c e", p=128), wr=[Trw], allow_slow_non_contiguous=True)
        rwh = P.sb(name + "_rwh", [128, 8, nexp], BF16)
        rwl = P.sb(name + "_rwl", [128, 8, nexp], BF16)
        Trwh, Trwl = T(name + "_rwh"), T(name + "_rwl")
        P.op("pool", lambda e: e.tensor_copy(out=rwh[:], in_=rw[:]), rd=[Trw], wr=[Trwh])
        P.op("pool", lambda e: e.tensor_tensor(out=rwl[:], in0=rw[:], in1=rwh[:], op=ALU.subtract),
             rd=[Trw, Trwh], wr=[Trwl])
        hlo = P.sb(name + "_hlo", [128, 8, 128], BF16)
        Thlo = T(name + "_hlo")
        h32 = P.sb(name + "_h32", [128, 8, 128], F32)
        Th32 = T(name + "_h32")
        gates = P.sb(name + "_gates", [128, NB, nexp], F32)
        Tgates = [T(name + "_gates%d" % i) for i in range(NB)]
        gs = {k: P.sb(name + "_g" + k, [128, 8], F32) for k in ("lg", "m8", "ex", "mk", "ge")}
        Tgs = {k: T(name + "_g" + k) for k in gs}
        gs1 = {k: P.sb(name + "_g" + k, [128, 1], F32) for k in ("nm", "den", "rden")}
        Tgs1 = {k: T(name + "_g" + k) for k in gs1}
    if 'nofinal' in DBG:
        g_final = None
    if g_final is not None:
        gfin = P.sb(name + "_gfin", [128, D], F32)
        Tgfin = T(name + "_gfin")
        P.dma("sp", gfin[:], g_final.partition_broadcast(128), wr=[Tgfin])

    def load_w(kind, par, e, f0, gsz):
        buf, Ts = wbuf[(kind, par)]
        if kind in ("w1", "w3"):
            src = w1 if kind == "w1" else w3
            for c in range(8):
                i = stg_i[0] % NSTG
                stg_i[0] += 1
                ncol = gsz * 128
                P.dma("sp", stg[i][:, 0:ncol], src[e, c * 128:(c + 1) * 128, f0 * 128:f0 * 128 + ncol],
                      wr=[Tstg[i]])
                P.op("pool", lambda eng, i=i, c=c, ncol=ncol, buf=buf: eng.tensor_copy(
                    out=buf[:, c, 0:ncol], in_=stg[i][:, 0:ncol]), rd=[Tstg[i]], wr=[Ts[c]])
        else:
            for fl in range(gsz):
                i = stg_i[0] % NSTG
                stg_i[0] += 1
                f = f0 + fl
                P.dma("sp", stg[i][:, :], w2[e, f * 128:(f + 1) * 128, :], wr=[Tstg[i]])
                P.op("pool", lambda eng, i=i, fl=fl, buf=buf: eng.tensor_copy(
                    out=buf[:, fl, :], in_=stg[i][:, :]), rd=[Tstg[i]], wr=[Ts[fl]])

    bA = [(C.bank[0], C.Tbank[0]), (C.bank[1], C.Tbank[1])]
    bB = [(C.bank[2], C.Tbank[2]), (C.bank[3], C.Tbank[3])]
    bO = [(C.bank[4], C.Tbank[4]), (C.bank[5], C.Tbank[5])]
    bR = (C.bank[6], C.Tbank[6])

    for ps_ in range(npass):
        tok0 = ps_ * NB * 128
        work = [(e, gi) for e in range(nexp) for gi in range(len(groups))]
        gcount = 0
        e0, gi0 = work[0]
        if 'noexp' not in DBG:
            load_w("w1", 0, e0, *groups[gi0])
            load_w("w3", 0, e0, *groups[gi0])
            load_w("w2", 0, e0, *groups[gi0])
        for b in range(NB):
            P.dma("act", xacc[:, b, :], x_in_ap[tok0 + b * 128: tok0 + (b + 1) * 128, :],
                  rd=[Tx_in], wr=[Txacc[b]])
        for b in range(NB):
            P.op("act", lambda e, b=b: e.activation(out=junk[:], in_=xacc[:, b, :], func=AF.Square,
                                                    accum_out=ss[:, b:b + 1]),
                 rd=[Txacc[b]], wr=[Tjunk, Tss[b]])
            P.op("act", lambda e, b=b: e.activation(out=rstd[:, b:b + 1], in_=ss[:, b:b + 1], func=AF.Sqrt,
                                                    scale=1.0 / D, bias=EPS), rd=[Tss[b]], wr=[Trstd[b]])
            P.op("dve", lambda e, b=b: e.reciprocal(out=rstd[:, b:b + 1], in_=rstd[:, b:b + 1]),
                 rd=[Trstd[b]], wr=[Trstd[b]])
            P.op("dve", lambda e, b=b: e.tensor_scalar(out=xn[:], in0=xacc[:, b, :], scalar1=rstd[:, b:b + 1],
                                                       scalar2=None, op0=ALU.mult),
                 rd=[Txacc[b], Trstd[b]], wr=[Txn])
            for hh in range(2):
                bk, Tbk = (bA[b % 2] if hh == 0 else bB[b % 2])
                for cc in range(4):
                    c = hh * 4 + cc
                    P.op("pe", lambda e, c=c, cc=cc, bk=bk: e.transpose(
                        out=bk[:, cc * 128:(cc + 1) * 128], in_=xn[:, c * 128:(c + 1) * 128],
                        identity=C.ident[:]), rd=[Txn, C.Tident], wr=[Tbk])
                if not moe:
                    P.op("dve", lambda e, hh=hh, bk=bk, b=b: e.tensor_tensor(
                        out=hT[:, hh * 4:(hh + 1) * 4, b * 128:(b + 1) * 128],
                        in0=bk[:].rearrange("p (c t) -> p c t", c=4),
                        in1=gexp[:, hh * 4:(hh + 1) * 4, :], op=ALU.mult),
                         rd=[Tbk, Tgexp], wr=[ThT[b]])
                else:
                    P.op("dve", lambda e, hh=hh, bk=bk, b=b: e.tensor_tensor(
                        out=h32[:, hh * 4:(hh + 1) * 4, :],
                        in0=bk[:].rearrange("p (c t) -> p c t", c=4),
                        in1=gexp[:, hh * 4:(hh + 1) * 4, :], op=ALU.mult),
                         rd=[Tbk, Tgexp], wr=[Th32])
            if moe:
                P.op("pool", lambda e, b=b: e.tensor_copy(out=hT[:, :, b * 128:(b + 1) * 128], in_=h32[:]),
                     rd=[Th32], wr=[ThT[b]])
                P.op("pool", lambda e, b=b: e.tensor_tensor(out=hlo[:], in0=h32[:],
                                                            in1=hT[:, :, b * 128:(b + 1) * 128], op=ALU.subtract),
                     rd=[Th32, ThT[b]], wr=[Thlo])
                if 'nogate' in DBG:
                    P.op("pool", lambda e, b=b: e.memset(gates[:, b, :], 0.25), wr=[Tgates[b]])
                    continue
                rb, Trb = bR
                k3 = 0
                for (lh, Tl, rh, Tr) in ((hT[:, :, b * 128:(b + 1) * 128], ThT[b], rwh, Trwh),
                                         (hlo[:], Thlo, rwh, Trwh),
                                         (hT[:, :, b * 128:(b + 1) * 128], ThT[b], rwl, Trwl)):
                    for c in range(8):
                        P.op("pe", lambda e, c=c, lh=lh, rh=rh, k3=k3: e.matmul(
                            rb[:, 0:nexp], lhsT=lh[:, c, :], rhs=rh[:, c, :],
                            start=(k3 == 0), stop=(k3 == 23)), rd=[Tl, Tr], wr=[Trb])
                        k3 += 1
                P.op("act", lambda e: e.activation(out=gs["lg"][:], in_=rb[:, 0:nexp], func=AF.Copy),
                     rd=[Trb], wr=[Tgs["lg"]])
                P.op("dve", lambda e: e.max(out=gs["m8"][:], in_=gs["lg"][:]), rd=[Tgs["lg"]], wr=[Tgs["m8"]])
                P.op("dve", lambda e: e.tensor_scalar(out=gs1["nm"][:], in0=gs["m8"][:, 0:1], scalar1=-1.0,
                                                      scalar2=None, op0=ALU.mult),
                     rd=[Tgs["m8"]], wr=[Tgs1["nm"]])
                P.op("act", lambda e: e.activation(out=gs["ex"][:], in_=gs["lg"][:], func=AF.Exp,
                                                   bias=gs1["nm"][:, 0:1]),
                     rd=[Tgs["lg"], Tgs1["nm"]], wr=[Tgs["ex"]])
                P.op("dve", lambda e: e.tensor_scalar(out=gs["mk"][:], in0=gs["lg"][:], scalar1=gs["m8"][:, 1:2],
                                                      scalar2=None, op0=ALU.is_ge),
                     rd=[Tgs["lg"], Tgs["m8"]], wr=[Tgs["mk"]])
                P.op("dve", lambda e: e.tensor_tensor(out=gs["ge"][:], in0=gs["ex"][:], in1=gs["mk"][:],
                                                      op=ALU.mult),
                     rd=[Tgs["ex"], Tgs["mk"]], wr=[Tgs["ge"]])
                P.op("dve", lambda e: e.reduce_sum(out=gs1["den"][:], in_=gs["ge"][:], axis=AX.X),
                     rd=[Tgs["ge"]], wr=[Tgs1["den"]])
                P.op("dve", lambda e: e.reciprocal(out=gs1["rden"][:], in_=gs1["den"][:]),
                     rd=[Tgs1["den"]], wr=[Tgs1["rden"]])
                P.op("dve", lambda e, b=b: e.tensor_scalar(out=gates[:, b, :], in0=gs["ge"][:],
                                                           scalar1=gs1["rden"][:, 0:1], scalar2=None,
                                                           op0=ALU.mult),
                     rd=[Tgs["ge"], Tgs1["rden"]], wr=[Tgates[b]])
        for wi, (e_, gi) in enumerate(work if 'noexp' not in DBG else []):
            par = wi % 2
            f0, gsz = groups[gi]
            if wi + 1 < len(work):
                en, gn = work[wi + 1]
                load_w("w1", 1 - par, en, *groups[gn])
                load_w("w3", 1 - par, en, *groups[gn])
                load_w("w2", 1 - par, en, *groups[gn])
            w1g, Tw1 = wbuf[("w1", par)]
            w3g, Tw3 = wbuf[("w3", par)]
            w2g, Tw2 = wbuf[("w2", par)]
            k = 0
            for fl in range(gsz):
                for t in range(4):
                    a, Ta = bA[k % 2]
                    bb, Tb = bB[k % 2]
                    s_, Ts_ = sa[k % 2], Tsa[k % 2]
                    k += 1
                    for c in range(8):
                        P.op("pe", lambda e, c=c, a=a, fl=fl, t=t, w1g=w1g: e.matmul(
                            a[:], lhsT=w1g[:, c, fl * 128:(fl + 1) * 128], rhs=hT[:, c, t * 512:(t + 1) * 512],
                            start=(c == 0), stop=(c == 7)), rd=[Tw1[c]] + ThT[t * 4:(t + 1) * 4], wr=[Ta])
                    for c in range(8):
                        P.op("pe", lambda e, c=c, bb=bb, fl=fl, t=t, w3g=w3g: e.matmul(
                            bb[:], lhsT=w3g[:, c, fl * 128:(fl + 1) * 128], rhs=hT[:, c, t * 512:(t + 1) * 512],
                            start=(c == 0), stop=(c == 7)), rd=[Tw3[c]] + ThT[t * 4:(t + 1) * 4], wr=[Tb])
                    P.op("act", lambda e, a=a, s_=s_: e.activation(out=s_[:], in_=a[:], func=AF.Sigmoid),
                         rd=[Ta], wr=[Ts_])
                    P.op("dve", lambda e, a=a, s_=s_: e.tensor_tensor(out=s_[:], in0=s_[:], in1=a[:], op=ALU.mult),
                         rd=[Ts_, Ta], wr=[Ts_])
                    P.op("dve", lambda e, s_=s_, bb=bb, fl=fl, t=t: e.tensor_tensor(
                        out=hid[:, fl, t * 512:(t + 1) * 512], in0=s_[:], in1=bb[:], op=ALU.mult),
                         rd=[Ts_, Tb], wr=[Thid[fl][t]])
            k = 0
            for b in range(NB):
                for half in range(2):
                    o, To = bO[k % 2]
                    k += 1
                    for fl in range(gsz):
                        P.op("pe", lambda e, o=o, fl=fl, b=b, half=half, w2g=w2g: e.matmul(
                            o[:], lhsT=hid[:, fl, b * 128:(b + 1) * 128],
                            rhs=w2g[:, fl, half * 512:(half + 1) * 512],
                            start=(fl == 0), stop=(fl == gsz - 1)),
                             rd=[Thid[fl][b // 4], Tw2[fl]], wr=[To])
                    if moe:
                        sc = gates[:, b, e_:e_ + 1]
                        rds = [To, Txacc[b], Tgates[b]]
                    else:
                        sc = 1.0
                        rds = [To, Txacc[b]]
                    P.op("dve", lambda e, o=o, b=b, half=half, sc=sc: e.scalar_tensor_tensor(
                        out=xacc[:, b, half * 512:(half + 1) * 512], in0=o[:], scalar=sc,
                        in1=xacc[:, b, half * 512:(half + 1) * 512], op0=ALU.mult, op1=ALU.add),
                         rd=rds, wr=[Txacc[b]])
        for b in range(NB):
            if g_final is not None:
                P.op("act", lambda e, b=b: e.activation(out=junk[:], in_=xacc[:, b, :], func=AF.Square,
                                                        accum_out=ss[:, b:b + 1]),
                     rd=[Txacc[b]], wr=[Tjunk, Tss[b]])
                P.op("act", lambda e, b=b: e.activation(out=rstd[:, b:b + 1], in_=ss[:, b:b + 1], func=AF.Sqrt,
                                                        scale=1.0 / D, bias=EPS), rd=[Tss[b]], wr=[Trstd[b]])
                P.op("dve", lambda e, b=b: e.reciprocal(out=rstd[:, b:b + 1], in_=rstd[:, b:b + 1]),
                     rd=[Trstd[b]], wr=[Trstd[b]])
                P.op("dve", lambda e, b=b: e.scalar_tensor_tensor(
                    out=xacc[:, b, :], in0=xacc[:, b, :], scalar=rstd[:, b:b + 1], in1=gfin[:],
                    op0=ALU.mult, op1=ALU.mult), rd=[Txacc[b], Trstd[b], Tgfin], wr=[Txacc[b]])
            P.dma("act", x_out_ap[tok0 + b * 128: tok0 + (b + 1) * 128, :], xacc[:, b, :],
                  rd=[Txacc[b]], wr=[Tx_out], store=True)


def load_weight_bf16(P, name, src, K, N, q="sp", stg=None):
    kc = max(1, K // 128)
    kp = min(K, 128)
    wb = P.sb(name, [kp, kc, N], BF16)
    Tw = T(name)
    if stg is None:
        stg = [P.sb(name + "_s%d" % i, [128, 1024], F32) for i in range(2)]
        Ts = [T(name + "_s%d" % i) for i in range(2)]
    else:
        stg, Ts = stg
    i = 0
    for c in range(kc):
        for n0 in range(0, N, 1024):
            n1 = min(N, n0 + 1024)
            s, Tst = stg[i % 2], Ts[i % 2]
            i += 1
            P.dma(q, s[0:kp, 0:n1 - n0], src[c * 128:c * 128 + kp, n0:n1], wr=[Tst])
            P.op("pool", lambda e, s=s, c=c, n0=n0, n1=n1: e.tensor_copy(out=wb[:, c, n0:n1], in_=s[0:kp, 0:n1 - n0]),
                 rd=[Tst], wr=[Tw])
    return wb, Tw


class Banks:
    def __init__(self, C):
        self.C = C
        self.i = 0

    def next(self):
        b = self.C.bank[self.i % 8], self.C.Tbank[self.i % 8]
        self.i += 1
        return b


def norm_T(P, C, BK, name, x_ap, Tx, gexp, Tgexp, scr):
    ss, Tss, rstd, Trstd, junk, Tjunk, xn, Txn, hT, ThT = scr
    P.op("act", lambda e: e.activation(out=junk[:], in_=x_ap, func=AF.Square, accum_out=ss[:, 0:1]),
         rd=[Tx], wr=[Tjunk, Tss])
    P.op("act", lambda e: e.activation(out=rstd[:, 0:1], in_=ss[:, 0:1], func=AF.Sqrt, scale=1.0 / D, bias=EPS),
         rd=[Tss], wr=[Trstd])
    P.op("dve", lambda e: e.reciprocal(out=rstd[:, 0:1], in_=rstd[:, 0:1]), rd=[Trstd], wr=[Trstd])
    P.op("dve", lambda e: e.tensor_scalar(out=xn[:], in0=x_ap, scalar1=rstd[:, 0:1], scalar2=None, op0=ALU.mult),
         rd=[Tx, Trstd], wr=[Txn])
    for hh in range(2):
        bk, Tbk = BK.next()
        for cc in range(4):
            c = hh * 4 + cc
            P.op("pe", lambda e, c=c, cc=cc, bk=bk: e.transpose(out=bk[:, cc * 128:(cc + 1) * 128],
                                                                in_=xn[:, c * 128:(c + 1) * 128],
                                                                identity=C.ident[:]),
                 rd=[Txn, C.Tident], wr=[Tbk])
        P.op("dve", lambda e, hh=hh, bk=bk: e.tensor_tensor(out=hT[:, hh * 4:(hh + 1) * 4, :],
                                                            in0=bk[:].rearrange("p (c t) -> p c t", c=4),
                                                            in1=gexp[:, hh * 4:(hh + 1) * 4, :], op=ALU.mult),
             rd=[Tbk, Tgexp], wr=[ThT])


def mk_norm_scr(P, name):
    ss = P.sb(name + "_ss", [128, 1], F32)
    rstd = P.sb(name + "_rstd", [128, 1], F32)
    junk = P.sb(name + "_junk", [128, D], BF16)
    xn = P.sb(name + "_xn", [128, D], F32)
    hT = P.sb(name + "_hT", [128, 8, 128], BF16)
    return (ss, T(name + "ss"), rstd, T(name + "rstd"), junk, T(name + "junk"), xn, T(name + "xn"), hT, T(name + "hT"))


def transpose_to(P, C, BK, src, Tsrc, ncol, dst, Tdst, npart=128):
    nch = ncol // 128
    for c0 in range(0, nch, 4):
        bk, Tbk = BK.next()
        n = min(4, nch - c0)
        for cc in range(n):
            c = c0 + cc
            P.op("pe", lambda e, c=c, cc=cc, bk=bk: e.transpose(out=bk[:, cc * npart:(cc + 1) * npart],
                                                                in_=src[0:npart, c * 128:(c + 1) * 128],
                                                                identity=C.ident[0:npart, 0:npart]),
                 rd=[Tsrc, C.Tident], wr=[Tbk])
        P.op("act", lambda e, c0=c0, n=n, bk=bk: e.activation(
            out=dst[:, c0:c0 + n, :], in_=bk[:, 0:n * npart].rearrange("p (c t) -> p c t", c=n), func=AF.Copy),
             rd=[Tbk], wr=[Tdst])


def gelu_tanh(P, name, src, Tsrc, dst, Tdst, tmp, Ttmp):
    P.op("act", lambda e: e.activation(out=tmp[:], in_=src, func=AF.Square), rd=[Tsrc], wr=[Ttmp])
    P.op("dve", lambda e: e.tensor_scalar(out=tmp[:], in0=tmp[:], scalar1=0.044715, scalar2=1.0, op0=ALU.mult,
                                          op1=ALU.add), rd=[Ttmp], wr=[Ttmp])
    P.op("dve", lambda e: e.tensor_tensor(out=tmp[:], in0=tmp[:], in1=src, op=ALU.mult), rd=[Ttmp, Tsrc], wr=[Ttmp])
    P.op("act", lambda e: e.activation(out=tmp[:], in_=tmp[:], func=AF.Sigmoid, scale=1.5957691216057308),
         rd=[Ttmp], wr=[Ttmp])
    P.op("dve", lambda e: e.tensor_tensor(out=dst, in0=tmp[:], in1=src, op=ALU.mult), rd=[Ttmp, Tsrc], wr=[Tdst])


def run_pipeline(gens):
    gens = list(gens)
    idx = 1
    old, new, new_mid = None, gens[0], False
    while old is not None or new is not None:
        if old is not None:
            try:
                next(old)
            except StopIteration:
                old = None
        if new is not None and not new_mid:
            if next(new) == "MID":
                new_mid = True
        if old is None and (new_mid or new is None):
            old = new
            new = gens[idx] if idx < len(gens) else None
            idx += 1
            new_mid = False


class HalfBanks:
    def __init__(self, C, base):
        self.C, self.base, self.i = C, base, 0

    def next(self):
        k = self.base + (self.i % 4)
        self.i += 1
        return self.C.bank[k], self.C.Tbank[k]


def l0_mixer_stage(P, C, x_in, x_out, ntok, W):
    x_in_ap, Tx_in = x_in
    x_out_ap, Tx_out = x_out
    BK0 = Banks(C)
    nblk = ntok // 128
    stg = ([P.sb("l0m_stg%d" % i, [128, 1024], F32) for i in range(2)], [T("l0m_stg%d" % i) for i in range(2)])
    w_in, Tw_in = load_weight_bf16(P, "l0m_w_in", W["w_in"], D, 2576, stg=stg)
    w_o, Tw_o = load_weight_bf16(P, "l0m_w_o", W["w_o"], D, D, stg=stg)
    gup, Tgup = load_weight_bf16(P, "l0m_gup", W["gate_up"], 16, 256, stg=stg)
    gexp, Tgexp = load_gexp(P, C, "l0m_gn", W["norm_mix"])

    def sbt(name, shape, dt=F32):
        return P.sb("l0m_" + name, shape, dt), T("l0m_" + name)

    mask, Tmask = sbt("mask", [128, 128])
    P.op("pool", lambda e: e.memset(mask[:], 1.0), wr=[Tmask])
    P.op("pool", lambda e: e.affine_select(out=mask[:], in_=mask[:], pattern=[[1, 128]], compare_op=ALU.is_ge,
                                           fill=0.0, base=0, channel_multiplier=-1), rd=[Tmask], wr=[Tmask])
    wsT, TwsT = sbt("wsT", [128, 4, 128], BF16)
    ws32, Tws32 = sbt("ws32", [128, 4 * 128])
    P.dma("sp", ws32[:].rearrange("t (g s) -> t g s", g=4), W["w_s"].rearrange("g t s -> t g s"), wr=[Tws32])
    wsT32, TwsT32 = sbt("wsT32", [128, 4, 128])
    transpose_to(P, C, BK0, ws32, Tws32, 512, wsT32, TwsT32)
    P.op("dve", lambda e: e.tensor_tensor(out=wsT[:], in0=wsT32[:],
                                          in1=mask[:].unsqueeze(1).to_broadcast([128, 4, 128]), op=ALU.mult),
         rd=[TwsT32, Tmask], wr=[TwsT])
    bs, Tbs = sbt("bs", [128, 4])
    P.dma("sp", bs[:], W["b_s"].rearrange("g t -> t g"), wr=[Tbs], allow_slow_non_contiguous=True)
    lng, Tlng = sbt("lng", [128, 512])
    lnb, Tlnb = sbt("lnb", [128, 512])
    hdg, Thdg = sbt("hdg", [128, 512])
    P.dma("sp", lng[:], W["ln_g"].partition_broadcast(128), wr=[Tlng])
    P.dma("sp", lnb[:], W["ln_b"].partition_broadcast(128), wr=[Tlnb])
    P.dma("sp", hdg[:], W["head_g"].rearrange("h v -> (h v)").partition_broadcast(128), wr=[Thdg])
    ngb, Tngb = sbt("ngb", [64, 4])
    P.dma("sp", ngb[:], W["gate_bias"].rearrange("(h k) -> k h", k=64), wr=[Tngb], allow_slow_non_contiguous=True)
    P.op("dve", lambda e: e.tensor_scalar(out=ngb[:], in0=ngb[:], scalar1=-1.0, scalar2=None, op0=ALU.mult),
         rd=[Tngb], wr=[Tngb])
    S, TS = sbt("S", [64, 4, 128])
    Sb, TSb = sbt("Sb", [64, 4, 128], BF16)
    P.op("pool", lambda e: e.memset(S[:], 0.0), wr=[TS])
    P.op("pool", lambda e: e.memset(Sb[:], 0.0), wr=[TSb])
    junk, Tjunk = sbt("junk", [128, D], BF16)

    def mkset(i):
        def st(name, shape, dt=F32):
            return sbt("%s_%d" % (name, i), shape, dt)
        d = {}
        d["xb"] = st("xb", [128, D])
        d["ss"] = st("ss", [128, 1])
        d["rstd"] = st("rstd", [128, 1])
        d["xn"] = st("xn", [128, D])
        d["hT"] = st("hT", [128, 8, 128], BF16)
        for nm in ("ug", "vg", "tmp", "on", "sg"):
            d[nm] = st(nm, [128, 512])
        d["vnb"] = st("vnb", [128, 512], BF16)
        d["vbb"] = st("vbb", [128, 512], BF16)
        d["st6"] = st("st6", [128, 6])
        d["mv"] = st("mv", [128, 2])
        d["lrs"] = st("lrs", [128, 1])
        d["mix"] = st("mix", [128, D])
        d["mixT"] = st("mixT", [128, 8, 128], BF16)
        d["glr"] = st("glr", [16, 128], BF16)
        d["el"] = st("el", [64, 512])
        d["cs"] = st("cs", [64, 4, 128])
        d["ncl"] = st("ncl", [64, 4])
        d["egl"] = st("egl", [64, 4])
        d["e1"] = st("e1", [64, 512])
        d["qt"] = st("qt", [64, 4, 128], BF16)
        d["kt"] = st("kt", [64, 4, 128], BF16)
        d["kd32"] = st("kd32", [64, 512])
        d["kdec"] = st("kdec", [128, 4, 64], BF16)
        d["attT"] = st("attT", [128, 4, 128], BF16)
        d["ssq"] = st("ssq", [128, 4])
        d["xo"] = st("xo", [128, D])
        d["BK"] = HalfBanks(C, 4 * i)
        return d

    sets = [mkset(0), mkset(1)]

    def block(b):
        d = sets[b % 2]
        BK = d["BK"]
        (x, Txb), (ug, Tug), (vg, Tvg), (tmp, Ttmp), (on, Ton), (sg, Tsg) = [d[k] for k in ("xb", "ug", "vg", "tmp", "on", "sg")]
        (vnb, Tvnb), (vbb, Tvbb), (st6, Tst6), (mv, Tmv), (lrs, Tlrs) = [d[k] for k in ("vnb", "vbb", "st6", "mv", "lrs")]
        (mix, Tmix), (mixT, TmixT), (glr, Tglr), (el, Tel), (cs, Tcs) = [d[k] for k in ("mix", "mixT", "glr", "el", "cs")]
        (ncl, Tncl), (egl, Tegl), (e1, Te1), (qt, Tqt), (kt, Tkt) = [d[k] for k in ("ncl", "egl", "e1", "qt", "kt")]
        (kd32, Tkd32), (kdec, Tkdec), (attT, TattT), (ssq, Tssq), (xo_, Txo) = [d[k] for k in ("kd32", "kdec", "attT", "ssq", "xo")]
        hT, ThT = d["hT"]
        scr = (d["ss"][0], d["ss"][1], d["rstd"][0], d["rstd"][1], junk, Tjunk, d["xn"][0], d["xn"][1], hT, ThT)

        def proj_tok(col0, ncols=512):
            bk, Tbk = BK.next()
            for c in range(8):
                P.op("pe", lambda e, c=c, bk=bk: e.matmul(bk[:, 0:ncols], lhsT=hT[:, c, :],
                                                          rhs=w_in[:, c, col0:col0 + ncols],
                                                          start=(c == 0), stop=(c == 7)), rd=[ThT, Tw_in], wr=[Tbk])
            return bk, Tbk

        def proj_feat(col0, m, nh):
            bk, Tbk = BK.next()
            for h in range(nh):
                for c in range(8):
                    P.op("pe", lambda e, c=c, h=h, bk=bk: e.matmul(
                        bk[0:m, h * 128:(h + 1) * 128], lhsT=w_in[:, c, col0 + h * m: col0 + (h + 1) * m],
                        rhs=hT[:, c, :], start=(c == 0), stop=(c == 7)), rd=[ThT, Tw_in], wr=[Tbk])
            return bk, Tbk

        P.dma("act", x[:], x_in_ap[b * 128:(b + 1) * 128, :], rd=[Tx_in], wr=[Txb])
        norm_T(P, C, BK, "l0m", x[:], Txb, gexp, Tgexp, scr)
        yield
        pu, Tpu = proj_tok(0)
        yield
        gelu_tanh(P, "u", pu[:], Tpu, ug[:], Tug, tmp, Ttmp)
        yield
        pv, Tpv = proj_tok(512)
        yield
        gelu_tanh(P, "v", pv[:], Tpv, vg[:], Tvg, tmp, Ttmp)
        yield
        P.op("dve", lambda e: e.bn_stats(out=st6[:], in_=vg[:]), rd=[Tvg], wr=[Tst6])
        P.op("dve", lambda e: e.bn_aggr(out=mv[:], in_=st6[:]), rd=[Tst6], wr=[Tmv])
        P.op("act", lambda e: e.activation(out=lrs[:], in_=mv[:, 1:2], func=AF.Sqrt, bias=EPS), rd=[Tmv], wr=[Tlrs])
        P.op("dve", lambda e: e.reciprocal(out=lrs[:], in_=lrs[:]), rd=[Tlrs], wr=[Tlrs])
        yield
        P.op("dve", lambda e: e.tensor_scalar(out=vg[:], in0=vg[:], scalar1=mv[:, 0:1], scalar2=lrs[:, 0:1],
                                              op0=ALU.subtract, op1=ALU.mult), rd=[Tvg, Tmv, Tlrs], wr=[Tvg])
        P.op("dve", lambda e: e.tensor_tensor(out=vg[:], in0=vg[:], in1=lng[:], op=ALU.mult), rd=[Tvg, Tlng], wr=[Tvg])
        P.op("dve", lambda e: e.tensor_tensor(out=vnb[:], in0=vg[:], in1=lnb[:], op=ALU.add), rd=[Tvg, Tlnb], wr=[Tvnb])
        yield
        pvb, Tpvb = proj_tok(1552)
        P.op("act", lambda e, pvb=pvb: e.activation(out=vbb[:], in_=pvb[:], func=AF.Copy), rd=[Tpvb], wr=[Tvbb])
        yield
        pm, Tpm = BK.next()
        for g in range(4):
            P.op("pe", lambda e, g=g, pm=pm: e.matmul(pm[:, g * 128:(g + 1) * 128], lhsT=wsT[:, g, :],
                                                      rhs=vnb[:, g * 128:(g + 1) * 128], start=True, stop=True),
                 rd=[TwsT, Tvnb], wr=[Tpm])
        for g in range(4):
            P.op("dve", lambda e, g=g, pm=pm: e.scalar_tensor_tensor(
                out=mix[:, g * 128:(g + 1) * 128], in0=pm[:, g * 128:(g + 1) * 128], scalar=bs[:, g:g + 1],
                in1=ug[:, g * 128:(g + 1) * 128], op0=ALU.add, op1=ALU.mult), rd=[Tpm, Tbs, Tug], wr=[Tmix])
        yield
        pg, Tpg = proj_feat(1536, 16, 1)
        P.op("act", lambda e, pg=pg: e.activation(out=glr[:], in_=pg[0:16, 0:128], func=AF.Copy), rd=[Tpg], wr=[Tglr])
        yield
        pgt, Tpgt = BK.next()
        for h in range(4):
            P.op("pe", lambda e, h=h, pgt=pgt: e.matmul(pgt[0:64, h * 128:(h + 1) * 128],
                                                        lhsT=gup[:, 0, h * 64:(h + 1) * 64], rhs=glr[:],
                                                        start=True, stop=True), rd=[Tgup, Tglr], wr=[Tpgt])
        for h in range(4):
            P.op("act", lambda e, h=h, pgt=pgt: e.activation(out=el[:, h * 128:(h + 1) * 128],
                                                             in_=pgt[0:64, h * 128:(h + 1) * 128], func=AF.Exp,
                                                             scale=-1.0, bias=ngb[:, h:h + 1]),
                 rd=[Tpgt, Tngb], wr=[Tel])
        P.op("act", lambda e: e.activation(out=el[:], in_=el[:], func=AF.Ln, bias=1.0), rd=[Tel], wr=[Tel])
        yield
        for h in range(4):
            P.op("dve", lambda e, h=h: e.tensor_tensor_scan(out=cs[:, h, :], data0=C.ones[0:64, :],
                                                            data1=el[:, h * 128:(h + 1) * 128], initial=0.0,
                                                            op0=ALU.mult, op1=ALU.add),
                 rd=[Tel, C.Tones], wr=[Tcs])
        P.op("dve", lambda e: e.tensor_scalar(out=ncl[:], in0=cs[:, :, 127], scalar1=-1.0 / 16, scalar2=None,
                                              op0=ALU.mult), rd=[Tcs], wr=[Tncl])
        P.op("act", lambda e: e.activation(out=egl[:], in_=ncl[:], func=AF.Exp), rd=[Tncl], wr=[Tegl])
        yield
        pq, Tpq = proj_feat(1024, 64, 4)
        yield
        csf = cs[:].rearrange("p h t -> p (h t)")
        P.op("act", lambda e: e.activation(out=e1[:], in_=csf, func=AF.Exp, scale=-1.0 / 16), rd=[Tcs], wr=[Te1])
        P.op("dve", lambda e, pq=pq: e.scalar_tensor_tensor(out=qt[:].rearrange("p h t -> p (h t)"), in0=pq[0:64, :],
                                                            scalar=0.125, in1=e1[:], op0=ALU.mult, op1=ALU.mult),
             rd=[Tpq, Te1], wr=[Tqt])
        yield
        pk, Tpk = proj_feat(1280, 64, 4)
        yield
        P.op("act", lambda e: e.activation(out=e1[:], in_=csf, func=AF.Exp, scale=1.0 / 16), rd=[Tcs], wr=[Te1])
        P.op("dve", lambda e, pk=pk: e.tensor_tensor(out=kt[:].rearrange("p h t -> p (h t)"), in0=pk[0:64, :],
                                                     in1=e1[:], op=ALU.mult), rd=[Tpk, Te1], wr=[Tkt])
        for h in range(4):
            P.op("act", lambda e, h=h: e.activation(out=e1[:, h * 128:(h + 1) * 128], in_=cs[:, h, :], func=AF.Exp,
                                                    scale=1.0 / 16, bias=ncl[:, h:h + 1]), rd=[Tcs, Tncl], wr=[Te1])
        P.op("dve", lambda e, pk=pk: e.tensor_tensor(out=kd32[:], in0=pk[0:64, :], in1=e1[:], op=ALU.mult),
             rd=[Tpk, Te1], wr=[Tkd32])
        yield
        transpose_to(P, C, BK, kd32, Tkd32, 512, kdec, Tkdec, npart=64)
        yield "MID"
        pa, Tpa = BK.next()
        for h in range(4):
            P.op("pe", lambda e, h=h, pa=pa: e.matmul(pa[:, h * 128:(h + 1) * 128], lhsT=kt[:, h, :], rhs=qt[:, h, :],
                                                      start=True, stop=True), rd=[Tkt, Tqt], wr=[Tpa])
        P.op("dve", lambda e, pa=pa: e.tensor_tensor(out=attT[:], in0=pa[:].rearrange("p (h t) -> p h t", h=4),
                                                     in1=mask[:].unsqueeze(1).to_broadcast([128, 4, 128]),
                                                     op=ALU.mult), rd=[Tpa, Tmask], wr=[TattT])
        yield
        pS, TpS = BK.next()
        for h in range(4):
            P.op("pe", lambda e, h=h, pS=pS: e.matmul(pS[0:64, h * 128:(h + 1) * 128], lhsT=kdec[:, h, :],
                                                      rhs=vbb[:, h * 128:(h + 1) * 128], start=True, stop=True),
                 rd=[Tkdec, Tvbb], wr=[TpS])
        yield
        po, Tpo = BK.next()
        for h in range(4):
            P.op("pe", lambda e, h=h, po=po: e.matmul(po[:, h * 128:(h + 1) * 128], lhsT=attT[:, h, :],
                                                      rhs=vbb[:, h * 128:(h + 1) * 128], start=True, stop=False),
                 rd=[TattT, Tvbb], wr=[Tpo])
            P.op("pe", lambda e, h=h, po=po: e.matmul(po[:, h * 128:(h + 1) * 128], lhsT=qt[:, h, :],
                                                      rhs=Sb[:, h, :], start=False, stop=True),
                 rd=[Tqt, TSb], wr=[Tpo])
        yield
        for h in range(4):
            P.op("dve", lambda e, h=h, pS=pS: e.scalar_tensor_tensor(
                out=S[:, h, :], in0=S[:, h, :], scalar=egl[:, h:h + 1], in1=pS[0:64, h * 128:(h + 1) * 128],
                op0=ALU.mult, op1=ALU.add), rd=[TS, Tegl, TpS], wr=[TS])
        P.op("pool", lambda e: e.tensor_copy(out=Sb[:], in_=S[:]), rd=[TS], wr=[TSb])
        yield
        for h in range(4):
            P.op("act", lambda e, h=h, po=po: e.activation(out=sg[:, h * 128:(h + 1) * 128],
                                                           in_=po[:, h * 128:(h + 1) * 128], func=AF.Square,
                                                           accum_out=ssq[:, h:h + 1]), rd=[Tpo], wr=[Tsg, Tssq])
        P.op("act", lambda e: e.activation(out=ssq[:], in_=ssq[:], func=AF.Sqrt, scale=1.0 / 128, bias=EPS),
             rd=[Tssq], wr=[Tssq])
        P.op("dve", lambda e: e.reciprocal(out=ssq[:], in_=ssq[:]), rd=[Tssq], wr=[Tssq])
        yield
        P.op("dve", lambda e, po=po: e.tensor_tensor(out=on[:].rearrange("p (h v) -> p h v", h=4),
                                                     in0=po[:].rearrange("p (h v) -> p h v", h=4),
                                                     in1=ssq[:].unsqueeze(2).to_broadcast([128, 4, 128]),
                                                     op=ALU.mult), rd=[Tpo, Tssq], wr=[Ton])
        P.op("dve", lambda e: e.tensor_tensor(out=on[:], in0=on[:], in1=hdg[:], op=ALU.mult), rd=[Ton, Thdg], wr=[Ton])
        yield
        pog, Tpog = proj_tok(2064)
        yield
        P.op("act", lambda e, pog=pog: e.activation(out=sg[:], in_=pog[:], func=AF.Sigmoid), rd=[Tpog], wr=[Tsg])
        P.op("dve", lambda e, pog=pog: e.tensor_tensor(out=sg[:], in0=sg[:], in1=pog[:], op=ALU.mult),
             rd=[Tsg, Tpog], wr=[Tsg])
        P.op("dve", lambda e: e.tensor_tensor(out=mix[:, 512:1024], in0=on[:], in1=sg[:], op=ALU.mult),
             rd=[Ton, Tsg], wr=[Tmix])
        yield
        transpose_to(P, C, BK, mix, Tmix, 1024, mixT, TmixT)
        yield
        for half in range(2):
            pw, Tpw = BK.next()
            for c in range(8):
                P.op("pe", lambda e, c=c, pw=pw, half=half: e.matmul(pw[:], lhsT=mixT[:, c, :],
                                                                   rhs=w_o[:, c, half * 512:(half + 1) * 512],
                                                                   start=(c == 0), stop=(c == 7)),
                     rd=[TmixT, Tw_o], wr=[Tpw])
            P.op("dve", lambda e, pw=pw, half=half: e.tensor_tensor(
                out=xo_[:, half * 512:(half + 1) * 512], in0=pw[:], in1=x[:, half * 512:(half + 1) * 512], op=ALU.add),
                 rd=[Tpw, Txb], wr=[Txo])
            yield
        P.dma("act", x_out_ap[b * 128:(b + 1) * 128, :], xo_[:], rd=[Txo], wr=[Tx_out], store=True)

    run_pipeline(block(b) for b in range(nblk))


def head_norm(P, name, src, Tsrc, g_rep, Tg_rep, dst, Tdst, tmp, Ttmp, ss16, Tss16, scale):
    P.op("dve", lambda e: e.tensor_tensor(out=tmp[:], in0=src[:], in1=src[:], op=ALU.mult), rd=[Tsrc], wr=[Ttmp])
    P.op("dve", lambda e: e.tensor_reduce(out=ss16[:], in_=tmp[:].rearrange("p (h d) -> p h d", h=16), axis=AX.X,
                                          op=ALU.add), rd=[Ttmp], wr=[Tss16])
    P.op("act", lambda e: e.activation(out=ss16[:], in_=ss16[:], func=AF.Sqrt, scale=1.0 / 64, bias=EPS),
         rd=[Tss16], wr=[Tss16])
    P.op("dve", lambda e: e.reciprocal(out=ss16[:], in_=ss16[:]), rd=[Tss16], wr=[Tss16])
    P.op("dve", lambda e: e.tensor_tensor(out=tmp[:].rearrange("p (h d) -> p h d", h=16),
                                          in0=src[:].rearrange("p (h d) -> p h d", h=16),
                                          in1=ss16[:].unsqueeze(2).to_broadcast([128, 16, 64]), op=ALU.mult),
         rd=[Tsrc, Tss16], wr=[Ttmp])
    P.op("dve", lambda e: e.scalar_tensor_tensor(out=dst[:].rearrange("p (h d) -> p h d", h=16),
                                                 in0=tmp[:].rearrange("p (h d) -> p h d", h=16), scalar=scale,
                                                 in1=g_rep[:].unsqueeze(1).to_broadcast([128, 16, 64]),
                                                 op0=ALU.mult, op1=ALU.mult), rd=[Ttmp, Tg_rep], wr=[Tdst])


def split3(P, name, src, Tsrc, outs, Touts, r32, Tr32):
    P.op("dve", lambda e: e.tensor_copy(out=outs[0][:], in_=src), rd=[Tsrc], wr=[Touts[0]])
    P.op("dve", lambda e: e.tensor_tensor(out=r32[:], in0=src, in1=outs[0][:], op=ALU.subtract),
         rd=[Tsrc, Touts[0]], wr=[Tr32])
    P.op("dve", lambda e: e.tensor_copy(out=outs[1][:], in_=r32[:]), rd=[Tr32], wr=[Touts[1]])
    P.op("dve", lambda e: e.tensor_tensor(out=outs[2][:], in0=r32[:], in1=outs[1][:], op=ALU.subtract),
         rd=[Tr32, Touts[1]], wr=[Touts[2]])


def l1_proj_stage(P, C, x_in, W, sel_ap, scratch):
    x_in_ap, Tx_in = x_in
    KTd, VAd, NCd, CRd, QTd, SGd, XOd = [scratch[k] for k in ("KT", "VA", "NC", "CR", "QT", "SG", "XO")]
    BK = Banks(C)
    w_in, Tw_in = load_weight_bf16(P, "l1p_w_in", W["w_in"], D, 4112)
    gexp, Tgexp = load_gexp(P, C, "l1p_gn", W["norm_mix"])

    def sbt(name, shape, dt=F32):
        return P.sb("l1p_" + name, shape, dt), T("l1p_" + name)

    maskb, Tmaskb = sbt("maskb", [128, 128], BF16)
    P.op("pool", lambda e: e.memset(maskb[:], 1.0), wr=[Tmaskb])
    P.op("pool", lambda e: e.affine_select(out=maskb[:], in_=maskb[:], pattern=[[1, 128]], compare_op=ALU.is_ge,
                                           fill=0.0, base=0, channel_multiplier=-1), rd=[Tmaskb], wr=[Tmaskb])
    onesb, Tonesb = sbt("onesb", [128, 128], BF16)
    P.op("pool", lambda e: e.memset(onesb[:], 1.0), wr=[Tonesb])
    e127, Te127 = sbt("e127", [128, 128], BF16)
    P.op("pool", lambda e: e.memset(e127[:], 1.0), wr=[Te127])
    P.op("pool", lambda e: e.affine_select(out=e127[:], in_=e127[:], pattern=[[0, 128]], compare_op=ALU.is_equal,
                                           fill=0.0, base=-64, channel_multiplier=1), rd=[Te127], wr=[Te127])
    sel, Tsel = sbt("sel", [128, 2])
    P.dma("sp", sel[:], sel_ap, wr=[Tsel])
    kg, Tkg = sbt("kg", [128, 64])
    qg, Tqg = sbt("qg", [128, 64])
    fbr, Tfbr = sbt("fbr", [128, 16])
    P.dma("sp", kg[:], W["k_g"].partition_broadcast(128), wr=[Tkg])
    P.dma("sp", qg[:], W["q_g"].partition_broadcast(128), wr=[Tqg])
    P.dma("sp", fbr[:], W["forget_bias"].partition_broadcast(128), wr=[Tfbr])
    A, TA = sbt("A", [128, 16])
    P.op("pool", lambda e: e.memset(A[:], 0.0), wr=[TA])
    xb = [sbt("xb%d" % i, [128, D]) for i in range(4)]
    cum = [sbt("cum%d" % i, [128, 16]) for i in range(4)]
    xown, Txown = sbt("xown", [128, D])
    cown, Tcown = sbt("cown", [128, 16])
    junk, Tjunk = sbt("junk", [128, D], BF16)
    KTb = [sbt("KTb%d" % i, [128, 8, 128], BF16) for i in range(2)]
    VAb = [sbt("VAb%d" % i, [128, 16, 65], BF16) for i in range(2)]
    for i in range(2):
        P.op("pool", lambda e, i=i: e.memset(VAb[i][0][:], 1.0), wr=[VAb[i][1]])
    SGb, TSGb = sbt("SGb", [128, D], BF16)
    l32, Tl32 = sbt("l32", [128, 16])
    r32, Tr32 = sbt("r32", [128, 16])
    ls = [sbt("ls%d" % i, [128, 16], BF16) for i in range(3)]
    As = [sbt("As%d" % i, [128, 16], BF16) for i in range(3)]
    cs3 = [sbt("cs3%d" % i, [128, 16], BF16) for i in range(3)]
    crb, Tcrb = sbt("crb", [128, 16])
    TKT, TVA, TNC, TCR, TQT, TSG, TXO = [scratch["T" + k] for k in ("KT", "VA", "NC", "CR", "QT", "SG", "XO")]

    def mkset(i):
        d = {}
        d["ss"] = sbt("ss_%d" % i, [128, 1])
        d["rstd"] = sbt("rstd_%d" % i, [128, 1])
        d["xn"] = sbt("xn_%d" % i, [128, D])
        d["hT"] = sbt("hT_%d" % i, [128, 8, 128], BF16)
        d["kf"] = sbt("kf_%d" % i, [128, D])
        d["tmp"] = sbt("tmp_%d" % i, [128, D])
        d["kn"] = sbt("kn_%d" % i, [128, D])
        d["ss16"] = sbt("ss16_%d" % i, [128, 16])
        d["BK"] = HalfBanks(C, 4 * i)
        return d

    sets = [mkset(0), mkset(1)]

    def block(b):
        d = sets[b % 2]
        BK = d["BK"]
        hT, ThT = d["hT"]
        scr = (d["ss"][0], d["ss"][1], d["rstd"][0], d["rstd"][1], junk, Tjunk, d["xn"][0], d["xn"][1], hT, ThT)
        (kf, Tkf), (tmp, Ttmp), (kn, Tkn), (ss16, Tss16) = [d[k] for k in ("kf", "tmp", "kn", "ss16")]

        def proj(col0, ncols=512):
            bk, Tbk = BK.next()
            for c in range(8):
                P.op("pe", lambda e, c=c, bk=bk: e.matmul(bk[:, 0:ncols], lhsT=hT[:, c, :],
                                                          rhs=w_in[:, c, col0:col0 + ncols],
                                                          start=(c == 0), stop=(c == 7)), rd=[ThT, Tw_in], wr=[Tbk])
            return bk, Tbk

        x, Txb = xb[b % 4]
        cm, Tcm = cum[b % 4]
        P.dma("act", x[:], x_in_ap[b * 128:(b + 1) * 128, :], rd=[Tx_in], wr=[Txb])
        norm_T(P, C, BK, "l1p", x[:], Txb, gexp, Tgexp, scr)
        yield
        for half in range(2):
            bk, Tbk = proj(1024 + half * 512)
            P.op("act", lambda e, bk=bk, half=half: e.activation(out=kf[:, half * 512:(half + 1) * 512], in_=bk[:],
                                                                func=AF.Copy), rd=[Tbk], wr=[Tkf])
            yield
        head_norm(P, "k", kf, Tkf, kg, Tkg, kn, Tkn, tmp, Ttmp, ss16, Tss16, 1.0)
        yield
        ktb, Tktb = KTb[b % 2]
        transpose_to(P, C, BK, kn, Tkn, 1024, ktb, Tktb)
        P.dma("sp", KTd.rearrange("h p t -> p h t")[:, :, b * 128:(b + 1) * 128], ktb[:], rd=[Tktb], wr=[TKT], store=True)
        yield
        vab, Tvab = VAb[b % 2]
        for half in range(2):
            bk, Tbk = proj(2048 + half * 512)
            P.op("act", lambda e, bk=bk, half=half, vab=vab: e.activation(
                out=vab[:, half * 8:(half + 1) * 8, 0:64], in_=bk[:].rearrange("p (h d) -> p h d", h=8),
                func=AF.Copy), rd=[Tbk], wr=[Tvab])
            yield
        P.dma("sp", VAd[b * 128:(b + 1) * 128, :], vab[:].rearrange("p h d -> p (h d)"), rd=[Tvab], wr=[TVA], store=True)
        bkf, Tbkf = proj(4096, 16)
        yield "MID"
        P.op("dve", lambda e, bkf=bkf: e.tensor_tensor(out=l32[:], in0=bkf[:, 0:16], in1=fbr[:], op=ALU.add),
             rd=[Tbkf, Tfbr], wr=[Tl32])
        P.op("act", lambda e: e.activation(out=l32[:], in_=l32[:], func=AF.Exp, scale=-1.0), rd=[Tl32], wr=[Tl32])
        P.op("act", lambda e: e.activation(out=l32[:], in_=l32[:], func=AF.Ln, bias=1.0), rd=[Tl32], wr=[Tl32])
        yield
        split3(P, "l", l32[:], Tl32, [t[0] for t in ls], [t[1] for t in ls], r32, Tr32)
        yield
        split3(P, "A", A[:], TA, [t[0] for t in As], [t[1] for t in As], r32, Tr32)
        yield
        bk, Tbk = BK.next()
        for i in range(3):
            P.op("pe", lambda e, i=i, bk=bk: e.matmul(bk[:, 0:16], lhsT=maskb[:], rhs=ls[i][0][:], start=(i == 0),
                                                      stop=False), rd=[Tmaskb, ls[i][1]], wr=[Tbk])
        for i in range(3):
            P.op("pe", lambda e, i=i, bk=bk: e.matmul(bk[:, 0:16], lhsT=onesb[:], rhs=As[i][0][:], start=False,
                                                      stop=(i == 2)), rd=[Tonesb, As[i][1]], wr=[Tbk])
        P.op("act", lambda e, bk=bk, cm=cm: e.activation(out=cm[:], in_=bk[:, 0:16], func=AF.Copy), rd=[Tbk], wr=[Tcm])
        P.op("dve", lambda e: e.tensor_tensor(out=A[:], in0=A[:], in1=l32[:], op=ALU.add), rd=[TA, Tl32], wr=[TA])
        P.dma("sp", NCd[b * 128:(b + 1) * 128, :], cm[:], rd=[Tcm], wr=[TNC], store=True)
        yield
        if b % 2 == 0:
            return
        j = b // 2
        xa, Txa = xb[(b - 1) % 4]
        xb_, Txb_ = xb[b % 4]
        ca, Tca = cum[(b - 1) % 4]
        cb, Tcb = cum[b % 4]
        P.op("dve", lambda e: e.tensor_scalar(out=xown[:], in0=xa[:], scalar1=sel[:, 0:1], scalar2=None,
                                              op0=ALU.mult), rd=[Txa, Tsel], wr=[Txown])
        P.op("dve", lambda e: e.scalar_tensor_tensor(out=xown[:], in0=xb_[:], scalar=sel[:, 1:2], in1=xown[:],
                                                     op0=ALU.mult, op1=ALU.add), rd=[Txb_, Tsel, Txown], wr=[Txown])
        P.dma("sp", XOd[j * 128:(j + 1) * 128, :], xown[:], rd=[Txown], wr=[TXO], store=True)
        yield
        P.op("dve", lambda e: e.tensor_scalar(out=cown[:], in0=ca[:], scalar1=sel[:, 0:1], scalar2=None,
                                              op0=ALU.mult), rd=[Tca, Tsel], wr=[Tcown])
        P.op("dve", lambda e: e.scalar_tensor_tensor(out=cown[:], in0=cb[:], scalar=sel[:, 1:2], in1=cown[:],
                                                     op0=ALU.mult, op1=ALU.add), rd=[Tcb, Tsel, Tcown], wr=[Tcown])
        split3(P, "c", cown[:], Tcown, [t[0] for t in cs3], [t[1] for t in cs3], r32, Tr32)
        yield
        bk, Tbk = BK.next()
        for i in range(3):
            P.op("pe", lambda e, i=i, bk=bk: e.matmul(bk[:, 0:16], lhsT=e127[:], rhs=cs3[i][0][:], start=(i == 0),
                                                      stop=(i == 2)), rd=[Te127, cs3[i][1]], wr=[Tbk])
        P.op("act", lambda e, bk=bk: e.activation(out=crb[:], in_=bk[:, 0:16], func=AF.Copy), rd=[Tbk], wr=[Tcrb])
        P.dma("sp", CRd[j], crb[:], rd=[Tcrb], wr=[TCR], store=True)
        yield
        norm_T(P, C, BK, "l1p", xown[:], Txown, gexp, Tgexp, scr)
        yield
        for half in range(2):
            bk, Tbk = proj(half * 512)
            P.op("act", lambda e, bk=bk, half=half: e.activation(out=kf[:, half * 512:(half + 1) * 512], in_=bk[:],
                                                                func=AF.Copy), rd=[Tbk], wr=[Tkf])
            yield
        head_norm(P, "q", kf, Tkf, qg, Tqg, kn, Tkn, tmp, Ttmp, ss16, Tss16, 0.125)
        yield
        qtb, Tqtb = KTb[b % 2]
        transpose_to(P, C, BK, kn, Tkn, 1024, qtb, Tqtb)
        P.dma("sp", QTd.rearrange("h p t -> p h t")[:, :, j * 128:(j + 1) * 128], qtb[:], rd=[Tqtb], wr=[TQT], store=True)
        yield
        for half in range(2):
            bk, Tbk = proj(3072 + half * 512)
            P.op("act", lambda e, bk=bk, half=half: e.activation(out=SGb[:, half * 512:(half + 1) * 512], in_=bk[:],
                                                                func=AF.Sigmoid), rd=[Tbk], wr=[TSGb])
            yield
        P.dma("sp", SGd[j * 128:(j + 1) * 128, :], SGb[:], rd=[TSGb], wr=[TSG], store=True)

    run_pipeline(block(b) for b in range(32))


def l1_attn_stage(P, C, x_out, W, msk_ap, scratch, sel_ap):
    x_out_ap, Tx_out = x_out
    KTd, VAd, NCd, CRd, QTd, SGd, XOd = [scratch[k] for k in ("KT", "VA", "NC", "CR", "QT", "SG", "XO")]
    TKT, TVA, TNC, TCR, TQT, TSG, TXO = [scratch["T" + k] for k in ("KT", "VA", "NC", "CR", "QT", "SG", "XO")]
    BK = Banks(C)
    w_o, Tw_o = load_weight_bf16(P, "l1a_w_o", W["w_o"], D, D)

    def sbt(name, shape, dt=F32):
        return P.sb("l1a_" + name, shape, dt), T("l1a_" + name)

    msk32, Tmsk32 = sbt("msk32", [128, 2, 128])
    P.dma("sp", msk32[:], msk_ap.rearrange("m s t -> s m t"), wr=[Tmsk32])
    mskb, Tmskb = sbt("mskb", [128, 2, 128], BF16)
    P.op("pool", lambda e: e.tensor_copy(out=mskb[:], in_=msk32[:]), rd=[Tmsk32], wr=[Tmskb])
    NC_, TNC_ = sbt("NC", [128, 32, 16])
    P.dma("sp", NC_[:], NCd.rearrange("(b p) h -> p b h", p=128), rd=[TNC], wr=[TNC_])
    CR_, TCR_ = sbt("CR", [128, 16, 16])
    P.dma("sp", CR_[:], CRd.rearrange("j p h -> p j h"), rd=[TCR], wr=[TCR_])
    jj = [(j, J) for j in range(16) for J in range(2 * j + 2)]
    bidx = {k: i for i, k in enumerate(jj)}
    bias, Tbias = sbt("bias", [128, len(jj), 16])
    for j in range(16):
        nJ = 2 * j + 2
        i0 = bidx[(j, 0)]
        P.op("dve", lambda e, j=j, nJ=nJ, i0=i0: e.tensor_tensor(
            out=bias[:, i0:i0 + nJ, :], in0=NC_[:, 0:nJ, :],
            in1=CR_[:, j:j + 1, :].to_broadcast([128, nJ, 16]), op=ALU.subtract), rd=[TNC_, TCR_], wr=[Tbias])
    sel, Tsel = sbt("sel", [128, 2])
    P.dma("sp", sel[:], sel_ap, wr=[Tsel])
    nbig, Tnbig = sbt("nbig", [128, 1])
    P.op("dve", lambda e: e.tensor_scalar(out=nbig[:], in0=sel[:, 0:1], scalar1=-30000.0, scalar2=None, op0=ALU.mult),
         rd=[Tsel], wr=[Tnbig])
    for j in range(16):
        bi_ = bidx[(j, 2 * j + 1)]
        P.op("dve", lambda e, bi_=bi_: e.tensor_scalar(out=bias[:, bi_, :], in0=bias[:, bi_, :], scalar1=nbig[:, 0:1],
                                                     scalar2=None, op0=ALU.add), rd=[Tbias, Tnbig], wr=[Tbias])
    nb_ = len(jj)
    bm, Tbm = sbt("bm", [128, nb_, 8])
    b4 = bias[:].rearrange("p n (hp two) -> p n hp two", two=2)
    P.op("dve", lambda e: e.tensor_tensor(out=bm[:], in0=b4[:, :, :, 0], in1=b4[:, :, :, 1], op=ALU.max),
         rd=[Tbias], wr=[Tbm])
    P.op("dve", lambda e: e.tensor_tensor(out=b4, in0=b4, in1=bm[:].unsqueeze(3).to_broadcast([128, nb_, 8, 2]),
                                          op=ALU.subtract), rd=[Tbias, Tbm], wr=[Tbias])
    P.op("act", lambda e: e.activation(out=bias[:], in_=bias[:], func=AF.Exp), rd=[Tbias], wr=[Tbias])
    vsb = [sbt("vs%d" % i, [128, 2, 65], BF16) for i in range(4)]
    QTP = [sbt("QTP%d" % i, [128, 16, 2, 128], BF16) for i in range(2)]
    for i in range(2):
        P.op("pool", lambda e, i=i: e.memset(QTP[i][0][:], 0.0), wr=[QTP[i][1]])
    oT, ToT = sbt("oT", [128, 8, 2048], BF16)
    KT = [sbt("KT%d" % i, [128, 4096], BF16) for i in range(2)]
    VA = [sbt("VA%d" % i, [128, 32, 130], BF16) for i in range(2)]
    SG = [sbt("SG%d" % i, [128, 16, 128], BF16) for i in range(2)]
    pT = [sbt("pT%d" % i, [128, 2, 128], BF16) for i in range(4)]
    rec, Trec = sbt("rec", [128, 2])
    o2s = [sbt("o2_%d" % i, [128, 128]) for i in range(2)]
    xo = [sbt("xo%d" % i, [128, D]) for i in range(2)]
    xr = [sbt("xr%d" % i, [128, D]) for i in range(2)]
    pcount = 0
    pocount = 0
    if 'a_noloop' in DBG:
        P.op("pool", lambda e: e.memset(oT[:], 0.0), wr=[ToT])
    for hp in range(8 if 'a_noloop' not in DBG else 0):
        kt, Tkt = KT[hp % 2]
        va, Tva = VA[hp % 2]
        sg, Tsg = SG[hp % 2]
        P.dma("sp", kt[:], KTd[hp], rd=[TKT], wr=[Tkt])
        qtp, Tqtp = QTP[hp % 2]
        for h2 in range(2):
            P.dma("sp", qtp[h2 * 64:(h2 + 1) * 64, :, h2, :],
                  QTd[hp][h2 * 64:(h2 + 1) * 64, :].rearrange("p (j t) -> p j t", t=128), rd=[TQT], wr=[Tqtp])
        P.dma("sp", va[:].rearrange("p b (h d) -> p b h d", h=2),
              VAd.rearrange("(b p) (h d) -> p b h d", p=128, d=65)[:, :, 2 * hp:2 * hp + 2, :], rd=[TVA], wr=[Tva])
        P.dma("sp", sg[:], SGd.rearrange("(j p) c -> p j c", p=128)[:, :, hp * 128:(hp + 1) * 128], rd=[TSG], wr=[Tsg])
        items = [(j, J) for j in range(16) for J in range(2 * j + 2)]
        LOOK = 2

        def emit_qk(i, kt=kt, Tkt=Tkt, qtp=qtp, Tqtp=Tqtp):
            j, J = items[i]
            ps, Tps = C.bank[i % 3], C.Tbank[i % 3]
            P.op("pe", lambda e, ps=ps, J=J, j=j: e.matmul(
                ps[:, 0:256], lhsT=kt[:, J * 128:(J + 1) * 128],
                rhs=qtp[:, j, :, :].rearrange("p a t -> p (a t)"), start=True, stop=True),
                 rd=[Tkt, Tqtp], wr=[Tps])

        def emit_tr(pend):
            j_, o2_, To2_ = pend
            bk, Tbk = C.bank[3], C.Tbank[3]
            P.op("pe", lambda e, bk=bk, o2_=o2_: e.transpose(out=bk[:, 0:128], in_=o2_[:], identity=C.ident[:]),
                 rd=[To2_, C.Tident], wr=[Tbk])
            P.op("act", lambda e, bk=bk, hp=hp, j_=j_: e.activation(out=oT[:, hp, j_ * 128:(j_ + 1) * 128],
                                                                    in_=bk[:, 0:128], func=AF.Copy), rd=[Tbk], wr=[ToT])

        for i in range(LOOK):
            emit_qk(i)
        pending = None
        for i, (j, J) in enumerate(items):
            nJ = 2 * j + 2
            if i + LOOK < len(items):
                emit_qk(i + LOOK)
            ps, Tps = C.bank[i % 3], C.Tbank[i % 3]
            p_, Tp_ = pT[i % 4]
            if J == 0:
                pob = [(C.bank[4 + 2 * (pocount % 2) + h2], C.Tbank[4 + 2 * (pocount % 2) + h2]) for h2 in range(2)]
                o2, To2 = o2s[pocount % 2]
                pocount += 1
            bi = bidx[(j, J)]
            P.op("act", lambda e, ps=ps, p_=p_, bi=bi, hp=hp: e.activation(
                out=p_[:].rearrange("p a t -> p (a t)"), in_=ps[:, 0:256], func=AF.Exp,
                bias=bm[:, bi, hp:hp + 1]), rd=[Tps, Tbm], wr=[Tp_])
            vs, Tvs = vsb[i % 4]
            P.op("dve", lambda e, vs=vs, va=va, J=J, bi=bi, hp=hp: e.tensor_tensor(
                out=vs[:], in0=va[:, J, :].rearrange("p (h d) -> p h d", h=2),
                in1=bias[:, bi, 2 * hp:2 * hp + 2].unsqueeze(2).to_broadcast([128, 2, 65]), op=ALU.mult),
                 rd=[Tva, Tbias], wr=[Tvs])
            if J >= 2 * j:
                m = J - 2 * j
                P.op("pool", lambda e, p_=p_, m=m: e.tensor_tensor(
                    out=p_[:], in0=p_[:], in1=mskb[:, m:m + 1, :].to_broadcast([128, 2, 128]), op=ALU.mult),
                     rd=[Tp_, Tmskb], wr=[Tp_])
            for h2 in range(2):
                po, Tpo = pob[h2]
                P.op("pe", lambda e, po=po, h2=h2, p_=p_, vs=vs, J=J, nJ=nJ: e.matmul(
                    po[:, 0:65], lhsT=p_[:, h2, :], rhs=vs[:, h2, :],
                    start=(J == 0), stop=(J == nJ - 1)), rd=[Tp_, Tvs], wr=[Tpo])
            if pending is not None and J == 1:
                emit_tr(pending)
                pending = None
            if J == nJ - 1:
                for h2 in range(2):
                    po, Tpo = pob[h2]
                    P.op("dve", lambda e, po=po, h2=h2: e.reciprocal(out=rec[:, h2:h2 + 1], in_=po[:, 64:65]),
                         rd=[Tpo], wr=[Trec])
                    P.op("dve", lambda e, po=po, h2=h2, sg=sg, j=j, o2=o2: e.scalar_tensor_tensor(
                        out=o2[:, h2 * 64:(h2 + 1) * 64], in0=po[:, 0:64], scalar=rec[:, h2:h2 + 1],
                        in1=sg[:, j, h2 * 64:(h2 + 1) * 64], op0=ALU.mult, op1=ALU.mult),
                         rd=[Tpo, Trec, Tsg], wr=[To2])
                pending = (j, o2, To2)
        emit_tr(pending)
    for j in range(16):
        xr_, Txr = xr[j % 2]
        xo_, Txo = xo[j % 2]
        P.dma("sp", xr_[:], XOd[j * 128:(j + 1) * 128, :], rd=[TXO], wr=[Txr])
        for half in range(2):
            pw, Tpw = BK.next()
            for c in range(8):
                P.op("pe", lambda e, c=c, pw=pw, half=half, j=j: e.matmul(
                    pw[:], lhsT=oT[:, c, j * 128:(j + 1) * 128], rhs=w_o[:, c, half * 512:(half + 1) * 512],
                    start=(c == 0), stop=(c == 7)), rd=[ToT, Tw_o], wr=[Tpw])
            P.op("dve", lambda e, pw=pw, half=half, xr_=xr_, xo_=xo_: e.tensor_tensor(
                out=xo_[:, half * 512:(half + 1) * 512], in0=pw[:], in1=xr_[:, half * 512:(half + 1) * 512], op=ALU.add),
                 rd=[Tpw, Txr], wr=[Txo])
        P.dma("act", x_out_ap[j * 128:(j + 1) * 128, :], xo_[:], rd=[Txo], wr=[Tx_out], store=True)


W_SHAPES = {
    "even_norm_mix": [D], "even_w_in": [D, 2576], "even_gate_up": [16, 256], "even_gate_bias": [256],
    "even_w_s": [4, 128, 128], "even_b_s": [4, 128], "even_ln_g": [512], "even_ln_b": [512],
    "even_head_g": [4, 128], "even_w_o": [D, D], "even_norm_ffn": [D], "even_ffn_w1": [1, D, 2816],
    "even_ffn_w3": [1, D, 2816], "even_ffn_w2": [1, 2816, D], "odd_norm_mix": [D], "odd_w_in": [D, 4112],
    "odd_forget_bias": [16], "odd_q_g": [64], "odd_k_g": [64], "odd_w_o": [D, D], "odd_norm_ffn": [D],
    "odd_router": [D, 8], "odd_exp_w1": [8, D, 3584], "odd_exp_w3": [8, D, 3584], "odd_exp_w2": [8, 3584, D],
    "final_norm": [D],
}
SEQ = 4096


def build_program(stop=None):
    nc = bass.Bass("TRN2", target_bir_lowering=False)
    x = nc.dram_tensor("x", [SEQ, D], F32, kind="ExternalInput").ap()
    Wd = {k: nc.dram_tensor(k, s, F32, kind="ExternalInput").ap() for k, s in W_SHAPES.items()}
    sel = nc.dram_tensor("sel", [128, 2], F32, kind="ExternalInput").ap()
    msk = nc.dram_tensor("msk", [2, 128, 128], F32, kind="ExternalInput").ap()
    y = nc.dram_tensor("y", [SEQ if stop in ("l0m", "l0f") else SEQ // 2, D], F32, kind="ExternalOutput").ap()
    xmid = y if stop == "l0m" else nc.dram_tensor("xmid", [SEQ, D], F32).ap()
    x1 = y if stop == "l0f" else nc.dram_tensor("x1", [SEQ, D], F32).ap()
    x2 = y if stop == "l1a" else nc.dram_tensor("x2", [SEQ // 2, D], F32).ap()
    scratch = {
        "KT": nc.dram_tensor("KTd", [8, 128, SEQ], BF16).ap(),
        "VA": nc.dram_tensor("VAd", [SEQ, 16 * 65], BF16).ap(),
        "NC": nc.dram_tensor("NCd", [SEQ, 16], F32).ap(),
        "CR": nc.dram_tensor("CRd", [16, 128, 16], F32).ap(),
        "QT": nc.dram_tensor("QTd", [8, 128, SEQ // 2], BF16).ap(),
        "SG": nc.dram_tensor("SGd", [SEQ // 2, D], BF16).ap(),
        "XO": nc.dram_tensor("XOd", [SEQ // 2, D], F32).ap(),
    }
    for k in list(scratch.keys()):
        scratch["T" + k] = T(k)
    with ExitStack() as es:
        P = Prog(nc, es)
        C = Ctx(P)
        Tx, Txmid, Tx1, Tx2, Ty = T("x"), T("xmid"), T("x1"), T("x2"), T("y")
        P.emit()
        _skip = ""
        with P.stage():
            l0_mixer_stage(P, C, (x, Tx), (xmid, Txmid), SEQ if "l0m" not in _skip else 256, {
                "norm_mix": Wd["even_norm_mix"], "w_in": Wd["even_w_in"], "gate_up": Wd["even_gate_up"],
                "gate_bias": Wd["even_gate_bias"], "w_s": Wd["even_w_s"], "b_s": Wd["even_b_s"],
                "ln_g": Wd["even_ln_g"], "ln_b": Wd["even_ln_b"], "head_g": Wd["even_head_g"],
                "w_o": Wd["even_w_o"]})
            P.wait_all("sp", [Txmid])
        if stop == "l0m":
            return nc
        with P.stage():
            swiglu_stage(P, C, "f0", (xmid, Txmid), (x1, Tx1), SEQ, Wd["even_norm_ffn"], Wd["even_ffn_w1"],
                         Wd["even_ffn_w3"], Wd["even_ffn_w2"], 2816)
            P.wait_all("sp", [Tx1])
        if stop == "l0f":
            return nc
        with P.stage():
            l1_proj_stage(P, C, (x1, Tx1), {"norm_mix": Wd["odd_norm_mix"], "w_in": Wd["odd_w_in"],
                                            "forget_bias": Wd["odd_forget_bias"], "q_g": Wd["odd_q_g"],
                                            "k_g": Wd["odd_k_g"]}, sel, scratch)
            P.wait_all("sp", [scratch["T" + k] for k in ("KT", "VA", "NC", "CR", "QT", "SG", "XO")])
        with P.stage():
            l1_attn_stage(P, C, (x2, Tx2), {"w_o": Wd["odd_w_o"]}, msk, scratch, sel)
            P.wait_all("sp", [Tx2])
        if stop == "l1a":
            return nc
        with P.stage():
            swiglu_stage(P, C, "moe", (x2, Tx2), (y, Ty), SEQ // 2, Wd["odd_norm_ffn"], Wd["odd_exp_w1"],
                         Wd["odd_exp_w3"], Wd["odd_exp_w2"], 3584, router=Wd["odd_router"], nexp=8,
                         g_final=Wd["final_norm"])
            P.wait_all("sp", [Ty])
    return nc


def kernel(**inputs):
    x = np.ascontiguousarray(np.asarray(inputs["x"], dtype=np.float32))
    wmap = {}
    for k, shp in W_SHAPES.items():
        a = np.asarray(inputs[k], dtype=np.float32)
        if k in ("even_ffn_w1", "even_ffn_w3", "even_ffn_w2", "odd_exp_w1", "odd_exp_w3", "odd_exp_w2"):
            a = a.reshape(shp)
        elif k != "final_norm":
            a = a[0]
        wmap[k] = np.ascontiguousarray(a.reshape(shp))
    tril = np.triu(np.ones((128, 128), np.float32))
    msks = [np.stack([tril, np.zeros_like(tril)]), np.stack([np.ones_like(tril), tril])]
    sels = [np.tile(np.array([[1.0, 0.0]], np.float32), (128, 1)), np.tile(np.array([[0.0, 1.0]], np.float32), (128, 1))]
    nc = build_program()
    in_maps = []
    for core in range(8):
        b, par = core // 2, core % 2
        m = {"x": x[b], "sel": sels[par], "msk": msks[par]}
        m.update(wmap)
        in_maps.append(m)
    res = run_bass_kernel_spmd(nc, in_maps, core_ids=list(range(8)))
    out = np.empty((4, SEQ, D), np.float32)
    for core in range(8):
        b, par = core // 2, core % 2
        yv = np.asarray(res.results[core]["y"]).reshape(16, 128, D)
        out[b].reshape(16, 2, 128, D)[:, par] = yv
    return out


SCR_SPECS = {"KT": ([8, 128, SEQ], BF16), "VA": ([SEQ, 16 * 65], BF16), "NC": ([SEQ, 16], F32),
             "CR": ([16, 128, 16], F32), "QT": ([8, 128, SEQ // 2], BF16), "SG": ([SEQ // 2, D], BF16),
             "XO": ([SEQ // 2, D], F32)}
STAGE_W = {
    "l0m": ["even_norm_mix", "even_w_in", "even_gate_up", "even_gate_bias", "even_w_s", "even_b_s", "even_ln_g",
            "even_ln_b", "even_head_g", "even_w_o"],
    "l0f": ["even_norm_ffn", "even_ffn_w1", "even_ffn_w3", "even_ffn_w2"],
    "l1p": ["odd_norm_mix", "odd_w_in", "odd_forget_bias", "odd_q_g", "odd_k_g"],
    "l1a": ["odd_w_o"],
    "moe": ["odd_norm_ffn", "odd_router", "odd_exp_w1", "odd_exp_w3", "odd_exp_w2", "final_norm"],
}


def build_single(stage):
    nc = bass.Bass("TRN2", target_bir_lowering=False)
    Wd = {k: nc.dram_tensor(k, W_SHAPES[k], F32, kind="ExternalInput").ap() for k in STAGE_W[stage]}
    n_in = SEQ if stage in ("l0m", "l0f", "l1p") else SEQ // 2
    n_out = SEQ if stage in ("l0m", "l0f") else SEQ // 2
    xin, y, scratch = None, None, {}
    if stage != "l1a":
        xin = nc.dram_tensor("xin", [n_in, D], F32, kind="ExternalInput").ap()
    if stage != "l1p":
        y = nc.dram_tensor("y", [n_out, D], F32, kind="ExternalOutput").ap()
    if stage in ("l1p", "l1a"):
        kind = "ExternalOutput" if stage == "l1p" else "ExternalInput"
        for k, (shp, dt) in SCR_SPECS.items():
            scratch[k] = nc.dram_tensor("s_" + k, shp, dt, kind=kind).ap()
            scratch["T" + k] = T(k)
    if stage in ("l1p", "l1a"):
        sel = nc.dram_tensor("sel", [128, 2], F32, kind="ExternalInput").ap()
    if stage == "l1a":
        msk = nc.dram_tensor("msk", [2, 128, 128], F32, kind="ExternalInput").ap()
    with ExitStack() as es:
        P = Prog(nc, es)
        C = Ctx(P)
        Tx, Ty = T("xin"), T("y")
        P.emit()
        with P.stage():
            if stage == "l0m":
                l0_mixer_stage(P, C, (xin, Tx), (y, Ty), SEQ, {k[5:]: Wd[k] for k in STAGE_W[stage]})
                P.wait_all("sp", [Ty])
            elif stage == "l0f":
                swiglu_stage(P, C, "f0", (xin, Tx), (y, Ty), SEQ, Wd["even_norm_ffn"], Wd["even_ffn_w1"],
                             Wd["even_ffn_w3"], Wd["even_ffn_w2"], 2816)
                P.wait_all("sp", [Ty])
            elif stage == "l1p":
                l1_proj_stage(P, C, (xin, Tx), {k[4:]: Wd[k] for k in STAGE_W[stage]}, sel, scratch)
                P.wait_all("sp", [scratch["T" + k] for k in SCR_SPECS])
            elif stage == "l1a":
                l1_attn_stage(P, C, (y, Ty), {"w_o": Wd["odd_w_o"]}, msk, scratch, sel)
                P.wait_all("sp", [Ty])
            elif stage == "moe":
                swiglu_stage(P, C, "moe", (xin, Tx), (y, Ty), SEQ // 2, Wd["odd_norm_ffn"], Wd["odd_exp_w1"],
                             Wd["odd_exp_w3"], Wd["odd_exp_w2"], 3584, router=Wd["odd_router"], nexp=8,
                             g_final=Wd["final_norm"])
                P.wait_all("sp", [Ty])
    return nc


def kernel_unfused(**inputs):
    x = np.ascontiguousarray(np.asarray(inputs["x"], dtype=np.float32))
    wmap = {k: np.ascontiguousarray(np.asarray(inputs[k], dtype=np.float32).reshape(shp)) for k, shp in W_SHAPES.items()}
    tril = np.triu(np.ones((128, 128), np.float32))
    msks = [np.stack([tril, np.zeros_like(tril)]), np.stack([np.ones_like(tril), tril])]
    sels = [np.tile(np.array([[1.0, 0.0]], np.float32), (128, 1)), np.tile(np.array([[0.0, 1.0]], np.float32), (128, 1))]
    cores = list(range(8))
    cur = [{"xin": x[c // 2]} for c in cores]
    for stage in ("l0m", "l0f", "l1p", "l1a", "moe"):
        nc = build_single(stage)
        in_maps = []
        for c in cores:
            m = dict(cur[c])
            for k in STAGE_W[stage]:
                m[k] = wmap[k]
            if stage in ("l1p", "l1a"):
                m["sel"] = sels[c % 2]
            if stage == "l1a":
                m["msk"] = msks[c % 2]
            in_maps.append(m)
        res = run_bass_kernel_spmd(nc, in_maps, core_ids=cores)
        if stage == "l1p":
            cur = [{"s_" + k: np.asarray(res.results[c]["s_" + k]) for k in SCR_SPECS} for c in cores]
        else:
            cur = [{"xin": np.asarray(res.results[c]["y"])} for c in cores]
    out = np.empty((4, SEQ, D), np.float32)
    for c in cores:
        b, par = c // 2, c % 2
        out[b].reshape(16, 2, 128, D)[:, par] = cur[c]["xin"].reshape(16, 128, D)
    return out
```

```python
import numpy as np
from contextlib import ExitStack, contextmanager
import concourse.bass as bass
import concourse.mybir as mybir
from concourse.bass_utils import run_bass_kernel_spmd

F32 = mybir.dt.float32
BF16 = mybir.dt.bfloat16
I32 = mybir.dt.int32
AF = mybir.ActivationFunctionType
ALU = mybir.AluOpType
AX = mybir.AxisListType


class T:
    __slots__ = ("name", "w", "r", "dsem", "dcnt")

    def __init__(self, name):
        self.name = name
        self.w = {}
        self.r = {}
        self.dsem = None
        self.dcnt = 0


class Prog:
    ENGS = ("pe", "act", "dve", "pool", "sp")

    def __init__(self, nc, es):
        self.nc = nc
        self.es = es
        self.es_outer = es
        self.ops = {e: [] for e in self.ENGS}
        self.seen = {e: {} for e in self.ENGS}
        self.ndsem = 0
        self.dsems = []

    @contextmanager
    def stage(self):
        with ExitStack() as es:
            old = self.es
            self.es = es
            yield
            self.emit()
            self.es = old

    def sb(self, name, shape, dtype):
        nb = 1
        for d_ in shape[1:]:
            nb *= d_
        nb *= 2 if dtype == BF16 else 4
        self.sb_bytes = getattr(self, "sb_bytes", 0) + ((nb + 31) // 32) * 32
        self.sb_log = getattr(self, "sb_log", [])
        self.sb_log.append((self.es, ((nb + 31) // 32) * 32))
        live = sum(b for (es_, b) in self.sb_log if es_ is self.es or es_ is self.es_outer)
        assert live <= 196 * 1024, "SBUF budget exceeded: %d" % live
        return self.es.enter_context(self.nc.sbuf_tensor(name, list(shape), dtype))

    def ps(self, name, shape, dtype):
        return self.es.enter_context(self.nc.psum_tensor(name, list(shape), dtype))

    def _dsem(self, t):
        if t.dsem is None:
            t.dsem = self.ndsem
            self.ndsem += 1
        return t.dsem

    def _deps(self, eng, rd, wr, is_dma_group_sem=None, merge=False):
        deps = []
        for t in rd:
            for tok in t.w.values():
                deps.append((tok, True))
        for t in wr:
            if not merge:
                for tok in t.w.values():
                    if not (is_dma_group_sem is not None and tok[0] == "D" and tok[1] == is_dma_group_sem):
                        deps.append((tok, False))
            for tok in t.r.values():
                deps.append((tok, False))
        waits = []
        seen = self.seen[eng]
        for tok, raw in deps:
            key = (tok[0], tok[1])
            if tok[0] == "E" and tok[1] == eng and not raw:
                continue
            if seen.get(key, -1) >= tok[2]:
                continue
            seen[key] = tok[2]
            waits.append(tok)
        best = {}
        for tok in waits:
            key = (tok[0], tok[1])
            if key not in best or best[key][2] < tok[2]:
                best[key] = tok
        return list(best.values())

    def op(self, eng, fn, rd=(), wr=()):
        rd = [t for t in rd if t is not None]
        wr = [t for t in wr if t is not None]
        waits = self._deps(eng, rd, wr)
        idx = len(self.ops[eng])
        tok = ("E", eng, idx)
        self.ops[eng].append({"fn": fn, "waits": waits, "tok": tok, "need_inc": False})
        for t in rd:
            t.r[("E", eng)] = tok
        for t in wr:
            t.w = {("E", eng): tok}
            t.r = {}
        return tok

    def dma(self, q, out, in_, rd=(), wr=(), **kw):
        rd = [t for t in rd if t is not None]
        wr = [t for t in wr if t is not None]
        store = kw.pop("store", False)
        owner = rd[0] if store else wr[0]
        ds = self._dsem(owner)
        waits = self._deps(q, rd, wr, is_dma_group_sem=ds, merge=store)
        owner.dcnt += 16
        tok = ("D", ds, owner.dcnt)
        self.ops[q].append({"dma": (out, in_, kw), "waits": waits, "tok": tok})
        for t in rd:
            t.r[("D", ds)] = tok
        for t in wr:
            if store:
                t.w[("D", ds)] = tok
            else:
                t.w = {("D", ds): tok}
            t.r = {}
        return tok

    def wait_all(self, eng, tiles):
        waits = self._deps(eng, tiles, [])
        self.ops[eng].append({"fn": None, "waits": waits, "tok": None, "need_inc": False})

    def emit(self):
        nc = self.nc
        if not hasattr(self, "_start"):
            self._start = {e: 0 for e in self.ENGS}
            self._rank = {e: 0 for e in self.ENGS}
            self._esem = {e: self.es_outer.enter_context(nc.semaphore("s_" + e)) for e in self.ENGS}
            self._dsl = []
        start = self._start
        for e in self.ENGS:
            for o in self.ops[e][start[e]:]:
                for tok in o["waits"]:
                    if tok[0] == "E":
                        assert tok[2] >= start[tok[1]], "wait on op from an earlier block"
                        self.ops[tok[1]][tok[2]]["need_inc"] = True
        rank = {}
        for e in self.ENGS:
            c = self._rank[e]
            for i in range(start[e], len(self.ops[e])):
                if self.ops[e][i].get("need_inc"):
                    c += 1
                    rank[(e, i)] = c
            self._rank[e] = c
        while len(self._dsl) < self.ndsem:
            self._dsl.append(self.es_outer.enter_context(nc.semaphore("d%d" % len(self._dsl))))
        esem, dsem = self._esem, self._dsl
        engobj = {"pe": "tensor", "act": "scalar", "dve": "vector", "pool": "gpsimd", "sp": "sync"}

        def body(e):
            def run(engine):
                for i in range(start[e], len(self.ops[e])):
                    o = self.ops[e][i]
                    for tok in o["waits"]:
                        if tok[0] == "E":
                            engine.wait_ge(esem[tok[1]], rank[(tok[1], tok[2])])
                        else:
                            engine.wait_ge(dsem[tok[1]], tok[2])
                    if "dma" in o:
                        out, in_, kw = o["dma"]
                        engine.dma_start(out=out, in_=in_, **kw).then_inc(dsem[o["tok"][1]], 16)
                    elif o["fn"] is not None:
                        ins = o["fn"](engine)
                        if o["need_inc"]:
                            ins.then_inc(esem[e], 1)
            return run

        with nc.Block() as block:
            for e in self.ENGS:
                if len(self.ops[e]) > start[e]:
                    getattr(block, engobj[e])(body(e))
        for e in self.ENGS:
            start[e] = len(self.ops[e])
        for e in self.ENGS:
            for e2 in self.ENGS:
                if self.ops[e2]:
                    self.seen[e][("E", e2)] = len(self.ops[e2]) - 1


import os
DBG = os.environ.get("KDBG", "")

D = 1024
EPS = 1e-6


class Ctx:
    def __init__(self, P):
        self.P = P
        nc = P.nc
        self.bank = [P.ps("bank%d" % i, [128, 512], F32) for i in range(8)]
        self.Tbank = [T("bank%d" % i) for i in range(8)]
        self.ident = P.sb("ident", [128, 128], F32)
        self.Tident = T("ident")
        self.identb = P.sb("identb", [128, 128], BF16)
        self.Tidentb = T("identb")
        self.ones = P.sb("ones", [128, 128], F32)
        self.Tones = T("ones")
        P.op("pool", lambda e: e.memset(self.ones[:], 1.0), wr=[self.Tones])
        P.op("pool", lambda e: e.memset(self.ident[:], 1.0), wr=[self.Tident])
        P.op("pool", lambda e: e.affine_select(out=self.ident[:], in_=self.ident[:], pattern=[[-1, 128]],
                                               compare_op=ALU.is_equal, fill=0.0, base=0,
                                               channel_multiplier=1), rd=[self.Tident], wr=[self.Tident])
        P.op("pool", lambda e: e.tensor_copy(out=self.identb[:], in_=self.ident[:]),
             rd=[self.Tident], wr=[self.Tidentb])


def load_gexp(P, C, name, g_dram):
    gT = P.sb(name + "_gT", [128, 8], F32)
    TgT = T(name + "_gT")
    gexp = P.sb(name + "_gexp", [128, 8, 128], F32)
    Tg = T(name + "_gexp")
    P.dma("sp", gT[:], g_dram.rearrange("(c p) -> p c", p=128), wr=[TgT], allow_slow_non_contiguous=True)
    for c in range(8):
        P.op("dve", lambda e, c=c: e.tensor_scalar(out=gexp[:, c, :], in0=C.ones[:], scalar1=gT[:, c:c + 1],
                                                   scalar2=None, op0=ALU.mult),
             rd=[C.Tones, TgT], wr=[Tg])
    return gexp, Tg


def swiglu_stage(P, C, name, x_in, x_out, ntok, g_norm, w1, w3, w2, dff, router=None, nexp=1,
                 g_final=None):
    nc = P.nc
    x_in_ap, Tx_in = x_in
    x_out_ap, Tx_out = x_out
    NB = 16
    npass = ntok // (NB * 128)
    nff = dff // 128
    groups = []
    f0 = 0
    while f0 < nff:
        gsz = min(4, nff - f0)
        groups.append((f0, gsz))
        f0 += gsz
    moe = router is not None

    gexp, Tgexp = load_gexp(P, C, name + "_gn", g_norm)
    xacc = P.sb(name + "_xacc", [128, NB, D], F32)
    Txacc = [T(name + "_xacc%d" % i) for i in range(NB)]
    hT = P.sb(name + "_hT", [128, 8, NB * 128], BF16)
    ThT = [T(name + "_hT%d" % i) for i in range(NB)]
    hid = P.sb(name + "_hid", [128, 4, NB * 128], BF16)
    Thid = [[T(name + "_hid%d_%d" % (f, t)) for t in range(4)] for f in range(4)]
    junk = P.sb(name + "_junk", [128, D], BF16)
    Tjunk = T(name + "_junk")
    xn = P.sb(name + "_xn", [128, D], F32)
    Txn = T(name + "_xn")
    ss = P.sb(name + "_ss", [128, NB], F32)
    Tss = [T(name + "_ss%d" % i) for i in range(NB)]
    rstd = P.sb(name + "_rstd", [128, NB], F32)
    Trstd = [T(name + "_rstd%d" % i) for i in range(NB)]
    sa = [P.sb(name + "_sa%d" % i, [128, 512], F32) for i in range(2)]
    Tsa = [T(name + "_sa%d" % i) for i in range(2)]
    wbuf = {}
    for par in range(2):
        wbuf[("w1", par)] = (P.sb(name + "_w1g%d" % par, [128, 8, 512], BF16), [T("w1g") for _ in range(8)])
        wbuf[("w3", par)] = (P.sb(name + "_w3g%d" % par, [128, 8, 512], BF16), [T("w3g") for _ in range(8)])
        wbuf[("w2", par)] = (P.sb(name + "_w2g%d" % par, [128, 4, 1024], BF16), [T("w2g") for _ in range(4)])
    NSTG = 2
    stg = [P.sb(name + "_stg%d" % i, [128, 1024], F32) for i in range(NSTG)]
    Tstg = [T(name + "_stg%d" % i) for i in range(NSTG)]
    stg_i = [0]
    if moe:
        rw = P.sb(name + "_rw", [128, 8, nexp], F32)
        Trw = T(name + "_rw")
        P.dma("sp", rw[:], router.rearrange("(c p) e -> p c e", p=128), wr=[Trw], allow_slow_non_contiguous=True)
        rwh = P.sb(name + "_rwh", [128, 8, nexp], BF16)
        rwl = P.sb(name + "_rwl", [128, 8, nexp], BF16)
        Trwh, Trwl = T(name + "_rwh"), T(name + "_rwl")
        P.op("pool", lambda e: e.tensor_copy(out=rwh[:], in_=rw[:]), rd=[Trw], wr=[Trwh])
        P.op("pool", lambda e: e.tensor_tensor(out=rwl[:], in0=rw[:], in1=rwh[:], op=ALU.subtract),
             rd=[Trw, Trwh], wr=[Trwl])
        hlo = P.sb(name + "_hlo", [128, 8, 128], BF16)
        Thlo = T(name + "_hlo")
        h32 = P.sb(name + "_h32", [128, 8, 128], F32)
        Th32 = T(name + "_h32")
        gates = P.sb(name + "_gates", [128, NB, nexp], F32)
        Tgates = [T(name + "_gates%d" % i) for i in range(NB)]
        gs = {k: P.sb(name + "_g" + k, [128, 8], F32) for k in ("lg", "m8", "ex", "mk", "ge")}
        Tgs = {k: T(name + "_g" + k) for k in gs}
        gs1 = {k: P.sb(name + "_g" + k, [128, 1], F32) for k in ("nm", "den", "rden")}
        Tgs1 = {k: T(name + "_g" + k) for k in gs1}
    if 'nofinal' in DBG:
        g_final = None
    if g_final is not None:
        gfin = P.sb(name + "_gfin", [128, D], F32)
        Tgfin = T(name + "_gfin")
        P.dma("sp", gfin[:], g_final.partition_broadcast(128), wr=[Tgfin])

    def load_w(kind, par, e, f0, gsz):
        buf, Ts = wbuf[(kind, par)]
        if kind in ("w1", "w3"):
            src = w1 if kind == "w1" else w3
            for c in range(8):
                i = stg_i[0] % NSTG
                stg_i[0] += 1
                ncol = gsz * 128
                P.dma("sp", stg[i][:, 0:ncol], src[e, c * 128:(c + 1) * 128, f0 * 128:f0 * 128 + ncol],
                      wr=[Tstg[i]])
                P.op("pool", lambda eng, i=i, c=c, ncol=ncol, buf=buf: eng.tensor_copy(
                    out=buf[:, c, 0:ncol], in_=stg[i][:, 0:ncol]), rd=[Tstg[i]], wr=[Ts[c]])
        else:
            for fl in range(gsz):
                i = stg_i[0] % NSTG
                stg_i[0] += 1
                f = f0 + fl
                P.dma("sp", stg[i][:, :], w2[e, f * 128:(f + 1) * 128, :], wr=[Tstg[i]])
                P.op("pool", lambda eng, i=i, fl=fl, buf=buf: eng.tensor_copy(
                    out=buf[:, fl, :], in_=stg[i][:, :]), rd=[Tstg[i]], wr=[Ts[fl]])

    bA = [(C.bank[0], C.Tbank[0]), (C.bank[1], C.Tbank[1])]
    bB = [(C.bank[2], C.Tbank[2]), (C.bank[3], C.Tbank[3])]
    bO = [(C.bank[4 + i], C.Tbank[4 + i]) for i in range(4)]
    bR = (C.bank[6], C.Tbank[6])

    for ps_ in range(npass):
        tok0 = ps_ * NB * 128
        work = [(e, gi) for e in range(nexp) for gi in range(len(groups))]
        gcount = 0
        e0, gi0 = work[0]
        if 'noexp' not in DBG:
            load_w("w1", 0, e0, *groups[gi0])
            load_w("w3", 0, e0, *groups[gi0])
            load_w("w2", 0, e0, *groups[gi0])
        for b in range(NB):
            P.dma("act", xacc[:, b, :], x_in_ap[tok0 + b * 128: tok0 + (b + 1) * 128, :],
                  rd=[Tx_in], wr=[Txacc[b]])
        for b in range(NB):
            P.op("act", lambda e, b=b: e.activation(out=junk[:], in_=xacc[:, b, :], func=AF.Square,
                                                    accum_out=ss[:, b:b + 1]),
                 rd=[Txacc[b]], wr=[Tjunk, Tss[b]])
            P.op("act", lambda e, b=b: e.activation(out=rstd[:, b:b + 1], in_=ss[:, b:b + 1], func=AF.Sqrt,
                                                    scale=1.0 / D, bias=EPS), rd=[Tss[b]], wr=[Trstd[b]])
            P.op("dve", lambda e, b=b: e.reciprocal(out=rstd[:, b:b + 1], in_=rstd[:, b:b + 1]),
                 rd=[Trstd[b]], wr=[Trstd[b]])
            P.op("dve", lambda e, b=b: e.tensor_scalar(out=xn[:], in0=xacc[:, b, :], scalar1=rstd[:, b:b + 1],
                                                       scalar2=None, op0=ALU.mult),
                 rd=[Txacc[b], Trstd[b]], wr=[Txn])
            for hh in range(2):
                bk, Tbk = (bA[b % 2] if hh == 0 else bB[b % 2])
                for cc in range(4):
                    c = hh * 4 + cc
                    P.op("pe", lambda e, c=c, cc=cc, bk=bk: e.transpose(
                        out=bk[:, cc * 128:(cc + 1) * 128], in_=xn[:, c * 128:(c + 1) * 128],
                        identity=C.ident[:]), rd=[Txn, C.Tident], wr=[Tbk])
                if not moe:
                    P.op("dve", lambda e, hh=hh, bk=bk, b=b: e.tensor_tensor(
                        out=hT[:, hh * 4:(hh + 1) * 4, b * 128:(b + 1) * 128],
                        in0=bk[:].rearrange("p (c t) -> p c t", c=4),
                        in1=gexp[:, hh * 4:(hh + 1) * 4, :], op=ALU.mult),
                         rd=[Tbk, Tgexp], wr=[ThT[b]])
                else:
                    P.op("dve", lambda e, hh=hh, bk=bk, b=b: e.tensor_tensor(
                        out=h32[:, hh * 4:(hh + 1) * 4, :],
                        in0=bk[:].rearrange("p (c t) -> p c t", c=4),
                        in1=gexp[:, hh * 4:(hh + 1) * 4, :], op=ALU.mult),
                         rd=[Tbk, Tgexp], wr=[Th32])
            if moe:
                P.op("pool", lambda e, b=b: e.tensor_copy(out=hT[:, :, b * 128:(b + 1) * 128], in_=h32[:]),
                     rd=[Th32], wr=[ThT[b]])
                P.op("pool", lambda e, b=b: e.tensor_tensor(out=hlo[:], in0=h32[:],
                                                            in1=hT[:, :, b * 128:(b + 1) * 128], op=ALU.subtract),
                     rd=[Th32, ThT[b]], wr=[Thlo])
                if 'nogate' in DBG:
                    P.op("pool", lambda e, b=b: e.memset(gates[:, b, :], 0.25), wr=[Tgates[b]])
                    continue
                rb, Trb = bR
                k3 = 0
                for (lh, Tl, rh, Tr) in ((hT[:, :, b * 128:(b + 1) * 128], ThT[b], rwh, Trwh),
                                         (hlo[:], Thlo, rwh, Trwh),
                                         (hT[:, :, b * 128:(b + 1) * 128], ThT[b], rwl, Trwl)):
                    for c in range(8):
                        P.op("pe", lambda e, c=c, lh=lh, rh=rh, k3=k3: e.matmul(
                            rb[:, 0:nexp], lhsT=lh[:, c, :], rhs=rh[:, c, :],
                            start=(k3 == 0), stop=(k3 == 23)), rd=[Tl, Tr], wr=[Trb])
                        k3 += 1
                P.op("act", lambda e: e.activation(out=gs["lg"][:], in_=rb[:, 0:nexp], func=AF.Copy),
                     rd=[Trb], wr=[Tgs["lg"]])
                P.op("dve", lambda e: e.max(out=gs["m8"][:], in_=gs["lg"][:]), rd=[Tgs["lg"]], wr=[Tgs["m8"]])
                P.op("dve", lambda e: e.tensor_scalar(out=gs1["nm"][:], in0=gs["m8"][:, 0:1], scalar1=-1.0,
                                                      scalar2=None, op0=ALU.mult),
                     rd=[Tgs["m8"]], wr=[Tgs1["nm"]])
                P.op("act", lambda e: e.activation(out=gs["ex"][:], in_=gs["lg"][:], func=AF.Exp,
                                                   bias=gs1["nm"][:, 0:1]),
                     rd=[Tgs["lg"], Tgs1["nm"]], wr=[Tgs["ex"]])
                P.op("dve", lambda e: e.tensor_scalar(out=gs["mk"][:], in0=gs["lg"][:], scalar1=gs["m8"][:, 1:2],
                                                      scalar2=None, op0=ALU.is_ge),
                     rd=[Tgs["lg"], Tgs["m8"]], wr=[Tgs["mk"]])
                P.op("dve", lambda e: e.tensor_tensor(out=gs["ge"][:], in0=gs["ex"][:], in1=gs["mk"][:],
                                                      op=ALU.mult),
                     rd=[Tgs["ex"], Tgs["mk"]], wr=[Tgs["ge"]])
                P.op("dve", lambda e: e.reduce_sum(out=gs1["den"][:], in_=gs["ge"][:], axis=AX.X),
                     rd=[Tgs["ge"]], wr=[Tgs1["den"]])
                P.op("dve", lambda e: e.reciprocal(out=gs1["rden"][:], in_=gs1["den"][:]),
                     rd=[Tgs1["den"]], wr=[Tgs1["rden"]])
                P.op("dve", lambda e, b=b: e.tensor_scalar(out=gates[:, b, :], in0=gs["ge"][:],
                                                           scalar1=gs1["rden"][:, 0:1], scalar2=None,
                                                           op0=ALU.mult),
                     rd=[Tgs["ge"], Tgs1["rden"]], wr=[Tgates[b]])
        for wi, (e_, gi) in enumerate(work if 'noexp' not in DBG else []):
            par = wi % 2
            f0, gsz = groups[gi]
            if wi + 1 < len(work):
                en, gn = work[wi + 1]
                load_w("w1", 1 - par, en, *groups[gn])
                load_w("w3", 1 - par, en, *groups[gn])
                load_w("w2", 1 - par, en, *groups[gn])
            w1g, Tw1 = wbuf[("w1", par)]
            w3g, Tw3 = wbuf[("w3", par)]
            w2g, Tw2 = wbuf[("w2", par)]
            k = 0
            for fl in range(gsz):
                for t in range(4):
                    a, Ta = bA[k % 2]
                    bb, Tb = bB[k % 2]
                    s_, Ts_ = sa[k % 2], Tsa[k % 2]
                    k += 1
                    for c in range(8):
                        P.op("pe", lambda e, c=c, a=a, fl=fl, t=t, w1g=w1g: e.matmul(
                            a[:], lhsT=w1g[:, c, fl * 128:(fl + 1) * 128], rhs=hT[:, c, t * 512:(t + 1) * 512],
                            start=(c == 0), stop=(c == 7)), rd=[Tw1[c]] + ThT[t * 4:(t + 1) * 4], wr=[Ta])
                    for c in range(8):
                        P.op("pe", lambda e, c=c, bb=bb, fl=fl, t=t, w3g=w3g: e.matmul(
                            bb[:], lhsT=w3g[:, c, fl * 128:(fl + 1) * 128], rhs=hT[:, c, t * 512:(t + 1) * 512],
                            start=(c == 0), stop=(c == 7)), rd=[Tw3[c]] + ThT[t * 4:(t + 1) * 4], wr=[Tb])
                    P.op("act", lambda e, a=a, s_=s_: e.activation(out=s_[:], in_=a[:], func=AF.Sigmoid),
                         rd=[Ta], wr=[Ts_])
                    P.op("dve", lambda e, a=a, s_=s_: e.tensor_tensor(out=s_[:], in0=s_[:], in1=a[:], op=ALU.mult),
                         rd=[Ts_, Ta], wr=[Ts_])
                    P.op("dve", lambda e, s_=s_, bb=bb, fl=fl, t=t: e.tensor_tensor(
                        out=hid[:, fl, t * 512:(t + 1) * 512], in0=s_[:], in1=bb[:], op=ALU.mult),
                         rd=[Ts_, Tb], wr=[Thid[fl][t]])
            k = 0
            for b in range(NB):
                for half in range(2):
                    o, To = bO[k % 4]
                    k += 1
                    for fl in range(gsz):
                        P.op("pe", lambda e, o=o, fl=fl, b=b, half=half, w2g=w2g, gsz=gsz: e.matmul(
                            o[:], lhsT=hid[:, fl, b * 128:(b + 1) * 128],
                            rhs=w2g[:, fl, half * 512:(half + 1) * 512],
                            start=(fl == 0), stop=(fl == gsz - 1)),
                             rd=[Thid[fl][b // 4], Tw2[fl]], wr=[To])
                    if moe:
                        sc = gates[:, b, e_:e_ + 1]
                        rds = [To, Txacc[b], Tgates[b]]
                    else:
                        sc = 1.0
                        rds = [To, Txacc[b]]
                    P.op("dve", lambda e, o=o, b=b, half=half, sc=sc: e.scalar_tensor_tensor(
                        out=xacc[:, b, half * 512:(half + 1) * 512], in0=o[:], scalar=sc,
                        in1=xacc[:, b, half * 512:(half + 1) * 512], op0=ALU.mult, op1=ALU.add),
                         rd=rds, wr=[Txacc[b]])
        for b in range(NB):
            if g_final is not None:
                P.op("act", lambda e, b=b: e.activation(out=junk[:], in_=xacc[:, b, :], func=AF.Square,
                                                        accum_out=ss[:, b:b + 1]),
                     rd=[Txacc[b]], wr=[Tjunk, Tss[b]])
                P.op("act", lambda e, b=b: e.activation(out=rstd[:, b:b + 1], in_=ss[:, b:b + 1], func=AF.Sqrt,
                                                        scale=1.0 / D, bias=EPS), rd=[Tss[b]], wr=[Trstd[b]])
                P.op("dve", lambda e, b=b: e.reciprocal(out=rstd[:, b:b + 1], in_=rstd[:, b:b + 1]),
                     rd=[Trstd[b]], wr=[Trstd[b]])
                P.op("dve", lambda e, b=b: e.scalar_tensor_tensor(
                    out=xacc[:, b, :], in0=xacc[:, b, :], scalar=rstd[:, b:b + 1], in1=gfin[:],
                    op0=ALU.mult, op1=ALU.mult), rd=[Txacc[b], Trstd[b], Tgfin], wr=[Txacc[b]])
            P.dma("act", x_out_ap[tok0 + b * 128: tok0 + (b + 1) * 128, :], xacc[:, b, :],
                  rd=[Txacc[b]], wr=[Tx_out], store=True)


def load_weight_bf16(P, name, src, K, N, q="sp", stg=None):
    kc = max(1, K // 128)
    kp = min(K, 128)
    wb = P.sb(name, [kp, kc, N], BF16)
    Tw = T(name)
    if stg is None:
        stg = [P.sb(name + "_s%d" % i, [128, 1024], F32) for i in range(2)]
        Ts = [T(name + "_s%d" % i) for i in range(2)]
    else:
        stg, Ts = stg
    i = 0
    for c in range(kc):
        for n0 in range(0, N, 1024):
            n1 = min(N, n0 + 1024)
            s, Tst = stg[i % 2], Ts[i % 2]
            i += 1
            P.dma(q, s[0:kp, 0:n1 - n0], src[c * 128:c * 128 + kp, n0:n1], wr=[Tst])
            P.op("pool", lambda e, s=s, c=c, n0=n0, n1=n1: e.tensor_copy(out=wb[:, c, n0:n1], in_=s[0:kp, 0:n1 - n0]),
                 rd=[Tst], wr=[Tw])
    return wb, Tw


class Banks:
    def __init__(self, C):
        self.C = C
        self.i = 0

    def next(self):
        b = self.C.bank[self.i % 8], self.C.Tbank[self.i % 8]
        self.i += 1
        return b


def norm_T(P, C, BK, name, x_ap, Tx, gexp, Tgexp, scr):
    ss, Tss, rstd, Trstd, junk, Tjunk, xn, Txn, hT, ThT = scr
    P.op("act", lambda e: e.activation(out=junk[:], in_=x_ap, func=AF.Square, accum_out=ss[:, 0:1]),
         rd=[Tx], wr=[Tjunk, Tss])
    P.op("act", lambda e: e.activation(out=rstd[:, 0:1], in_=ss[:, 0:1], func=AF.Sqrt, scale=1.0 / D, bias=EPS),
         rd=[Tss], wr=[Trstd])
    P.op("dve", lambda e: e.reciprocal(out=rstd[:, 0:1], in_=rstd[:, 0:1]), rd=[Trstd], wr=[Trstd])
    P.op("dve", lambda e: e.tensor_scalar(out=xn[:], in0=x_ap, scalar1=rstd[:, 0:1], scalar2=None, op0=ALU.mult),
         rd=[Tx, Trstd], wr=[Txn])
    for hh in range(2):
        bk, Tbk = BK.next()
        for cc in range(4):
            c = hh * 4 + cc
            P.op("pe", lambda e, c=c, cc=cc, bk=bk: e.transpose(out=bk[:, cc * 128:(cc + 1) * 128],
                                                                in_=xn[:, c * 128:(c + 1) * 128],
                                                                identity=C.ident[:]),
                 rd=[Txn, C.Tident], wr=[Tbk])
        P.op("dve", lambda e, hh=hh, bk=bk: e.tensor_tensor(out=hT[:, hh * 4:(hh + 1) * 4, :],
                                                            in0=bk[:].rearrange("p (c t) -> p c t", c=4),
                                                            in1=gexp[:, hh * 4:(hh + 1) * 4, :], op=ALU.mult),
             rd=[Tbk, Tgexp], wr=[ThT])


def mk_norm_scr(P, name):
    ss = P.sb(name + "_ss", [128, 1], F32)
    rstd = P.sb(name + "_rstd", [128, 1], F32)
    junk = P.sb(name + "_junk", [128, D], BF16)
    xn = P.sb(name + "_xn", [128, D], F32)
    hT = P.sb(name + "_hT", [128, 8, 128], BF16)
    return (ss, T(name + "ss"), rstd, T(name + "rstd"), junk, T(name + "junk"), xn, T(name + "xn"), hT, T(name + "hT"))


def transpose_to(P, C, BK, src, Tsrc, ncol, dst, Tdst, npart=128):
    nch = ncol // 128
    for c0 in range(0, nch, 4):
        bk, Tbk = BK.next()
        n = min(4, nch - c0)
        for cc in range(n):
            c = c0 + cc
            P.op("pe", lambda e, c=c, cc=cc, bk=bk: e.transpose(out=bk[:, cc * npart:(cc + 1) * npart],
                                                                in_=src[0:npart, c * 128:(c + 1) * 128],
                                                                identity=C.ident[0:npart, 0:npart]),
                 rd=[Tsrc, C.Tident], wr=[Tbk])
        P.op("act", lambda e, c0=c0, n=n, bk=bk: e.activation(
            out=dst[:, c0:c0 + n, :], in_=bk[:, 0:n * npart].rearrange("p (c t) -> p c t", c=n), func=AF.Copy),
             rd=[Tbk], wr=[Tdst])


def gelu_tanh(P, name, src, Tsrc, dst, Tdst, tmp, Ttmp):
    P.op("act", lambda e: e.activation(out=tmp[:], in_=src, func=AF.Square), rd=[Tsrc], wr=[Ttmp])
    P.op("dve", lambda e: e.tensor_scalar(out=tmp[:], in0=tmp[:], scalar1=0.044715, scalar2=1.0, op0=ALU.mult,
                                          op1=ALU.add), rd=[Ttmp], wr=[Ttmp])
    P.op("dve", lambda e: e.tensor_tensor(out=tmp[:], in0=tmp[:], in1=src, op=ALU.mult), rd=[Ttmp, Tsrc], wr=[Ttmp])
    P.op("act", lambda e: e.activation(out=tmp[:], in_=tmp[:], func=AF.Sigmoid, scale=1.5957691216057308),
         rd=[Ttmp], wr=[Ttmp])
    P.op("dve", lambda e: e.tensor_tensor(out=dst, in0=tmp[:], in1=src, op=ALU.mult), rd=[Ttmp, Tsrc], wr=[Tdst])


def run_pipeline(gens):
    gens = list(gens)
    idx = 1
    old, new, new_mid = None, gens[0], False
    while old is not None or new is not None:
        if old is not None:
            try:
                next(old)
            except StopIteration:
                old = None
        if new is not None and not new_mid:
            if next(new) == "MID":
                new_mid = True
        if old is None and (new_mid or new is None):
            old = new
            new = gens[idx] if idx < len(gens) else None
            idx += 1
            new_mid = False


class HalfBanks:
    def __init__(self, C, base):
        self.C, self.base, self.i = C, base, 0

    def next(self):
        k = self.base + (self.i % 4)
        self.i += 1
        return self.C.bank[k], self.C.Tbank[k]


def l0_mixer_stage(P, C, x_in, x_out, ntok, W):
    x_in_ap, Tx_in = x_in
    x_out_ap, Tx_out = x_out
    BK0 = Banks(C)
    nblk = ntok // 128
    stg = ([P.sb("l0m_stg%d" % i, [128, 1024], F32) for i in range(2)], [T("l0m_stg%d" % i) for i in range(2)])
    w_in, Tw_in = load_weight_bf16(P, "l0m_w_in", W["w_in"], D, 2576, stg=stg)
    w_o, Tw_o = load_weight_bf16(P, "l0m_w_o", W["w_o"], D, D, stg=stg)
    gup, Tgup = load_weight_bf16(P, "l0m_gup", W["gate_up"], 16, 256, stg=stg)
    gexp, Tgexp = load_gexp(P, C, "l0m_gn", W["norm_mix"])

    def sbt(name, shape, dt=F32):
        return P.sb("l0m_" + name, shape, dt), T("l0m_" + name)

    mask, Tmask = sbt("mask", [128, 128])
    P.op("pool", lambda e: e.memset(mask[:], 1.0), wr=[Tmask])
    P.op("pool", lambda e: e.affine_select(out=mask[:], in_=mask[:], pattern=[[1, 128]], compare_op=ALU.is_ge,
                                           fill=0.0, base=0, channel_multiplier=-1), rd=[Tmask], wr=[Tmask])
    wsT, TwsT = sbt("wsT", [128, 4, 128], BF16)
    ws32, Tws32 = sbt("ws32", [128, 4 * 128])
    P.dma("sp", ws32[:].rearrange("t (g s) -> t g s", g=4), W["w_s"].rearrange("g t s -> t g s"), wr=[Tws32])
    wsT32, TwsT32 = sbt("wsT32", [128, 4, 128])
    transpose_to(P, C, BK0, ws32, Tws32, 512, wsT32, TwsT32)
    P.op("dve", lambda e: e.tensor_tensor(out=wsT[:], in0=wsT32[:],
                                          in1=mask[:].unsqueeze(1).to_broadcast([128, 4, 128]), op=ALU.mult),
         rd=[TwsT32, Tmask], wr=[TwsT])
    bs, Tbs = sbt("bs", [128, 4])
    P.dma("sp", bs[:], W["b_s"].rearrange("g t -> t g"), wr=[Tbs], allow_slow_non_contiguous=True)
    lng, Tlng = sbt("lng", [128, 512])
    lnb, Tlnb = sbt("lnb", [128, 512])
    hdg, Thdg = sbt("hdg", [128, 512])
    P.dma("sp", lng[:], W["ln_g"].partition_broadcast(128), wr=[Tlng])
    P.dma("sp", lnb[:], W["ln_b"].partition_broadcast(128), wr=[Tlnb])
    P.dma("sp", hdg[:], W["head_g"].rearrange("h v -> (h v)").partition_broadcast(128), wr=[Thdg])
    ngb, Tngb = sbt("ngb", [64, 4])
    P.dma("sp", ngb[:], W["gate_bias"].rearrange("(h k) -> k h", k=64), wr=[Tngb], allow_slow_non_contiguous=True)
    P.op("dve", lambda e: e.tensor_scalar(out=ngb[:], in0=ngb[:], scalar1=-1.0, scalar2=None, op0=ALU.mult),
         rd=[Tngb], wr=[Tngb])
    S, TS = sbt("S", [64, 4, 128])
    Sb, TSb = sbt("Sb", [64, 4, 128], BF16)
    P.op("pool", lambda e: e.memset(S[:], 0.0), wr=[TS])
    P.op("pool", lambda e: e.memset(Sb[:], 0.0), wr=[TSb])
    junk, Tjunk = sbt("junk", [128, D], BF16)

    def mkset(i):
        def st(name, shape, dt=F32):
            return sbt("%s_%d" % (name, i), shape, dt)
        d = {}
        d["xb"] = st("xb", [128, D])
        d["ss"] = st("ss", [128, 1])
        d["rstd"] = st("rstd", [128, 1])
        d["xn"] = st("xn", [128, D])
        d["hT"] = st("hT", [128, 8, 128], BF16)
        for nm in ("ug", "vg", "tmp", "on", "sg"):
            d[nm] = st(nm, [128, 512])
        d["vnb"] = st("vnb", [128, 512], BF16)
        d["vbb"] = st("vbb", [128, 512], BF16)
        d["st6"] = st("st6", [128, 6])
        d["mv"] = st("mv", [128, 2])
        d["lrs"] = st("lrs", [128, 1])
        d["mix"] = st("mix", [128, D])
        d["mixT"] = st("mixT", [128, 8, 128], BF16)
        d["glr"] = st("glr", [16, 128], BF16)
        d["el"] = st("el", [64, 512])
        d["cs"] = st("cs", [64, 4, 128])
        d["ncl"] = st("ncl", [64, 4])
        d["egl"] = st("egl", [64, 4])
        d["e1"] = st("e1", [64, 512])
        d["qt"] = st("qt", [64, 4, 128], BF16)
        d["kt"] = st("kt", [64, 4, 128], BF16)
        d["kd32"] = st("kd32", [64, 512])
        d["kdec"] = st("kdec", [128, 4, 64], BF16)
        d["attT"] = st("attT", [128, 4, 128], BF16)
        d["ssq"] = st("ssq", [128, 4])
        d["xo"] = st("xo", [128, D])
        d["BK"] = HalfBanks(C, 4 * i)
        return d

    sets = [mkset(0), mkset(1)]

    def block(b):
        d = sets[b % 2]
        BK = d["BK"]
        (x, Txb), (ug, Tug), (vg, Tvg), (tmp, Ttmp), (on, Ton), (sg, Tsg) = [d[k] for k in ("xb", "ug", "vg", "tmp", "on", "sg")]
        (vnb, Tvnb), (vbb, Tvbb), (st6, Tst6), (mv, Tmv), (lrs, Tlrs) = [d[k] for k in ("vnb", "vbb", "st6", "mv", "lrs")]
        (mix, Tmix), (mixT, TmixT), (glr, Tglr), (el, Tel), (cs, Tcs) = [d[k] for k in ("mix", "mixT", "glr", "el", "cs")]
        (ncl, Tncl), (egl, Tegl), (e1, Te1), (qt, Tqt), (kt, Tkt) = [d[k] for k in ("ncl", "egl", "e1", "qt", "kt")]
        (kd32, Tkd32), (kdec, Tkdec), (attT, TattT), (ssq, Tssq), (xo_, Txo) = [d[k] for k in ("kd32", "kdec", "attT", "ssq", "xo")]
        hT, ThT = d["hT"]
        scr = (d["ss"][0], d["ss"][1], d["rstd"][0], d["rstd"][1], junk, Tjunk, d["xn"][0], d["xn"][1], hT, ThT)

        def proj_tok(col0, ncols=512):
            bk, Tbk = BK.next()
            for c in range(8):
                P.op("pe", lambda e, c=c, bk=bk: e.matmul(bk[:, 0:ncols], lhsT=hT[:, c, :],
                                                          rhs=w_in[:, c, col0:col0 + ncols],
                                                          start=(c == 0), stop=(c == 7)), rd=[ThT, Tw_in], wr=[Tbk])
            return bk, Tbk

        def proj_feat(col0, m, nh):
            bk, Tbk = BK.next()
            for h in range(nh):
                for c in range(8):
                    P.op("pe", lambda e, c=c, h=h, bk=bk: e.matmul(
                        bk[0:m, h * 128:(h + 1) * 128], lhsT=w_in[:, c, col0 + h * m: col0 + (h + 1) * m],
                        rhs=hT[:, c, :], start=(c == 0), stop=(c == 7)), rd=[ThT, Tw_in], wr=[Tbk])
            return bk, Tbk

        P.dma("act", x[:], x_in_ap[b * 128:(b + 1) * 128, :], rd=[Tx_in], wr=[Txb])
        norm_T(P, C, BK, "l0m", x[:], Txb, gexp, Tgexp, scr)
        yield
        pu, Tpu = proj_tok(0)
        yield
        gelu_tanh(P, "u", pu[:], Tpu, ug[:], Tug, tmp, Ttmp)
        yield
        pv, Tpv = proj_tok(512)
        yield
        gelu_tanh(P, "v", pv[:], Tpv, vg[:], Tvg, tmp, Ttmp)
        yield
        P.op("dve", lambda e: e.bn_stats(out=st6[:], in_=vg[:]), rd=[Tvg], wr=[Tst6])
        P.op("dve", lambda e: e.bn_aggr(out=mv[:], in_=st6[:]), rd=[Tst6], wr=[Tmv])
        P.op("act", lambda e: e.activation(out=lrs[:], in_=mv[:, 1:2], func=AF.Sqrt, bias=EPS), rd=[Tmv], wr=[Tlrs])
        P.op("dve", lambda e: e.reciprocal(out=lrs[:], in_=lrs[:]), rd=[Tlrs], wr=[Tlrs])
        yield
        P.op("dve", lambda e: e.tensor_scalar(out=vg[:], in0=vg[:], scalar1=mv[:, 0:1], scalar2=lrs[:, 0:1],
                                              op0=ALU.subtract, op1=ALU.mult), rd=[Tvg, Tmv, Tlrs], wr=[Tvg])
        P.op("dve", lambda e: e.tensor_tensor(out=vg[:], in0=vg[:], in1=lng[:], op=ALU.mult), rd=[Tvg, Tlng], wr=[Tvg])
        P.op("dve", lambda e: e.tensor_tensor(out=vnb[:], in0=vg[:], in1=lnb[:], op=ALU.add), rd=[Tvg, Tlnb], wr=[Tvnb])
        yield
        pvb, Tpvb = proj_tok(1552)
        P.op("act", lambda e, pvb=pvb: e.activation(out=vbb[:], in_=pvb[:], func=AF.Copy), rd=[Tpvb], wr=[Tvbb])
        yield
        pm, Tpm = BK.next()
        for g in range(4):
            P.op("pe", lambda e, g=g, pm=pm: e.matmul(pm[:, g * 128:(g + 1) * 128], lhsT=wsT[:, g, :],
                                                      rhs=vnb[:, g * 128:(g + 1) * 128], start=True, stop=True),
                 rd=[TwsT, Tvnb], wr=[Tpm])
        for g in range(4):
            P.op("dve", lambda e, g=g, pm=pm: e.scalar_tensor_tensor(
                out=mix[:, g * 128:(g + 1) * 128], in0=pm[:, g * 128:(g + 1) * 128], scalar=bs[:, g:g + 1],
                in1=ug[:, g * 128:(g + 1) * 128], op0=ALU.add, op1=ALU.mult), rd=[Tpm, Tbs, Tug], wr=[Tmix])
        yield
        pg, Tpg = proj_feat(1536, 16, 1)
        P.op("act", lambda e, pg=pg: e.activation(out=glr[:], in_=pg[0:16, 0:128], func=AF.Copy), rd=[Tpg], wr=[Tglr])
        yield
        pgt, Tpgt = BK.next()
        for h in range(4):
            P.op("pe", lambda e, h=h, pgt=pgt: e.matmul(pgt[0:64, h * 128:(h + 1) * 128],
                                                        lhsT=gup[:, 0, h * 64:(h + 1) * 64], rhs=glr[:],
                                                        start=True, stop=True), rd=[Tgup, Tglr], wr=[Tpgt])
        for h in range(4):
            P.op("act", lambda e, h=h, pgt=pgt: e.activation(out=el[:, h * 128:(h + 1) * 128],
                                                             in_=pgt[0:64, h * 128:(h + 1) * 128], func=AF.Exp,
                                                             scale=-1.0, bias=ngb[:, h:h + 1]),
                 rd=[Tpgt, Tngb], wr=[Tel])
        P.op("act", lambda e: e.activation(out=el[:], in_=el[:], func=AF.Ln, bias=1.0), rd=[Tel], wr=[Tel])
        yield
        for h in range(4):
            P.op("dve", lambda e, h=h: e.tensor_tensor_scan(out=cs[:, h, :], data0=C.ones[0:64, :],
                                                            data1=el[:, h * 128:(h + 1) * 128], initial=0.0,
                                                            op0=ALU.mult, op1=ALU.add),
                 rd=[Tel, C.Tones], wr=[Tcs])
        P.op("dve", lambda e: e.tensor_scalar(out=ncl[:], in0=cs[:, :, 127], scalar1=-1.0 / 16, scalar2=None,
                                              op0=ALU.mult), rd=[Tcs], wr=[Tncl])
        P.op("act", lambda e: e.activation(out=egl[:], in_=ncl[:], func=AF.Exp), rd=[Tncl], wr=[Tegl])
        yield
        pq, Tpq = proj_feat(1024, 64, 4)
        yield
        csf = cs[:].rearrange("p h t -> p (h t)")
        P.op("act", lambda e: e.activation(out=e1[:], in_=csf, func=AF.Exp, scale=-1.0 / 16), rd=[Tcs], wr=[Te1])
        P.op("dve", lambda e, pq=pq: e.scalar_tensor_tensor(out=qt[:].rearrange("p h t -> p (h t)"), in0=pq[0:64, :],
                                                            scalar=0.125, in1=e1[:], op0=ALU.mult, op1=ALU.mult),
             rd=[Tpq, Te1], wr=[Tqt])
        yield
        pk, Tpk = proj_feat(1280, 64, 4)
        yield
        P.op("act", lambda e: e.activation(out=e1[:], in_=csf, func=AF.Exp, scale=1.0 / 16), rd=[Tcs], wr=[Te1])
        P.op("dve", lambda e, pk=pk: e.tensor_tensor(out=kt[:].rearrange("p h t -> p (h t)"), in0=pk[0:64, :],
                                                     in1=e1[:], op=ALU.mult), rd=[Tpk, Te1], wr=[Tkt])
        for h in range(4):
            P.op("act", lambda e, h=h: e.activation(out=e1[:, h * 128:(h + 1) * 128], in_=cs[:, h, :], func=AF.Exp,
                                                    scale=1.0 / 16, bias=ncl[:, h:h + 1]), rd=[Tcs, Tncl], wr=[Te1])
        P.op("dve", lambda e, pk=pk: e.tensor_tensor(out=kd32[:], in0=pk[0:64, :], in1=e1[:], op=ALU.mult),
             rd=[Tpk, Te1], wr=[Tkd32])
        yield
        transpose_to(P, C, BK, kd32, Tkd32, 512, kdec, Tkdec, npart=64)
        yield "MID"
        pa, Tpa = BK.next()
        for h in range(4):
            P.op("pe", lambda e, h=h, pa=pa: e.matmul(pa[:, h * 128:(h + 1) * 128], lhsT=kt[:, h, :], rhs=qt[:, h, :],
                                                      start=True, stop=True), rd=[Tkt, Tqt], wr=[Tpa])
        P.op("dve", lambda e, pa=pa: e.tensor_tensor(out=attT[:], in0=pa[:].rearrange("p (h t) -> p h t", h=4),
                                                     in1=mask[:].unsqueeze(1).to_broadcast([128, 4, 128]),
                                                     op=ALU.mult), rd=[Tpa, Tmask], wr=[TattT])
        yield
        pS, TpS = BK.next()
        for h in range(4):
            P.op("pe", lambda e, h=h, pS=pS: e.matmul(pS[0:64, h * 128:(h + 1) * 128], lhsT=kdec[:, h, :],
                                                      rhs=vbb[:, h * 128:(h + 1) * 128], start=True, stop=True),
                 rd=[Tkdec, Tvbb], wr=[TpS])
        yield
        po, Tpo = BK.next()
        for h in range(4):
            P.op("pe", lambda e, h=h, po=po: e.matmul(po[:, h * 128:(h + 1) * 128], lhsT=attT[:, h, :],
                                                      rhs=vbb[:, h * 128:(h + 1) * 128], start=True, stop=False),
                 rd=[TattT, Tvbb], wr=[Tpo])
            P.op("pe", lambda e, h=h, po=po: e.matmul(po[:, h * 128:(h + 1) * 128], lhsT=qt[:, h, :],
                                                      rhs=Sb[:, h, :], start=False, stop=True),
                 rd=[Tqt, TSb], wr=[Tpo])
        yield
        for h in range(4):
            P.op("dve", lambda e, h=h, pS=pS: e.scalar_tensor_tensor(
                out=S[:, h, :], in0=S[:, h, :], scalar=egl[:, h:h + 1], in1=pS[0:64, h * 128:(h + 1) * 128],
                op0=ALU.mult, op1=ALU.add), rd=[TS, Tegl, TpS], wr=[TS])
        P.op("pool", lambda e: e.tensor_copy(out=Sb[:], in_=S[:]), rd=[TS], wr=[TSb])
        yield
        for h in range(4):
            P.op("act", lambda e, h=h, po=po: e.activation(out=sg[:, h * 128:(h + 1) * 128],
                                                           in_=po[:, h * 128:(h + 1) * 128], func=AF.Square,
                                                           accum_out=ssq[:, h:h + 1]), rd=[Tpo], wr=[Tsg, Tssq])
        P.op("act", lambda e: e.activation(out=ssq[:], in_=ssq[:], func=AF.Sqrt, scale=1.0 / 128, bias=EPS),
             rd=[Tssq], wr=[Tssq])
        P.op("dve", lambda e: e.reciprocal(out=ssq[:], in_=ssq[:]), rd=[Tssq], wr=[Tssq])
        yield
        P.op("dve", lambda e, po=po: e.tensor_tensor(out=on[:].rearrange("p (h v) -> p h v", h=4),
                                                     in0=po[:].rearrange("p (h v) -> p h v", h=4),
                                                     in1=ssq[:].unsqueeze(2).to_broadcast([128, 4, 128]),
                                                     op=ALU.mult), rd=[Tpo, Tssq], wr=[Ton])
        P.op("dve", lambda e: e.tensor_tensor(out=on[:], in0=on[:], in1=hdg[:], op=ALU.mult), rd=[Ton, Thdg], wr=[Ton])
        yield
        pog, Tpog = proj_tok(2064)
        yield
        P.op("act", lambda e, pog=pog: e.activation(out=sg[:], in_=pog[:], func=AF.Sigmoid), rd=[Tpog], wr=[Tsg])
        P.op("dve", lambda e, pog=pog: e.tensor_tensor(out=sg[:], in0=sg[:], in1=pog[:], op=ALU.mult),
             rd=[Tsg, Tpog], wr=[Tsg])
        P.op("dve", lambda e: e.tensor_tensor(out=mix[:, 512:1024], in0=on[:], in1=sg[:], op=ALU.mult),
             rd=[Ton, Tsg], wr=[Tmix])
        yield
        transpose_to(P, C, BK, mix, Tmix, 1024, mixT, TmixT)
        yield
        for half in range(2):
            pw, Tpw = BK.next()
            for c in range(8):
                P.op("pe", lambda e, c=c, pw=pw, half=half: e.matmul(pw[:], lhsT=mixT[:, c, :],
                                                                   rhs=w_o[:, c, half * 512:(half + 1) * 512],
                                                                   start=(c == 0), stop=(c == 7)),
                     rd=[TmixT, Tw_o], wr=[Tpw])
            P.op("dve", lambda e, pw=pw, half=half: e.tensor_tensor(
                out=xo_[:, half * 512:(half + 1) * 512], in0=pw[:], in1=x[:, half * 512:(half + 1) * 512], op=ALU.add),
                 rd=[Tpw, Txb], wr=[Txo])
            yield
        P.dma("act", x_out_ap[b * 128:(b + 1) * 128, :], xo_[:], rd=[Txo], wr=[Tx_out], store=True)

    run_pipeline(block(b) for b in range(nblk))


def head_norm(P, name, src, Tsrc, g_rep, Tg_rep, dst, Tdst, tmp, Ttmp, ss16, Tss16, scale):
    P.op("dve", lambda e: e.tensor_tensor(out=tmp[:], in0=src[:], in1=src[:], op=ALU.mult), rd=[Tsrc], wr=[Ttmp])
    P.op("dve", lambda e: e.tensor_reduce(out=ss16[:], in_=tmp[:].rearrange("p (h d) -> p h d", h=16), axis=AX.X,
                                          op=ALU.add), rd=[Ttmp], wr=[Tss16])
    P.op("act", lambda e: e.activation(out=ss16[:], in_=ss16[:], func=AF.Sqrt, scale=1.0 / 64, bias=EPS),
         rd=[Tss16], wr=[Tss16])
    P.op("dve", lambda e: e.reciprocal(out=ss16[:], in_=ss16[:]), rd=[Tss16], wr=[Tss16])
    P.op("dve", lambda e: e.tensor_tensor(out=tmp[:].rearrange("p (h d) -> p h d", h=16),
                                          in0=src[:].rearrange("p (h d) -> p h d", h=16),
                                          in1=ss16[:].unsqueeze(2).to_broadcast([128, 16, 64]), op=ALU.mult),
         rd=[Tsrc, Tss16], wr=[Ttmp])
    P.op("dve", lambda e: e.scalar_tensor_tensor(out=dst[:].rearrange("p (h d) -> p h d", h=16),
                                                 in0=tmp[:].rearrange("p (h d) -> p h d", h=16), scalar=scale,
                                                 in1=g_rep[:].unsqueeze(1).to_broadcast([128, 16, 64]),
                                                 op0=ALU.mult, op1=ALU.mult), rd=[Ttmp, Tg_rep], wr=[Tdst])


def split3(P, name, src, Tsrc, outs, Touts, r32, Tr32):
    P.op("dve", lambda e: e.tensor_copy(out=outs[0][:], in_=src), rd=[Tsrc], wr=[Touts[0]])
    P.op("dve", lambda e: e.tensor_tensor(out=r32[:], in0=src, in1=outs[0][:], op=ALU.subtract),
         rd=[Tsrc, Touts[0]], wr=[Tr32])
    P.op("dve", lambda e: e.tensor_copy(out=outs[1][:], in_=r32[:]), rd=[Tr32], wr=[Touts[1]])
    P.op("dve", lambda e: e.tensor_tensor(out=outs[2][:], in0=r32[:], in1=outs[1][:], op=ALU.subtract),
         rd=[Tr32, Touts[1]], wr=[Touts[2]])


def l1_proj_stage(P, C, x_in, W, sel_ap, scratch):
    x_in_ap, Tx_in = x_in
    KTd, VAd, NCd, CRd, QTd, SGd, XOd = [scratch[k] for k in ("KT", "VA", "NC", "CR", "QT", "SG", "XO")]
    BK = Banks(C)
    w_in, Tw_in = load_weight_bf16(P, "l1p_w_in", W["w_in"], D, 4112)
    gexp, Tgexp = load_gexp(P, C, "l1p_gn", W["norm_mix"])

    def sbt(name, shape, dt=F32):
        return P.sb("l1p_" + name, shape, dt), T("l1p_" + name)

    maskb, Tmaskb = sbt("maskb", [128, 128], BF16)
    P.op("pool", lambda e: e.memset(maskb[:], 1.0), wr=[Tmaskb])
    P.op("pool", lambda e: e.affine_select(out=maskb[:], in_=maskb[:], pattern=[[1, 128]], compare_op=ALU.is_ge,
                                           fill=0.0, base=0, channel_multiplier=-1), rd=[Tmaskb], wr=[Tmaskb])
    onesb, Tonesb = sbt("onesb", [128, 128], BF16)
    P.op("pool", lambda e: e.memset(onesb[:], 1.0), wr=[Tonesb])
    e127, Te127 = sbt("e127", [128, 128], BF16)
    P.op("pool", lambda e: e.memset(e127[:], 1.0), wr=[Te127])
    P.op("pool", lambda e: e.affine_select(out=e127[:], in_=e127[:], pattern=[[0, 128]], compare_op=ALU.is_equal,
                                           fill=0.0, base=-64, channel_multiplier=1), rd=[Te127], wr=[Te127])
    sel, Tsel = sbt("sel", [128, 2])
    P.dma("sp", sel[:], sel_ap, wr=[Tsel])
    kg, Tkg = sbt("kg", [128, 64])
    qg, Tqg = sbt("qg", [128, 64])
    fbr, Tfbr = sbt("fbr", [128, 16])
    P.dma("sp", kg[:], W["k_g"].partition_broadcast(128), wr=[Tkg])
    P.dma("sp", qg[:], W["q_g"].partition_broadcast(128), wr=[Tqg])
    P.dma("sp", fbr[:], W["forget_bias"].partition_broadcast(128), wr=[Tfbr])
    A, TA = sbt("A", [128, 16])
    P.op("pool", lambda e: e.memset(A[:], 0.0), wr=[TA])
    xb = [sbt("xb%d" % i, [128, D]) for i in range(4)]
    cum = [sbt("cum%d" % i, [128, 16]) for i in range(4)]
    xown, Txown = sbt("xown", [128, D])
    cown, Tcown = sbt("cown", [128, 16])
    junk, Tjunk = sbt("junk", [128, D], BF16)
    KTb = [sbt("KTb%d" % i, [128, 8, 128], BF16) for i in range(2)]
    VAb = [sbt("VAb%d" % i, [128, 16, 65], BF16) for i in range(2)]
    for i in range(2):
        P.op("pool", lambda e, i=i: e.memset(VAb[i][0][:], 1.0), wr=[VAb[i][1]])
    SGb, TSGb = sbt("SGb", [128, D], BF16)
    l32, Tl32 = sbt("l32", [128, 16])
    r32, Tr32 = sbt("r32", [128, 16])
    ls = [sbt("ls%d" % i, [128, 16], BF16) for i in range(3)]
    As = [sbt("As%d" % i, [128, 16], BF16) for i in range(3)]
    cs3 = [sbt("cs3%d" % i, [128, 16], BF16) for i in range(3)]
    crb, Tcrb = sbt("crb", [128, 16])
    TKT, TVA, TNC, TCR, TQT, TSG, TXO = [scratch["T" + k] for k in ("KT", "VA", "NC", "CR", "QT", "SG", "XO")]

    def mkset(i):
        d = {}
        d["ss"] = sbt("ss_%d" % i, [128, 1])
        d["rstd"] = sbt("rstd_%d" % i, [128, 1])
        d["xn"] = sbt("xn_%d" % i, [128, D])
        d["hT"] = sbt("hT_%d" % i, [128, 8, 128], BF16)
        d["kf"] = sbt("kf_%d" % i, [128, D])
        d["tmp"] = sbt("tmp_%d" % i, [128, D])
        d["kn"] = sbt("kn_%d" % i, [128, D])
        d["ss16"] = sbt("ss16_%d" % i, [128, 16])
        d["BK"] = HalfBanks(C, 4 * i)
        return d

    sets = [mkset(0), mkset(1)]

    def block(b):
        d = sets[b % 2]
        BK = d["BK"]
        hT, ThT = d["hT"]
        scr = (d["ss"][0], d["ss"][1], d["rstd"][0], d["rstd"][1], junk, Tjunk, d["xn"][0], d["xn"][1], hT, ThT)
        (kf, Tkf), (tmp, Ttmp), (kn, Tkn), (ss16, Tss16) = [d[k] for k in ("kf", "tmp", "kn", "ss16")]

        def proj(col0, ncols=512):
            bk, Tbk = BK.next()
            for c in range(8):
                P.op("pe", lambda e, c=c, bk=bk: e.matmul(bk[:, 0:ncols], lhsT=hT[:, c, :],
                                                          rhs=w_in[:, c, col0:col0 + ncols],
                                                          start=(c == 0), stop=(c == 7)), rd=[ThT, Tw_in], wr=[Tbk])
            return bk, Tbk

        x, Txb = xb[b % 4]
        cm, Tcm = cum[b % 4]
        P.dma("act", x[:], x_in_ap[b * 128:(b + 1) * 128, :], rd=[Tx_in], wr=[Txb])
        norm_T(P, C, BK, "l1p", x[:], Txb, gexp, Tgexp, scr)
        yield
        for half in range(2):
            bk, Tbk = proj(1024 + half * 512)
            P.op("act", lambda e, bk=bk, half=half: e.activation(out=kf[:, half * 512:(half + 1) * 512], in_=bk[:],
                                                                func=AF.Copy), rd=[Tbk], wr=[Tkf])
            yield
        head_norm(P, "k", kf, Tkf, kg, Tkg, kn, Tkn, tmp, Ttmp, ss16, Tss16, 1.0)
        yield
        ktb, Tktb = KTb[b % 2]
        transpose_to(P, C, BK, kn, Tkn, 1024, ktb, Tktb)
        P.dma("sp", KTd.rearrange("h p t -> p h t")[:, :, b * 128:(b + 1) * 128], ktb[:], rd=[Tktb], wr=[TKT], store=True)
        yield
        vab, Tvab = VAb[b % 2]
        for half in range(2):
            bk, Tbk = proj(2048 + half * 512)
            P.op("act", lambda e, bk=bk, half=half, vab=vab: e.activation(
                out=vab[:, half * 8:(half + 1) * 8, 0:64], in_=bk[:].rearrange("p (h d) -> p h d", h=8),
                func=AF.Copy), rd=[Tbk], wr=[Tvab])
            yield
        P.dma("sp", VAd[b * 128:(b + 1) * 128, :], vab[:].rearrange("p h d -> p (h d)"), rd=[Tvab], wr=[TVA], store=True)
        bkf, Tbkf = proj(4096, 16)
        yield "MID"
        P.op("dve", lambda e, bkf=bkf: e.tensor_tensor(out=l32[:], in0=bkf[:, 0:16], in1=fbr[:], op=ALU.add),
             rd=[Tbkf, Tfbr], wr=[Tl32])
        P.op("act", lambda e: e.activation(out=l32[:], in_=l32[:], func=AF.Exp, scale=-1.0), rd=[Tl32], wr=[Tl32])
        P.op("act", lambda e: e.activation(out=l32[:], in_=l32[:], func=AF.Ln, bias=1.0), rd=[Tl32], wr=[Tl32])
        yield
        split3(P, "l", l32[:], Tl32, [t[0] for t in ls], [t[1] for t in ls], r32, Tr32)
        yield
        split3(P, "A", A[:], TA, [t[0] for t in As], [t[1] for t in As], r32, Tr32)
        yield
        bk, Tbk = BK.next()
        for i in range(3):
            P.op("pe", lambda e, i=i, bk=bk: e.matmul(bk[:, 0:16], lhsT=maskb[:], rhs=ls[i][0][:], start=(i == 0),
                                                      stop=False), rd=[Tmaskb, ls[i][1]], wr=[Tbk])
        for i in range(3):
            P.op("pe", lambda e, i=i, bk=bk: e.matmul(bk[:, 0:16], lhsT=onesb[:], rhs=As[i][0][:], start=False,
                                                      stop=(i == 2)), rd=[Tonesb, As[i][1]], wr=[Tbk])
        P.op("act", lambda e, bk=bk, cm=cm: e.activation(out=cm[:], in_=bk[:, 0:16], func=AF.Copy), rd=[Tbk], wr=[Tcm])
        P.op("dve", lambda e: e.tensor_tensor(out=A[:], in0=A[:], in1=l32[:], op=ALU.add), rd=[TA, Tl32], wr=[TA])
        P.dma("sp", NCd[b * 128:(b + 1) * 128, :], cm[:], rd=[Tcm], wr=[TNC], store=True)
        yield
        if b % 2 == 0:
            return
        j = b // 2
        xa, Txa = xb[(b - 1) % 4]
        xb_, Txb_ = xb[b % 4]
        ca, Tca = cum[(b - 1) % 4]
        cb, Tcb = cum[b % 4]
        P.op("dve", lambda e: e.tensor_scalar(out=xown[:], in0=xa[:], scalar1=sel[:, 0:1], scalar2=None,
                                              op0=ALU.mult), rd=[Txa, Tsel], wr=[Txown])
        P.op("dve", lambda e: e.scalar_tensor_tensor(out=xown[:], in0=xb_[:], scalar=sel[:, 1:2], in1=xown[:],
                                                     op0=ALU.mult, op1=ALU.add), rd=[Txb_, Tsel, Txown], wr=[Txown])
        P.dma("sp", XOd[j * 128:(j + 1) * 128, :], xown[:], rd=[Txown], wr=[TXO], store=True)
        yield
        P.op("dve", lambda e: e.tensor_scalar(out=cown[:], in0=ca[:], scalar1=sel[:, 0:1], scalar2=None,
                                              op0=ALU.mult), rd=[Tca, Tsel], wr=[Tcown])
        P.op("dve", lambda e: e.scalar_tensor_tensor(out=cown[:], in0=cb[:], scalar=sel[:, 1:2], in1=cown[:],
                                                     op0=ALU.mult, op1=ALU.add), rd=[Tcb, Tsel, Tcown], wr=[Tcown])
        split3(P, "c", cown[:], Tcown, [t[0] for t in cs3], [t[1] for t in cs3], r32, Tr32)
        yield
        bk, Tbk = BK.next()
        for i in range(3):
            P.op("pe", lambda e, i=i, bk=bk: e.matmul(bk[:, 0:16], lhsT=e127[:], rhs=cs3[i][0][:], start=(i == 0),
                                                      stop=(i == 2)), rd=[Te127, cs3[i][1]], wr=[Tbk])
        P.op("act", lambda e, bk=bk: e.activation(out=crb[:], in_=bk[:, 0:16], func=AF.Copy), rd=[Tbk], wr=[Tcrb])
        P.dma("sp", CRd[j], crb[:], rd=[Tcrb], wr=[TCR], store=True)
        yield
        norm_T(P, C, BK, "l1p", xown[:], Txown, gexp, Tgexp, scr)
        yield
        for half in range(2):
            bk, Tbk = proj(half * 512)
            P.op("act", lambda e, bk=bk, half=half: e.activation(out=kf[:, half * 512:(half + 1) * 512], in_=bk[:],
                                                                func=AF.Copy), rd=[Tbk], wr=[Tkf])
            yield
        head_norm(P, "q", kf, Tkf, qg, Tqg, kn, Tkn, tmp, Ttmp, ss16, Tss16, 0.125)
        yield
        qtb, Tqtb = KTb[b % 2]
        transpose_to(P, C, BK, kn, Tkn, 1024, qtb, Tqtb)
        P.dma("sp", QTd.rearrange("h p t -> p h t")[:, :, j * 128:(j + 1) * 128], qtb[:], rd=[Tqtb], wr=[TQT], store=True)
        yield
        for half in range(2):
            bk, Tbk = proj(3072 + half * 512)
            P.op("act", lambda e, bk=bk, half=half: e.activation(out=SGb[:, half * 512:(half + 1) * 512], in_=bk[:],
                                                                func=AF.Sigmoid), rd=[Tbk], wr=[TSGb])
            yield
        P.dma("sp", SGd[j * 128:(j + 1) * 128, :], SGb[:], rd=[TSGb], wr=[TSG], store=True)

    run_pipeline(block(b) for b in range(32))


def l1_attn_stage(P, C, x_out, W, msk_ap, scratch, sel_ap):
    x_out_ap, Tx_out = x_out
    KTd, VAd, NCd, CRd, QTd, SGd, XOd = [scratch[k] for k in ("KT", "VA", "NC", "CR", "QT", "SG", "XO")]
    TKT, TVA, TNC, TCR, TQT, TSG, TXO = [scratch["T" + k] for k in ("KT", "VA", "NC", "CR", "QT", "SG", "XO")]
    BK = Banks(C)
    w_o, Tw_o = load_weight_bf16(P, "l1a_w_o", W["w_o"], D, D)

    def sbt(name, shape, dt=F32):
        return P.sb("l1a_" + name, shape, dt), T("l1a_" + name)

    msk32, Tmsk32 = sbt("msk32", [128, 2, 128])
    P.dma("sp", msk32[:], msk_ap.rearrange("m s t -> s m t"), wr=[Tmsk32])
    mskb, Tmskb = sbt("mskb", [128, 2, 128], BF16)
    P.op("pool", lambda e: e.tensor_copy(out=mskb[:], in_=msk32[:]), rd=[Tmsk32], wr=[Tmskb])
    NC_, TNC_ = sbt("NC", [128, 32, 16])
    P.dma("sp", NC_[:], NCd.rearrange("(b p) h -> p b h", p=128), rd=[TNC], wr=[TNC_])
    CR_, TCR_ = sbt("CR", [128, 16, 16])
    P.dma("sp", CR_[:], CRd.rearrange("j p h -> p j h"), rd=[TCR], wr=[TCR_])
    jj = [(j, J) for j in range(16) for J in range(2 * j + 2)]
    bidx = {k: i for i, k in enumerate(jj)}
    bias, Tbias = sbt("bias", [128, len(jj), 16])
    for j in range(16):
        nJ = 2 * j + 2
        i0 = bidx[(j, 0)]
        P.op("dve", lambda e, j=j, nJ=nJ, i0=i0: e.tensor_tensor(
            out=bias[:, i0:i0 + nJ, :], in0=NC_[:, 0:nJ, :],
            in1=CR_[:, j:j + 1, :].to_broadcast([128, nJ, 16]), op=ALU.subtract), rd=[TNC_, TCR_], wr=[Tbias])
    sel, Tsel = sbt("sel", [128, 2])
    P.dma("sp", sel[:], sel_ap, wr=[Tsel])
    nbig, Tnbig = sbt("nbig", [128, 1])
    P.op("dve", lambda e: e.tensor_scalar(out=nbig[:], in0=sel[:, 0:1], scalar1=-30000.0, scalar2=None, op0=ALU.mult),
         rd=[Tsel], wr=[Tnbig])
    for j in range(16):
        bi_ = bidx[(j, 2 * j + 1)]
        P.op("dve", lambda e, bi_=bi_: e.tensor_scalar(out=bias[:, bi_, :], in0=bias[:, bi_, :], scalar1=nbig[:, 0:1],
                                                     scalar2=None, op0=ALU.add), rd=[Tbias, Tnbig], wr=[Tbias])
    nb_ = len(jj)
    bm, Tbm = sbt("bm", [128, nb_, 8])
    b4 = bias[:].rearrange("p n (hp two) -> p n hp two", two=2)
    P.op("dve", lambda e: e.tensor_tensor(out=bm[:], in0=b4[:, :, :, 0], in1=b4[:, :, :, 1], op=ALU.max),
         rd=[Tbias], wr=[Tbm])
    P.op("dve", lambda e: e.tensor_tensor(out=b4, in0=b4, in1=bm[:].unsqueeze(3).to_broadcast([128, nb_, 8, 2]),
                                          op=ALU.subtract), rd=[Tbias, Tbm], wr=[Tbias])
    P.op("act", lambda e: e.activation(out=bias[:], in_=bias[:], func=AF.Exp), rd=[Tbias], wr=[Tbias])
    vsb = [sbt("vs%d" % i, [128, 2, 65], BF16) for i in range(4)]
    QTP = [sbt("QTP%d" % i, [128, 16, 2, 128], BF16) for i in range(2)]
    for i in range(2):
        P.op("pool", lambda e, i=i: e.memset(QTP[i][0][:], 0.0), wr=[QTP[i][1]])
    oT, ToT = sbt("oT", [128, 8, 2048], BF16)
    KT = [sbt("KT%d" % i, [128, 4096], BF16) for i in range(2)]
    VA = [sbt("VA%d" % i, [128, 32, 130], BF16) for i in range(2)]
    SG = [sbt("SG%d" % i, [128, 16, 128], BF16) for i in range(2)]
    pT = [sbt("pT%d" % i, [128, 2, 128], BF16) for i in range(4)]
    rec, Trec = sbt("rec", [128, 2])
    o2s = [sbt("o2_%d" % i, [128, 128]) for i in range(2)]
    xo = [sbt("xo%d" % i, [128, D]) for i in range(2)]
    xr = [sbt("xr%d" % i, [128, D]) for i in range(2)]
    pcount = 0
    pocount = 0
    if 'a_noloop' in DBG:
        P.op("pool", lambda e: e.memset(oT[:], 0.0), wr=[ToT])
    for hp in range(8 if 'a_noloop' not in DBG else 0):
        kt, Tkt = KT[hp % 2]
        va, Tva = VA[hp % 2]
        sg, Tsg = SG[hp % 2]
        P.dma("sp", kt[:], KTd[hp], rd=[TKT], wr=[Tkt])
        qtp, Tqtp = QTP[hp % 2]
        for h2 in range(2):
            P.dma("sp", qtp[h2 * 64:(h2 + 1) * 64, :, h2, :],
                  QTd[hp][h2 * 64:(h2 + 1) * 64, :].rearrange("p (j t) -> p j t", t=128), rd=[TQT], wr=[Tqtp])
        P.dma("sp", va[:].rearrange("p b (h d) -> p b h d", h=2),
              VAd.rearrange("(b p) (h d) -> p b h d", p=128, d=65)[:, :, 2 * hp:2 * hp + 2, :], rd=[TVA], wr=[Tva])
        P.dma("sp", sg[:], SGd.rearrange("(j p) c -> p j c", p=128)[:, :, hp * 128:(hp + 1) * 128], rd=[TSG], wr=[Tsg])
        items = [(j, J) for j in range(16) for J in range(2 * j + 2)]
        LOOK = 2

        def emit_qk(i, kt=kt, Tkt=Tkt, qtp=qtp, Tqtp=Tqtp):
            j, J = items[i]
            ps, Tps = C.bank[i % 3], C.Tbank[i % 3]
            P.op("pe", lambda e, ps=ps, J=J, j=j: e.matmul(
                ps[:, 0:256], lhsT=kt[:, J * 128:(J + 1) * 128],
                rhs=qtp[:, j, :, :].rearrange("p a t -> p (a t)"), start=True, stop=True),
                 rd=[Tkt, Tqtp], wr=[Tps])

        def emit_tr(pend):
            j_, o2_, To2_ = pend
            bk, Tbk = C.bank[3], C.Tbank[3]
            P.op("pe", lambda e, bk=bk, o2_=o2_: e.transpose(out=bk[:, 0:128], in_=o2_[:], identity=C.ident[:]),
                 rd=[To2_, C.Tident], wr=[Tbk])
            P.op("act", lambda e, bk=bk, hp=hp, j_=j_: e.activation(out=oT[:, hp, j_ * 128:(j_ + 1) * 128],
                                                                    in_=bk[:, 0:128], func=AF.Copy), rd=[Tbk], wr=[ToT])

        for i in range(LOOK):
            emit_qk(i)
        pending = None
        for i, (j, J) in enumerate(items):
            nJ = 2 * j + 2
            if i + LOOK < len(items):
                emit_qk(i + LOOK)
            ps, Tps = C.bank[i % 3], C.Tbank[i % 3]
            p_, Tp_ = pT[i % 4]
            if J == 0:
                pob = [(C.bank[4 + 2 * (pocount % 2) + h2], C.Tbank[4 + 2 * (pocount % 2) + h2]) for h2 in range(2)]
                o2, To2 = o2s[pocount % 2]
                pocount += 1
            bi = bidx[(j, J)]
            P.op("act", lambda e, ps=ps, p_=p_, bi=bi, hp=hp: e.activation(
                out=p_[:].rearrange("p a t -> p (a t)"), in_=ps[:, 0:256], func=AF.Exp,
                bias=bm[:, bi, hp:hp + 1]), rd=[Tps, Tbm], wr=[Tp_])
            vs, Tvs = vsb[i % 4]
            P.op("dve", lambda e, vs=vs, va=va, J=J, bi=bi, hp=hp: e.tensor_tensor(
                out=vs[:], in0=va[:, J, :].rearrange("p (h d) -> p h d", h=2),
                in1=bias[:, bi, 2 * hp:2 * hp + 2].unsqueeze(2).to_broadcast([128, 2, 65]), op=ALU.mult),
                 rd=[Tva, Tbias], wr=[Tvs])
            if J >= 2 * j:
                m = J - 2 * j
                P.op("pool", lambda e, p_=p_, m=m: e.tensor_tensor(
                    out=p_[:], in0=p_[:], in1=mskb[:, m:m + 1, :].to_broadcast([128, 2, 128]), op=ALU.mult),
                     rd=[Tp_, Tmskb], wr=[Tp_])
            for h2 in range(2):
                po, Tpo = pob[h2]
                P.op("pe", lambda e, po=po, h2=h2, p_=p_, vs=vs, J=J, nJ=nJ: e.matmul(
                    po[:, 0:65], lhsT=p_[:, h2, :], rhs=vs[:, h2, :],
                    start=(J == 0), stop=(J == nJ - 1)), rd=[Tp_, Tvs], wr=[Tpo])
            if pending is not None and J == 1:
                emit_tr(pending)
                pending = None
            if J == nJ - 1:
                for h2 in range(2):
                    po, Tpo = pob[h2]
                    P.op("dve", lambda e, po=po, h2=h2: e.reciprocal(out=rec[:, h2:h2 + 1], in_=po[:, 64:65]),
                         rd=[Tpo], wr=[Trec])
                    P.op("dve", lambda e, po=po, h2=h2, sg=sg, j=j, o2=o2: e.scalar_tensor_tensor(
                        out=o2[:, h2 * 64:(h2 + 1) * 64], in0=po[:, 0:64], scalar=rec[:, h2:h2 + 1],
                        in1=sg[:, j, h2 * 64:(h2 + 1) * 64], op0=ALU.mult, op1=ALU.mult),
                         rd=[Tpo, Trec, Tsg], wr=[To2])
                pending = (j, o2, To2)
        emit_tr(pending)
    for j in range(16):
        xr_, Txr = xr[j % 2]
        xo_, Txo = xo[j % 2]
        P.dma("sp", xr_[:], XOd[j * 128:(j + 1) * 128, :], rd=[TXO], wr=[Txr])
        for half in range(2):
            pw, Tpw = BK.next()
            for c in range(8):
                P.op("pe", lambda e, c=c, pw=pw, half=half, j=j: e.matmul(
                    pw[:], lhsT=oT[:, c, j * 128:(j + 1) * 128], rhs=w_o[:, c, half * 512:(half + 1) * 512],
                    start=(c == 0), stop=(c == 7)), rd=[ToT, Tw_o], wr=[Tpw])
            P.op("dve", lambda e, pw=pw, half=half, xr_=xr_, xo_=xo_: e.tensor_tensor(
                out=xo_[:, half * 512:(half + 1) * 512], in0=pw[:], in1=xr_[:, half * 512:(half + 1) * 512], op=ALU.add),
                 rd=[Tpw, Txr], wr=[Txo])
        P.dma("act", x_out_ap[j * 128:(j + 1) * 128, :], xo_[:], rd=[Txo], wr=[Tx_out], store=True)


W_SHAPES = {
    "even_norm_mix": [D], "even_w_in": [D, 2576], "even_gate_up": [16, 256], "even_gate_bias": [256],
    "even_w_s": [4, 128, 128], "even_b_s": [4, 128], "even_ln_g": [512], "even_ln_b": [512],
    "even_head_g": [4, 128], "even_w_o": [D, D], "even_norm_ffn": [D], "even_ffn_w1": [1, D, 2816],
    "even_ffn_w3": [1, D, 2816], "even_ffn_w2": [1, 2816, D], "odd_norm_mix": [D], "odd_w_in": [D, 4112],
    "odd_forget_bias": [16], "odd_q_g": [64], "odd_k_g": [64], "odd_w_o": [D, D], "odd_norm_ffn": [D],
    "odd_router": [D, 8], "odd_exp_w1": [8, D, 3584], "odd_exp_w3": [8, D, 3584], "odd_exp_w2": [8, 3584, D],
    "final_norm": [D],
}
SEQ = 4096


def build_program(stop=None):
    nc = bass.Bass("TRN2", target_bir_lowering=False)
    x = nc.dram_tensor("x", [SEQ, D], F32, kind="ExternalInput").ap()
    Wd = {k: nc.dram_tensor(k, s, F32, kind="ExternalInput").ap() for k, s in W_SHAPES.items()}
    sel = nc.dram_tensor("sel", [128, 2], F32, kind="ExternalInput").ap()
    msk = nc.dram_tensor("msk", [2, 128, 128], F32, kind="ExternalInput").ap()
    y = nc.dram_tensor("y", [SEQ if stop in ("l0m", "l0f") else SEQ // 2, D], F32, kind="ExternalOutput").ap()
    xmid = y if stop == "l0m" else nc.dram_tensor("xmid", [SEQ, D], F32).ap()
    x1 = y if stop == "l0f" else nc.dram_tensor("x1", [SEQ, D], F32).ap()
    x2 = y if stop == "l1a" else nc.dram_tensor("x2", [SEQ // 2, D], F32).ap()
    scratch = {
        "KT": nc.dram_tensor("KTd", [8, 128, SEQ], BF16).ap(),
        "VA": nc.dram_tensor("VAd", [SEQ, 16 * 65], BF16).ap(),
        "NC": nc.dram_tensor("NCd", [SEQ, 16], F32).ap(),
        "CR": nc.dram_tensor("CRd", [16, 128, 16], F32).ap(),
        "QT": nc.dram_tensor("QTd", [8, 128, SEQ // 2], BF16).ap(),
        "SG": nc.dram_tensor("SGd", [SEQ // 2, D], BF16).ap(),
        "XO": nc.dram_tensor("XOd", [SEQ // 2, D], F32).ap(),
    }
    for k in list(scratch.keys()):
        scratch["T" + k] = T(k)
    with ExitStack() as es:
        P = Prog(nc, es)
        C = Ctx(P)
        Tx, Txmid, Tx1, Tx2, Ty = T("x"), T("xmid"), T("x1"), T("x2"), T("y")
        P.emit()
        _skip = ""
        with P.stage():
            l0_mixer_stage(P, C, (x, Tx), (xmid, Txmid), SEQ if "l0m" not in _skip else 256, {
                "norm_mix": Wd["even_norm_mix"], "w_in": Wd["even_w_in"], "gate_up": Wd["even_gate_up"],
                "gate_bias": Wd["even_gate_bias"], "w_s": Wd["even_w_s"], "b_s": Wd["even_b_s"],
                "ln_g": Wd["even_ln_g"], "ln_b": Wd["even_ln_b"], "head_g": Wd["even_head_g"],
                "w_o": Wd["even_w_o"]})
            P.wait_all("sp", [Txmid])
        if stop == "l0m":
            return nc
        with P.stage():
            swiglu_stage(P, C, "f0", (xmid, Txmid), (x1, Tx1), SEQ, Wd["even_norm_ffn"], Wd["even_ffn_w1"],
                         Wd["even_ffn_w3"], Wd["even_ffn_w2"], 2816)
            P.wait_all("sp", [Tx1])
        if stop == "l0f":
            return nc
        with P.stage():
            l1_proj_stage(P, C, (x1, Tx1), {"norm_mix": Wd["odd_norm_mix"], "w_in": Wd["odd_w_in"],
                                            "forget_bias": Wd["odd_forget_bias"], "q_g": Wd["odd_q_g"],
                                            "k_g": Wd["odd_k_g"]}, sel, scratch)
            P.wait_all("sp", [scratch["T" + k] for k in ("KT", "VA", "NC", "CR", "QT", "SG", "XO")])
        with P.stage():
            l1_attn_stage(P, C, (x2, Tx2), {"w_o": Wd["odd_w_o"]}, msk, scratch, sel)
            P.wait_all("sp", [Tx2])
        if stop == "l1a":
            return nc
        with P.stage():
            swiglu_stage(P, C, "moe", (x2, Tx2), (y, Ty), SEQ // 2, Wd["odd_norm_ffn"], Wd["odd_exp_w1"],
                         Wd["odd_exp_w3"], Wd["odd_exp_w2"], 3584, router=Wd["odd_router"], nexp=8,
                         g_final=Wd["final_norm"])
            P.wait_all("sp", [Ty])
    return nc


def kernel(**inputs):
    x = np.ascontiguousarray(np.asarray(inputs["x"], dtype=np.float32))
    wmap = {}
    for k, shp in W_SHAPES.items():
        a = np.asarray(inputs[k], dtype=np.float32)
        if k in ("even_ffn_w1", "even_ffn_w3", "even_ffn_w2", "odd_exp_w1", "odd_exp_w3", "odd_exp_w2"):
            a = a.reshape(shp)
        elif k != "final_norm":
            a = a[0]
        wmap[k] = np.ascontiguousarray(a.reshape(shp))
    tril = np.triu(np.ones((128, 128), np.float32))
    msks = [np.stack([tril, np.zeros_like(tril)]), np.stack([np.ones_like(tril), tril])]
    sels = [np.tile(np.array([[1.0, 0.0]], np.float32), (128, 1)), np.tile(np.array([[0.0, 1.0]], np.float32), (128, 1))]
    nc = build_program()
    in_maps = []
    for core in range(8):
        b, par = core // 2, core % 2
        m = {"x": x[b], "sel": sels[par], "msk": msks[par]}
        m.update(wmap)
        in_maps.append(m)
    res = run_bass_kernel_spmd(nc, in_maps, core_ids=list(range(8)))
    out = np.empty((4, SEQ, D), np.float32)
    for core in range(8):
        b, par = core // 2, core % 2
        yv = np.asarray(res.results[core]["y"]).reshape(16, 128, D)
        out[b].reshape(16, 2, 128, D)[:, par] = yv
    return out


SCR_SPECS = {"KT": ([8, 128, SEQ], BF16), "VA": ([SEQ, 16 * 65], BF16), "NC": ([SEQ, 16], F32),
             "CR": ([16, 128, 16], F32), "QT": ([8, 128, SEQ // 2], BF16), "SG": ([SEQ // 2, D], BF16),
             "XO": ([SEQ // 2, D], F32)}
STAGE_W = {
    "l0m": ["even_norm_mix", "even_w_in", "even_gate_up", "even_gate_bias", "even_w_s", "even_b_s", "even_ln_g",
            "even_ln_b", "even_head_g", "even_w_o"],
    "l0f": ["even_norm_ffn", "even_ffn_w1", "even_ffn_w3", "even_ffn_w2"],
    "l1p": ["odd_norm_mix", "odd_w_in", "odd_forget_bias", "odd_q_g", "odd_k_g"],
    "l1a": ["odd_w_o"],
    "moe": ["odd_norm_ffn", "odd_router", "odd_exp_w1", "odd_exp_w3", "odd_exp_w2", "final_norm"],
}


def build_single(stage):
    nc = bass.Bass("TRN2", target_bir_lowering=False)
    Wd = {k: nc.dram_tensor(k, W_SHAPES[k], F32, kind="ExternalInput").ap() for k in STAGE_W[stage]}
    n_in = SEQ if stage in ("l0m", "l0f", "l1p") else SEQ // 2
    n_out = SEQ if stage in ("l0m", "l0f") else SEQ // 2
    xin, y, scratch = None, None, {}
    if stage != "l1a":
        xin = nc.dram_tensor("xin", [n_in, D], F32, kind="ExternalInput").ap()
    if stage != "l1p":
        y = nc.dram_tensor("y", [n_out, D], F32, kind="ExternalOutput").ap()
    if stage in ("l1p", "l1a"):
        kind = "ExternalOutput" if stage == "l1p" else "ExternalInput"
        for k, (shp, dt) in SCR_SPECS.items():
            scratch[k] = nc.dram_tensor("s_" + k, shp, dt, kind=kind).ap()
            scratch["T" + k] = T(k)
    if stage in ("l1p", "l1a"):
        sel = nc.dram_tensor("sel", [128, 2], F32, kind="ExternalInput").ap()
    if stage == "l1a":
        msk = nc.dram_tensor("msk", [2, 128, 128], F32, kind="ExternalInput").ap()
    with ExitStack() as es:
        P = Prog(nc, es)
        C = Ctx(P)
        Tx, Ty = T("xin"), T("y")
        P.emit()
        with P.stage():
            if stage == "l0m":
                l0_mixer_stage(P, C, (xin, Tx), (y, Ty), SEQ, {k[5:]: Wd[k] for k in STAGE_W[stage]})
                P.wait_all("sp", [Ty])
            elif stage == "l0f":
                swiglu_stage(P, C, "f0", (xin, Tx), (y, Ty), SEQ, Wd["even_norm_ffn"], Wd["even_ffn_w1"],
                             Wd["even_ffn_w3"], Wd["even_ffn_w2"], 2816)
                P.wait_all("sp", [Ty])
            elif stage == "l1p":
                l1_proj_stage(P, C, (xin, Tx), {k[4:]: Wd[k] for k in STAGE_W[stage]}, sel, scratch)
                P.wait_all("sp", [scratch["T" + k] for k in SCR_SPECS])
            elif stage == "l1a":
                l1_attn_stage(P, C, (y, Ty), {"w_o": Wd["odd_w_o"]}, msk, scratch, sel)
                P.wait_all("sp", [Ty])
            elif stage == "moe":
                swiglu_stage(P, C, "moe", (xin, Tx), (y, Ty), SEQ // 2, Wd["odd_norm_ffn"], Wd["odd_exp_w1"],
                             Wd["odd_exp_w3"], Wd["odd_exp_w2"], 3584, router=Wd["odd_router"], nexp=8,
                             g_final=Wd["final_norm"])
                P.wait_all("sp", [Ty])
    return nc


def kernel_unfused(**inputs):
    x = np.ascontiguousarray(np.asarray(inputs["x"], dtype=np.float32))
    wmap = {k: np.ascontiguousarray(np.asarray(inputs[k], dtype=np.float32).reshape(shp)) for k, shp in W_SHAPES.items()}
    tril = np.triu(np.ones((128, 128), np.float32))
    msks = [np.stack([tril, np.zeros_like(tril)]), np.stack([np.ones_like(tril), tril])]
    sels = [np.tile(np.array([[1.0, 0.0]], np.float32), (128, 1)), np.tile(np.array([[0.0, 1.0]], np.float32), (128, 1))]
    cores = list(range(8))
    cur = [{"xin": x[c // 2]} for c in cores]
    for stage in ("l0m", "l0f", "l1p", "l1a", "moe"):
        nc = build_single(stage)
        in_maps = []
        for c in cores:
            m = dict(cur[c])
            for k in STAGE_W[stage]:
                m[k] = wmap[k]
            if stage in ("l1p", "l1a"):
                m["sel"] = sels[c % 2]
            if stage == "l1a":
                m["msk"] = msks[c % 2]
            in_maps.append(m)
        res = run_bass_kernel_spmd(nc, in_maps, core_ids=cores)
        if stage == "l1p":
            cur = [{"s_" + k: np.asarray(res.results[c]["s_" + k]) for k in SCR_SPECS} for c in cores]
        else:
            cur = [{"xin": np.asarray(res.results[c]["y"])} for c in cores]
    out = np.empty((4, SEQ, D), np.float32)
    for c in cores:
        b, par = c // 2, c % 2
        out[b].reshape(16, 2, 128, D)[:, par] = cur[c]["xin"].reshape(16, 128, D)
    return out
```

```python
import numpy as np
from contextlib import ExitStack, contextmanager
import concourse.bass as bass
import concourse.mybir as mybir
from concourse.bass_utils import run_bass_kernel_spmd

F32 = mybir.dt.float32
BF16 = mybir.dt.bfloat16
I32 = mybir.dt.int32
AF = mybir.ActivationFunctionType
ALU = mybir.AluOpType
AX = mybir.AxisListType


class T:
    __slots__ = ("name", "w", "r", "dsem", "dcnt")

    def __init__(self, name):
        self.name = name
        self.w = {}
        self.r = {}
        self.dsem = None
        self.dcnt = 0


class Prog:
    ENGS = ("pe", "act", "dve", "pool", "sp")

    def __init__(self, nc, es):
        self.nc = nc
        self.es = es
        self.es_outer = es
        self.ops = {e: [] for e in self.ENGS}
        self.seen = {e: {} for e in self.ENGS}
        self.ndsem = 0
        self.dsems = []

    @contextmanager
    def stage(self):
        with ExitStack() as es:
            old = self.es
            self.es = es
            yield
            self.emit()
            self.es = old

    def sb(self, name, shape, dtype):
        nb = 1
        for d_ in shape[1:]:
            nb *= d_
        nb *= 2 if dtype == BF16 else 4
        self.sb_bytes = getattr(self, "sb_bytes", 0) + ((nb + 31) // 32) * 32
        self.sb_log = getattr(self, "sb_log", [])
        self.sb_log.append((self.es, ((nb + 31) // 32) * 32))
        live = sum(b for (es_, b) in self.sb_log if es_ is self.es or es_ is self.es_outer)
        assert live <= 196 * 1024, "SBUF budget exceeded: %d" % live
        return self.es.enter_context(self.nc.sbuf_tensor(name, list(shape), dtype))

    def ps(self, name, shape, dtype):
        return self.es.enter_context(self.nc.psum_tensor(name, list(shape), dtype))

    def _dsem(self, t):
        if t.dsem is None:
            t.dsem = self.ndsem
            self.ndsem += 1
        return t.dsem

    def _deps(self, eng, rd, wr, is_dma_group_sem=None, merge=False):
        deps = []
        for t in rd:
            for tok in t.w.values():
                deps.append((tok, True))
        for t in wr:
            if not merge:
                for tok in t.w.values():
                    if not (is_dma_group_sem is not None and tok[0] == "D" and tok[1] == is_dma_group_sem):
                        deps.append((tok, False))
            for tok in t.r.values():
                deps.append((tok, False))
        waits = []
        seen = self.seen[eng]
        for tok, raw in deps:
            key = (tok[0], tok[1])
            if tok[0] == "E" and tok[1] == eng and not raw:
                continue
            if seen.get(key, -1) >= tok[2]:
                continue
            seen[key] = tok[2]
            waits.append(tok)
        best = {}
        for tok in waits:
            key = (tok[0], tok[1])
            if key not in best or best[key][2] < tok[2]:
                best[key] = tok
        return list(best.values())

    def op(self, eng, fn, rd=(), wr=()):
        rd = [t for t in rd if t is not None]
        wr = [t for t in wr if t is not None]
        waits = self._deps(eng, rd, wr)
        idx = len(self.ops[eng])
        tok = ("E", eng, idx)
        self.ops[eng].append({"fn": fn, "waits": waits, "tok": tok, "need_inc": False})
        for t in rd:
            t.r[("E", eng)] = tok
        for t in wr:
            t.w = {("E", eng): tok}
            t.r = {}
        return tok

    def dma(self, q, out, in_, rd=(), wr=(), **kw):
        rd = [t for t in rd if t is not None]
        wr = [t for t in wr if t is not None]
        store = kw.pop("store", False)
        owner = rd[0] if store else wr[0]
        ds = self._dsem(owner)
        waits = self._deps(q, rd, wr, is_dma_group_sem=ds, merge=store)
        owner.dcnt += 16
        tok = ("D", ds, owner.dcnt)
        self.ops[q].append({"dma": (out, in_, kw), "waits": waits, "tok": tok})
        for t in rd:
            t.r[("D", ds)] = tok
        for t in wr:
            if store:
                t.w[("D", ds)] = tok
            else:
                t.w = {("D", ds): tok}
            t.r = {}
        return tok

    def wait_all(self, eng, tiles):
        waits = self._deps(eng, tiles, [])
        self.ops[eng].append({"fn": None, "waits": waits, "tok": None, "need_inc": False})

    def emit(self):
        nc = self.nc
        if not hasattr(self, "_start"):
            self._start = {e: 0 for e in self.ENGS}
            self._rank = {e: 0 for e in self.ENGS}
            self._esem = {e: self.es_outer.enter_context(nc.semaphore("s_" + e)) for e in self.ENGS}
            self._dsl = []
        start = self._start
        for e in self.ENGS:
            for o in self.ops[e][start[e]:]:
                for tok in o["waits"]:
                    if tok[0] == "E":
                        assert tok[2] >= start[tok[1]], "wait on op from an earlier block"
                        self.ops[tok[1]][tok[2]]["need_inc"] = True
        rank = {}
        for e in self.ENGS:
            c = self._rank[e]
            for i in range(start[e], len(self.ops[e])):
                if self.ops[e][i].get("need_inc"):
                    c += 1
                    rank[(e, i)] = c
            self._rank[e] = c
        while len(self._dsl) < self.ndsem:
            self._dsl.append(self.es_outer.enter_context(nc.semaphore("d%d" % len(self._dsl))))
        esem, dsem = self._esem, self._dsl
        engobj = {"pe": "tensor", "act": "scalar", "dve": "vector", "pool": "gpsimd", "sp": "sync"}

        def body(e):
            def run(engine):
                for i in range(start[e], len(self.ops[e])):
                    o = self.ops[e][i]
                    for tok in o["waits"]:
                        if tok[0] == "E":
                            engine.wait_ge(esem[tok[1]], rank[(tok[1], tok[2])])
                        else:
                            engine.wait_ge(dsem[tok[1]], tok[2])
                    if "dma" in o:
                        out, in_, kw = o["dma"]
                        engine.dma_start(out=out, in_=in_, **kw).then_inc(dsem[o["tok"][1]], 16)
                    elif o["fn"] is not None:
                        ins = o["fn"](engine)
                        if o["need_inc"]:
                            ins.then_inc(esem[e], 1)
            return run

        with nc.Block() as block:
            for e in self.ENGS:
                if len(self.ops[e]) > start[e]:
                    getattr(block, engobj[e])(body(e))
        for e in self.ENGS:
            start[e] = len(self.ops[e])
        for e in self.ENGS:
            for e2 in self.ENGS:
                if self.ops[e2]:
                    self.seen[e][("E", e2)] = len(self.ops[e2]) - 1


import os
DBG = os.environ.get("KDBG", "")

D = 1024
EPS = 1e-6


class Ctx:
    def __init__(self, P):
        self.P = P
        nc = P.nc
        self.bank = [P.ps("bank%d" % i, [128, 512], F32) for i in range(8)]
        self.Tbank = [T("bank%d" % i) for i in range(8)]
        self.ident = P.sb("ident", [128, 128], F32)
        self.Tident = T("ident")
        self.identb = P.sb("identb", [128, 128], BF16)
        self.Tidentb = T("identb")
        self.ones = P.sb("ones", [128, 128], F32)
        self.Tones = T("ones")
        P.op("pool", lambda e: e.memset(self.ones[:], 1.0), wr=[self.Tones])
        P.op("pool", lambda e: e.memset(self.ident[:], 1.0), wr=[self.Tident])
        P.op("pool", lambda e: e.affine_select(out=self.ident[:], in_=self.ident[:], pattern=[[-1, 128]],
                                               compare_op=ALU.is_equal, fill=0.0, base=0,
                                               channel_multiplier=1), rd=[self.Tident], wr=[self.Tident])
        P.op("pool", lambda e: e.tensor_copy(out=self.identb[:], in_=self.ident[:]),
             rd=[self.Tident], wr=[self.Tidentb])


def load_gexp(P, C, name, g_dram):
    gT = P.sb(name + "_gT", [128, 8], F32)
    TgT = T(name + "_gT")
    gexp = P.sb(name + "_gexp", [128, 8, 128], F32)
    Tg = T(name + "_gexp")
    P.dma("sp", gT[:], g_dram.rearrange("(c p) -> p c", p=128), wr=[TgT], allow_slow_non_contiguous=True)
    for c in range(8):
        P.op("dve", lambda e, c=c: e.tensor_scalar(out=gexp[:, c, :], in0=C.ones[:], scalar1=gT[:, c:c + 1],
                                                   scalar2=None, op0=ALU.mult),
             rd=[C.Tones, TgT], wr=[Tg])
    return gexp, Tg


def swiglu_stage(P, C, name, x_in, x_out, ntok, g_norm, w1, w3, w2, dff, router=None, nexp=1,
                 g_final=None):
    nc = P.nc
    x_in_ap, Tx_in = x_in
    x_out_ap, Tx_out = x_out
    NB = 16
    npass = ntok // (NB * 128)
    nff = dff // 128
    groups = []
    f0 = 0
    while f0 < nff:
        gsz = min(4, nff - f0)
        groups.append((f0, gsz))
        f0 += gsz
    moe = router is not None

    gexp, Tgexp = load_gexp(P, C, name + "_gn", g_norm)
    xacc = P.sb(name + "_xacc", [128, NB, D], F32)
    Txacc = [T(name + "_xacc%d" % i) for i in range(NB)]
    hT = P.sb(name + "_hT", [128, 8, NB * 128], BF16)
    ThT = [T(name + "_hT%d" % i) for i in range(NB)]
    hid = P.sb(name + "_hid", [128, 4, NB * 128], BF16)
    Thid = [[T(name + "_hid%d_%d" % (f, t)) for t in range(4)] for f in range(4)]
    junk = P.sb(name + "_junk", [128, D], BF16)
    Tjunk = T(name + "_junk")
    xn = P.sb(name + "_xn", [128, D], F32)
    Txn = T(name + "_xn")
    ss = P.sb(name + "_ss", [128, NB], F32)
    Tss = [T(name + "_ss%d" % i) for i in range(NB)]
    rstd = P.sb(name + "_rstd", [128, NB], F32)
    Trstd = [T(name + "_rstd%d" % i) for i in range(NB)]
    sa = [P.sb(name + "_sa%d" % i, [128, 512], F32) for i in range(2)]
    Tsa = [T(name + "_sa%d" % i) for i in range(2)]
    wbuf = {}
    for par in range(2):
        wbuf[("w1", par)] = (P.sb(name + "_w1g%d" % par, [128, 8, 512], BF16), [T("w1g") for _ in range(8)])
        wbuf[("w3", par)] = (P.sb(name + "_w3g%d" % par, [128, 8, 512], BF16), [T("w3g") for _ in range(8)])
        wbuf[("w2", par)] = (P.sb(name + "_w2g%d" % par, [128, 4, 1024], BF16), [T("w2g") for _ in range(4)])
    NSTG = 2
    stg = [P.sb(name + "_stg%d" % i, [128, 1024], F32) for i in range(NSTG)]
    Tstg = [T(name + "_stg%d" % i) for i in range(NSTG)]
    stg_i = [0]
    if moe:
        rw = P.sb(name + "_rw", [128, 8, nexp], F32)
        Trw = T(name + "_rw")
        P.dma("sp", rw[:], router.rearrange("(c p) e -> p c e", p=128), wr=[Trw], allow_slow_non_contiguous=True)
        rwh = P.sb(name + "_rwh", [128, 8, nexp], BF16)
        rwl = P.sb(name + "_rwl", [128, 8, nexp], BF16)
        Trwh, Trwl = T(name + "_rwh"), T(name + "_rwl")
        P.op("pool", lambda e: e.tensor_copy(out=rwh[:], in_=rw[:]), rd=[Trw], wr=[Trwh])
        P.op("pool", lambda e: e.tensor_tensor(out=rwl[:], in0=rw[:], in1=rwh[:], op=ALU.subtract),
             rd=[Trw, Trwh], wr=[Trwl])
        hlo = P.sb(name + "_hlo", [128, 8, 128], BF16)
        Thlo = T(name + "_hlo")
        h32 = P.sb(name + "_h32", [128, 8, 128], F32)
        Th32 = T(name + "_h32")
        gates = P.sb(name + "_gates", [128, NB, nexp], F32)
        Tgates = [T(name + "_gates%d" % i) for i in range(NB)]
        gs = {k: P.sb(name + "_g" + k, [128, 8], F32) for k in ("lg", "m8", "ex", "mk", "ge")}
        Tgs = {k: T(name + "_g" + k) for k in gs}
        gs1 = {k: P.sb(name + "_g" + k, [128, 1], F32) for k in ("nm", "den", "rden")}
        Tgs1 = {k: T(name + "_g" + k) for k in gs1}
    if 'nofinal' in DBG:
        g_final = None
    if g_final is not None:
        gfin = P.sb(name + "_gfin", [128, D], F32)
        Tgfin = T(name + "_gfin")
        P.dma("sp", gfin[:], g_final.partition_broadcast(128), wr=[Tgfin])

    def load_w(kind, par, e, f0, gsz):
        buf, Ts = wbuf[(kind, par)]
        if kind in ("w1", "w3"):
            src = w1 if kind == "w1" else w3
            for c in range(8):
                i = stg_i[0] % NSTG
                stg_i[0] += 1
                ncol = gsz * 128
                P.dma("sp", stg[i][:, 0:ncol], src[e, c * 128:(c + 1) * 128, f0 * 128:f0 * 128 + ncol],
                      wr=[Tstg[i]])
                P.op("pool", lambda eng, i=i, c=c, ncol=ncol, buf=buf: eng.tensor_copy(
                    out=buf[:, c, 0:ncol], in_=stg[i][:, 0:ncol]), rd=[Tstg[i]], wr=[Ts[c]])
        else:
            for fl in range(gsz):
                i = stg_i[0] % NSTG
                stg_i[0] += 1
                f = f0 + fl
                P.dma("sp", stg[i][:, :], w2[e, f * 128:(f + 1) * 128, :], wr=[Tstg[i]])
                P.op("pool", lambda eng, i=i, fl=fl, buf=buf: eng.tensor_copy(
                    out=buf[:, fl, :], in_=stg[i][:, :]), rd=[Tstg[i]], wr=[Ts[fl]])

    bA = [(C.bank[0], C.Tbank[0]), (C.bank[1], C.Tbank[1])]
    bB = [(C.bank[2], C.Tbank[2]), (C.bank[3], C.Tbank[3])]
    bO = [(C.bank[4 + i], C.Tbank[4 + i]) for i in range(4)]
    bR = (C.bank[6], C.Tbank[6])

    for ps_ in range(npass):
        tok0 = ps_ * NB * 128
        work = [(e, gi) for e in range(nexp) for gi in range(len(groups))]
        gcount = 0
        e0, gi0 = work[0]
        if 'noexp' not in DBG:
            load_w("w1", 0, e0, *groups[gi0])
            load_w("w3", 0, e0, *groups[gi0])
            load_w("w2", 0, e0, *groups[gi0])
        for b in range(NB):
            P.dma("act", xacc[:, b, :], x_in_ap[tok0 + b * 128: tok0 + (b + 1) * 128, :],
                  rd=[Tx_in], wr=[Txacc[b]])
        for b in range(NB):
            P.op("act", lambda e, b=b: e.activation(out=junk[:], in_=xacc[:, b, :], func=AF.Square,
                                                    accum_out=ss[:, b:b + 1]),
                 rd=[Txacc[b]], wr=[Tjunk, Tss[b]])
            P.op("act", lambda e, b=b: e.activation(out=rstd[:, b:b + 1], in_=ss[:, b:b + 1], func=AF.Sqrt,
                                                    scale=1.0 / D, bias=EPS), rd=[Tss[b]], wr=[Trstd[b]])
            P.op("dve", lambda e, b=b: e.reciprocal(out=rstd[:, b:b + 1], in_=rstd[:, b:b + 1]),
                 rd=[Trstd[b]], wr=[Trstd[b]])
            P.op("dve", lambda e, b=b: e.tensor_scalar(out=xn[:], in0=xacc[:, b, :], scalar1=rstd[:, b:b + 1],
                                                       scalar2=None, op0=ALU.mult),
                 rd=[Txacc[b], Trstd[b]], wr=[Txn])
            for hh in range(2):
                bk, Tbk = (bA[b % 2] if hh == 0 else bB[b % 2])
                for cc in range(4):
                    c = hh * 4 + cc
                    P.op("pe", lambda e, c=c, cc=cc, bk=bk: e.transpose(
                        out=bk[:, cc * 128:(cc + 1) * 128], in_=xn[:, c * 128:(c + 1) * 128],
                        identity=C.ident[:]), rd=[Txn, C.Tident], wr=[Tbk])
                if not moe:
                    P.op("dve", lambda e, hh=hh, bk=bk, b=b: e.tensor_tensor(
                        out=hT[:, hh * 4:(hh + 1) * 4, b * 128:(b + 1) * 128],
                        in0=bk[:].rearrange("p (c t) -> p c t", c=4),
                        in1=gexp[:, hh * 4:(hh + 1) * 4, :], op=ALU.mult),
                         rd=[Tbk, Tgexp], wr=[ThT[b]])
                else:
                    P.op("dve", lambda e, hh=hh, bk=bk, b=b: e.tensor_tensor(
                        out=h32[:, hh * 4:(hh + 1) * 4, :],
                        in0=bk[:].rearrange("p (c t) -> p c t", c=4),
                        in1=gexp[:, hh * 4:(hh + 1) * 4, :], op=ALU.mult),
                         rd=[Tbk, Tgexp], wr=[Th32])
            if moe:
                P.op("pool", lambda e, b=b: e.tensor_copy(out=hT[:, :, b * 128:(b + 1) * 128], in_=h32[:]),
                     rd=[Th32], wr=[ThT[b]])
                P.op("pool", lambda e, b=b: e.tensor_tensor(out=hlo[:], in0=h32[:],
                                                            in1=hT[:, :, b * 128:(b + 1) * 128], op=ALU.subtract),
                     rd=[Th32, ThT[b]], wr=[Thlo])
                if 'nogate' in DBG:
                    P.op("pool", lambda e, b=b: e.memset(gates[:, b, :], 0.25), wr=[Tgates[b]])
                    continue
                rb, Trb = bR
                k3 = 0
                for (lh, Tl, rh, Tr) in ((hT[:, :, b * 128:(b + 1) * 128], ThT[b], rwh, Trwh),
                                         (hlo[:], Thlo, rwh, Trwh),
                                         (hT[:, :, b * 128:(b + 1) * 128], ThT[b], rwl, Trwl)):
                    for c in range(8):
                        P.op("pe", lambda e, c=c, lh=lh, rh=rh, k3=k3: e.matmul(
                            rb[:, 0:nexp], lhsT=lh[:, c, :], rhs=rh[:, c, :],
                            start=(k3 == 0), stop=(k3 == 23)), rd=[Tl, Tr], wr=[Trb])
                        k3 += 1
                P.op("act", lambda e: e.activation(out=gs["lg"][:], in_=rb[:, 0:nexp], func=AF.Copy),
                     rd=[Trb], wr=[Tgs["lg"]])
                P.op("dve", lambda e: e.max(out=gs["m8"][:], in_=gs["lg"][:]), rd=[Tgs["lg"]], wr=[Tgs["m8"]])
                P.op("dve", lambda e: e.tensor_scalar(out=gs1["nm"][:], in0=gs["m8"][:, 0:1], scalar1=-1.0,
                                                      scalar2=None, op0=ALU.mult),
                     rd=[Tgs["m8"]], wr=[Tgs1["nm"]])
                P.op("act", lambda e: e.activation(out=gs["ex"][:], in_=gs["lg"][:], func=AF.Exp,
                                                   bias=gs1["nm"][:, 0:1]),
                     rd=[Tgs["lg"], Tgs1["nm"]], wr=[Tgs["ex"]])
                P.op("dve", lambda e: e.tensor_scalar(out=gs["mk"][:], in0=gs["lg"][:], scalar1=gs["m8"][:, 1:2],
                                                      scalar2=None, op0=ALU.is_ge),
                     rd=[Tgs["lg"], Tgs["m8"]], wr=[Tgs["mk"]])
                P.op("dve", lambda e: e.tensor_tensor(out=gs["ge"][:], in0=gs["ex"][:], in1=gs["mk"][:],
                                                      op=ALU.mult),
                     rd=[Tgs["ex"], Tgs["mk"]], wr=[Tgs["ge"]])
                P.op("dve", lambda e: e.reduce_sum(out=gs1["den"][:], in_=gs["ge"][:], axis=AX.X),
                     rd=[Tgs["ge"]], wr=[Tgs1["den"]])
                P.op("dve", lambda e: e.reciprocal(out=gs1["rden"][:], in_=gs1["den"][:]),
                     rd=[Tgs1["den"]], wr=[Tgs1["rden"]])
                P.op("dve", lambda e, b=b: e.tensor_scalar(out=gates[:, b, :], in0=gs["ge"][:],
                                                           scalar1=gs1["rden"][:, 0:1], scalar2=None,
                                                           op0=ALU.mult),
                     rd=[Tgs["ge"], Tgs1["rden"]], wr=[Tgates[b]])
        for wi, (e_, gi) in enumerate(work if 'noexp' not in DBG else []):
            par = wi % 2
            f0, gsz = groups[gi]
            if wi + 1 < len(work):
                en, gn = work[wi + 1]
                load_w("w1", 1 - par, en, *groups[gn])
                load_w("w3", 1 - par, en, *groups[gn])
                load_w("w2", 1 - par, en, *groups[gn])
            w1g, Tw1 = wbuf[("w1", par)]
            w3g, Tw3 = wbuf[("w3", par)]
            w2g, Tw2 = wbuf[("w2", par)]
            k = 0
            for fl in range(gsz):
                for t in range(4):
                    a, Ta = bA[k % 2]
                    bb, Tb = bB[k % 2]
                    s_, Ts_ = sa[k % 2], Tsa[k % 2]
                    k += 1
                    for c in range(8):
                        P.op("pe", lambda e, c=c, a=a, fl=fl, t=t, w1g=w1g: e.matmul(
                            a[:], lhsT=w1g[:, c, fl * 128:(fl + 1) * 128], rhs=hT[:, c, t * 512:(t + 1) * 512],
                            start=(c == 0), stop=(c == 7)), rd=[Tw1[c]] + ThT[t * 4:(t + 1) * 4], wr=[Ta])
                    for c in range(8):
                        P.op("pe", lambda e, c=c, bb=bb, fl=fl, t=t, w3g=w3g: e.matmul(
                            bb[:], lhsT=w3g[:, c, fl * 128:(fl + 1) * 128], rhs=hT[:, c, t * 512:(t + 1) * 512],
                            start=(c == 0), stop=(c == 7)), rd=[Tw3[c]] + ThT[t * 4:(t + 1) * 4], wr=[Tb])
                    P.op("act", lambda e, a=a, s_=s_: e.activation(out=s_[:], in_=a[:], func=AF.Sigmoid),
                         rd=[Ta], wr=[Ts_])
                    P.op("dve", lambda e, a=a, s_=s_: e.tensor_tensor(out=s_[:], in0=s_[:], in1=a[:], op=ALU.mult),
                         rd=[Ts_, Ta], wr=[Ts_])
                    P.op("dve", lambda e, s_=s_, bb=bb, fl=fl, t=t: e.tensor_tensor(
                        out=hid[:, fl, t * 512:(t + 1) * 512], in0=s_[:], in1=bb[:], op=ALU.mult),
                         rd=[Ts_, Tb], wr=[Thid[fl][t]])
            k = 0
            for b in range(NB):
                for half in range(2):
                    o, To = bO[k % 4]
                    k += 1
                    for fl in range(gsz):
                        P.op("pe", lambda e, o=o, fl=fl, b=b, half=half, w2g=w2g, gsz=gsz: e.matmul(
                            o[:], lhsT=hid[:, fl, b * 128:(b + 1) * 128],
                            rhs=w2g[:, fl, half * 512:(half + 1) * 512],
                            start=(fl == 0), stop=(fl == gsz - 1)),
                             rd=[Thid[fl][b // 4], Tw2[fl]], wr=[To])
                    if moe:
                        sc = gates[:, b, e_:e_ + 1]
                        rds = [To, Txacc[b], Tgates[b]]
                    else:
                        sc = 1.0
                        rds = [To, Txacc[b]]
                    P.op("dve", lambda e, o=o, b=b, half=half, sc=sc: e.scalar_tensor_tensor(
                        out=xacc[:, b, half * 512:(half + 1) * 512], in0=o[:], scalar=sc,
                        in1=xacc[:, b, half * 512:(half + 1) * 512], op0=ALU.mult, op1=ALU.add),
                         rd=rds, wr=[Txacc[b]])
        for b in range(NB):
            if g_final is not None:
                P.op("act", lambda e, b=b: e.activation(out=junk[:], in_=xacc[:, b, :], func=AF.Square,
                                                        accum_out=ss[:, b:b + 1]),
                     rd=[Txacc[b]], wr=[Tjunk, Tss[b]])
                P.op("act", lambda e, b=b: e.activation(out=rstd[:, b:b + 1], in_=ss[:, b:b + 1], func=AF.Sqrt,
                                                        scale=1.0 / D, bias=EPS), rd=[Tss[b]], wr=[Trstd[b]])
                P.op("dve", lambda e, b=b: e.reciprocal(out=rstd[:, b:b + 1], in_=rstd[:, b:b + 1]),
                     rd=[Trstd[b]], wr=[Trstd[b]])
                P.op("dve", lambda e, b=b: e.scalar_tensor_tensor(
                    out=xacc[:, b, :], in0=xacc[:, b, :], scalar=rstd[:, b:b + 1], in1=gfin[:],
                    op0=ALU.mult, op1=ALU.mult), rd=[Txacc[b], Trstd[b], Tgfin], wr=[Txacc[b]])
            P.dma("act", x_out_ap[tok0 + b * 128: tok0 + (b + 1) * 128, :], xacc[:, b, :],
                  rd=[Txacc[b]], wr=[Tx_out], store=True)


def load_weight_bf16(P, name, src, K, N, q="sp", stg=None):
    kc = max(1, K // 128)
    kp = min(K, 128)
    wb = P.sb(name, [kp, kc, N], BF16)
    Tw = T(name)
    if stg is None:
        stg = [P.sb(name + "_s%d" % i, [128, 1024], F32) for i in range(3)]
        Ts = [T(name + "_s%d" % i) for i in range(3)]
    else:
        stg, Ts = stg
    ns_ = len(stg)
    i = 0
    for c in range(kc):
        for n0 in range(0, N, 1024):
            n1 = min(N, n0 + 1024)
            s, Tst = stg[i % ns_], Ts[i % ns_]
            i += 1
            P.dma(q, s[0:kp, 0:n1 - n0], src[c * 128:c * 128 + kp, n0:n1], wr=[Tst])
            ce = ("pool", "dve", "act")[i % 3]
            if ce == "act":
                P.op("act", lambda e, s=s, c=c, n0=n0, n1=n1: e.activation(out=wb[:, c, n0:n1], in_=s[0:kp, 0:n1 - n0],
                                                                         func=AF.Copy), rd=[Tst], wr=[Tw])
            else:
                P.op(ce, lambda e, s=s, c=c, n0=n0, n1=n1: e.tensor_copy(out=wb[:, c, n0:n1], in_=s[0:kp, 0:n1 - n0]),
                     rd=[Tst], wr=[Tw])
    return wb, Tw


class Banks:
    def __init__(self, C):
        self.C = C
        self.i = 0

    def next(self):
        b = self.C.bank[self.i % 8], self.C.Tbank[self.i % 8]
        self.i += 1
        return b


def norm_T(P, C, BK, name, x_ap, Tx, gexp, Tgexp, scr):
    ss, Tss, rstd, Trstd, junk, Tjunk, xn, Txn, hT, ThT = scr
    P.op("act", lambda e: e.activation(out=junk[:], in_=x_ap, func=AF.Square, accum_out=ss[:, 0:1]),
         rd=[Tx], wr=[Tjunk, Tss])
    P.op("act", lambda e: e.activation(out=rstd[:, 0:1], in_=ss[:, 0:1], func=AF.Sqrt, scale=1.0 / D, bias=EPS),
         rd=[Tss], wr=[Trstd])
    P.op("dve", lambda e: e.reciprocal(out=rstd[:, 0:1], in_=rstd[:, 0:1]), rd=[Trstd], wr=[Trstd])
    P.op("dve", lambda e: e.tensor_scalar(out=xn[:], in0=x_ap, scalar1=rstd[:, 0:1], scalar2=None, op0=ALU.mult),
         rd=[Tx, Trstd], wr=[Txn])
    for hh in range(2):
        bk, Tbk = BK.next()
        for cc in range(4):
            c = hh * 4 + cc
            P.op("pe", lambda e, c=c, cc=cc, bk=bk: e.transpose(out=bk[:, cc * 128:(cc + 1) * 128],
                                                                in_=xn[:, c * 128:(c + 1) * 128],
                                                                identity=C.ident[:]),
                 rd=[Txn, C.Tident], wr=[Tbk])
        P.op("dve", lambda e, hh=hh, bk=bk: e.tensor_tensor(out=hT[:, hh * 4:(hh + 1) * 4, :],
                                                            in0=bk[:].rearrange("p (c t) -> p c t", c=4),
                                                            in1=gexp[:, hh * 4:(hh + 1) * 4, :], op=ALU.mult),
             rd=[Tbk, Tgexp], wr=[ThT])


def mk_norm_scr(P, name):
    ss = P.sb(name + "_ss", [128, 1], F32)
    rstd = P.sb(name + "_rstd", [128, 1], F32)
    junk = P.sb(name + "_junk", [128, D], BF16)
    xn = P.sb(name + "_xn", [128, D], F32)
    hT = P.sb(name + "_hT", [128, 8, 128], BF16)
    return (ss, T(name + "ss"), rstd, T(name + "rstd"), junk, T(name + "junk"), xn, T(name + "xn"), hT, T(name + "hT"))


def transpose_to(P, C, BK, src, Tsrc, ncol, dst, Tdst, npart=128):
    nch = ncol // 128
    for c0 in range(0, nch, 4):
        bk, Tbk = BK.next()
        n = min(4, nch - c0)
        for cc in range(n):
            c = c0 + cc
            P.op("pe", lambda e, c=c, cc=cc, bk=bk: e.transpose(out=bk[:, cc * npart:(cc + 1) * npart],
                                                                in_=src[0:npart, c * 128:(c + 1) * 128],
                                                                identity=C.ident[0:npart, 0:npart]),
                 rd=[Tsrc, C.Tident], wr=[Tbk])
        P.op("act", lambda e, c0=c0, n=n, bk=bk: e.activation(
            out=dst[:, c0:c0 + n, :], in_=bk[:, 0:n * npart].rearrange("p (c t) -> p c t", c=n), func=AF.Copy),
             rd=[Tbk], wr=[Tdst])


def gelu_tanh(P, name, src, Tsrc, dst, Tdst, tmp, Ttmp):
    P.op("act", lambda e: e.activation(out=tmp[:], in_=src, func=AF.Square), rd=[Tsrc], wr=[Ttmp])
    P.op("dve", lambda e: e.tensor_scalar(out=tmp[:], in0=tmp[:], scalar1=0.044715, scalar2=1.0, op0=ALU.mult,
                                          op1=ALU.add), rd=[Ttmp], wr=[Ttmp])
    P.op("dve", lambda e: e.tensor_tensor(out=tmp[:], in0=tmp[:], in1=src, op=ALU.mult), rd=[Ttmp, Tsrc], wr=[Ttmp])
    P.op("act", lambda e: e.activation(out=tmp[:], in_=tmp[:], func=AF.Sigmoid, scale=1.5957691216057308),
         rd=[Ttmp], wr=[Ttmp])
    P.op("dve", lambda e: e.tensor_tensor(out=dst, in0=tmp[:], in1=src, op=ALU.mult), rd=[Ttmp, Tsrc], wr=[Tdst])


def run_pipeline(gens):
    gens = list(gens)
    idx = 1
    old, new, new_mid = None, gens[0], False
    while old is not None or new is not None:
        if old is not None:
            try:
                next(old)
            except StopIteration:
                old = None
        if new is not None and not new_mid:
            if next(new) == "MID":
                new_mid = True
        if old is None and (new_mid or new is None):
            old = new
            new = gens[idx] if idx < len(gens) else None
            idx += 1
            new_mid = False


class HalfBanks:
    def __init__(self, C, base):
        self.C, self.base, self.i = C, base, 0

    def next(self):
        k = self.base + (self.i % 4)
        self.i += 1
        return self.C.bank[k], self.C.Tbank[k]


def l0_mixer_stage(P, C, x_in, x_out, ntok, W):
    x_in_ap, Tx_in = x_in
    x_out_ap, Tx_out = x_out
    BK0 = Banks(C)
    nblk = ntok // 128
    stg = ([P.sb("l0m_stg%d" % i, [128, 1024], F32) for i in range(3)], [T("l0m_stg%d" % i) for i in range(3)])
    w_in, Tw_in = load_weight_bf16(P, "l0m_w_in", W["w_in"], D, 2576, stg=stg)
    w_o, Tw_o = load_weight_bf16(P, "l0m_w_o", W["w_o"], D, D, stg=stg)
    gup, Tgup = load_weight_bf16(P, "l0m_gup", W["gate_up"], 16, 256, stg=stg)
    gexp, Tgexp = load_gexp(P, C, "l0m_gn", W["norm_mix"])

    def sbt(name, shape, dt=F32):
        return P.sb("l0m_" + name, shape, dt), T("l0m_" + name)

    mask, Tmask = sbt("mask", [128, 128])
    P.op("pool", lambda e: e.memset(mask[:], 1.0), wr=[Tmask])
    P.op("pool", lambda e: e.affine_select(out=mask[:], in_=mask[:], pattern=[[1, 128]], compare_op=ALU.is_ge,
                                           fill=0.0, base=0, channel_multiplier=-1), rd=[Tmask], wr=[Tmask])
    wsT, TwsT = sbt("wsT", [128, 4, 128], BF16)
    ws32, Tws32 = sbt("ws32", [128, 4 * 128])
    P.dma("sp", ws32[:].rearrange("t (g s) -> t g s", g=4), W["w_s"].rearrange("g t s -> t g s"), wr=[Tws32])
    wsT32, TwsT32 = sbt("wsT32", [128, 4, 128])
    transpose_to(P, C, BK0, ws32, Tws32, 512, wsT32, TwsT32)
    P.op("dve", lambda e: e.tensor_tensor(out=wsT[:], in0=wsT32[:],
                                          in1=mask[:].unsqueeze(1).to_broadcast([128, 4, 128]), op=ALU.mult),
         rd=[TwsT32, Tmask], wr=[TwsT])
    bs, Tbs = sbt("bs", [128, 4])
    P.dma("sp", bs[:], W["b_s"].rearrange("g t -> t g"), wr=[Tbs], allow_slow_non_contiguous=True)
    lng, Tlng = sbt("lng", [128, 512])
    lnb, Tlnb = sbt("lnb", [128, 512])
    hdg, Thdg = sbt("hdg", [128, 512])
    P.dma("sp", lng[:], W["ln_g"].partition_broadcast(128), wr=[Tlng])
    P.dma("sp", lnb[:], W["ln_b"].partition_broadcast(128), wr=[Tlnb])
    P.dma("sp", hdg[:], W["head_g"].rearrange("h v -> (h v)").partition_broadcast(128), wr=[Thdg])
    ngb, Tngb = sbt("ngb", [64, 4])
    P.dma("sp", ngb[:], W["gate_bias"].rearrange("(h k) -> k h", k=64), wr=[Tngb], allow_slow_non_contiguous=True)
    P.op("dve", lambda e: e.tensor_scalar(out=ngb[:], in0=ngb[:], scalar1=-1.0, scalar2=None, op0=ALU.mult),
         rd=[Tngb], wr=[Tngb])
    S, TS = sbt("S", [64, 4, 128])
    Sb, TSb = sbt("Sb", [64, 4, 128], BF16)
    P.op("pool", lambda e: e.memset(S[:], 0.0), wr=[TS])
    P.op("pool", lambda e: e.memset(Sb[:], 0.0), wr=[TSb])
    junk, Tjunk = sbt("junk", [128, D], BF16)

    def mkset(i):
        def st(name, shape, dt=F32):
            return sbt("%s_%d" % (name, i), shape, dt)
        d = {}
        d["xb"] = st("xb", [128, D])
        d["ss"] = st("ss", [128, 1])
        d["rstd"] = st("rstd", [128, 1])
        d["xn"] = st("xn", [128, D])
        d["hT"] = st("hT", [128, 8, 128], BF16)
        for nm in ("ug", "vg", "tmp", "on", "sg"):
            d[nm] = st(nm, [128, 512])
        d["vnb"] = st("vnb", [128, 512], BF16)
        d["vbb"] = st("vbb", [128, 512], BF16)
        d["st6"] = st("st6", [128, 6])
        d["mv"] = st("mv", [128, 2])
        d["lrs"] = st("lrs", [128, 1])
        d["mix"] = st("mix", [128, D])
        d["mixT"] = st("mixT", [128, 8, 128], BF16)
        d["glr"] = st("glr", [16, 128], BF16)
        d["el"] = st("el", [64, 512])
        d["cs"] = st("cs", [64, 4, 128])
        d["ncl"] = st("ncl", [64, 4])
        d["egl"] = st("egl", [64, 4])
        d["e1"] = st("e1", [64, 512])
        d["qt"] = st("qt", [64, 4, 128], BF16)
        d["kt"] = st("kt", [64, 4, 128], BF16)
        d["kd32"] = st("kd32", [64, 512])
        d["kdec"] = st("kdec", [128, 4, 64], BF16)
        d["attT"] = st("attT", [128, 4, 128], BF16)
        d["ssq"] = st("ssq", [128, 4])
        d["xo"] = st("xo", [128, D])
        d["BK"] = HalfBanks(C, 4 * i)
        return d

    sets = [mkset(0), mkset(1)]

    def block(b):
        d = sets[b % 2]
        BK = d["BK"]
        (x, Txb), (ug, Tug), (vg, Tvg), (tmp, Ttmp), (on, Ton), (sg, Tsg) = [d[k] for k in ("xb", "ug", "vg", "tmp", "on", "sg")]
        (vnb, Tvnb), (vbb, Tvbb), (st6, Tst6), (mv, Tmv), (lrs, Tlrs) = [d[k] for k in ("vnb", "vbb", "st6", "mv", "lrs")]
        (mix, Tmix), (mixT, TmixT), (glr, Tglr), (el, Tel), (cs, Tcs) = [d[k] for k in ("mix", "mixT", "glr", "el", "cs")]
        (ncl, Tncl), (egl, Tegl), (e1, Te1), (qt, Tqt), (kt, Tkt) = [d[k] for k in ("ncl", "egl", "e1", "qt", "kt")]
        (kd32, Tkd32), (kdec, Tkdec), (attT, TattT), (ssq, Tssq), (xo_, Txo) = [d[k] for k in ("kd32", "kdec", "attT", "ssq", "xo")]
        hT, ThT = d["hT"]
        scr = (d["ss"][0], d["ss"][1], d["rstd"][0], d["rstd"][1], junk, Tjunk, d["xn"][0], d["xn"][1], hT, ThT)

        def proj_tok(col0, ncols=512):
            bk, Tbk = BK.next()
            for c in range(8):
                P.op("pe", lambda e, c=c, bk=bk: e.matmul(bk[:, 0:ncols], lhsT=hT[:, c, :],
                                                          rhs=w_in[:, c, col0:col0 + ncols],
                                                          start=(c == 0), stop=(c == 7)), rd=[ThT, Tw_in], wr=[Tbk])
            return bk, Tbk

        def proj_feat(col0, m, nh):
            bk, Tbk = BK.next()
            for h in range(nh):
                for c in range(8):
                    P.op("pe", lambda e, c=c, h=h, bk=bk: e.matmul(
                        bk[0:m, h * 128:(h + 1) * 128], lhsT=w_in[:, c, col0 + h * m: col0 + (h + 1) * m],
                        rhs=hT[:, c, :], start=(c == 0), stop=(c == 7)), rd=[ThT, Tw_in], wr=[Tbk])
            return bk, Tbk

        P.dma("act", x[:], x_in_ap[b * 128:(b + 1) * 128, :], rd=[Tx_in], wr=[Txb])
        norm_T(P, C, BK, "l0m", x[:], Txb, gexp, Tgexp, scr)
        yield
        pu, Tpu = proj_tok(0)
        yield
        gelu_tanh(P, "u", pu[:], Tpu, ug[:], Tug, tmp, Ttmp)
        yield
        pv, Tpv = proj_tok(512)
        yield
        gelu_tanh(P, "v", pv[:], Tpv, vg[:], Tvg, tmp, Ttmp)
        yield
        P.op("dve", lambda e: e.bn_stats(out=st6[:], in_=vg[:]), rd=[Tvg], wr=[Tst6])
        P.op("dve", lambda e: e.bn_aggr(out=mv[:], in_=st6[:]), rd=[Tst6], wr=[Tmv])
        P.op("act", lambda e: e.activation(out=lrs[:], in_=mv[:, 1:2], func=AF.Sqrt, bias=EPS), rd=[Tmv], wr=[Tlrs])
        P.op("dve", lambda e: e.reciprocal(out=lrs[:], in_=lrs[:]), rd=[Tlrs], wr=[Tlrs])
        yield
        P.op("dve", lambda e: e.tensor_scalar(out=vg[:], in0=vg[:], scalar1=mv[:, 0:1], scalar2=lrs[:, 0:1],
                                              op0=ALU.subtract, op1=ALU.mult), rd=[Tvg, Tmv, Tlrs], wr=[Tvg])
        P.op("dve", lambda e: e.tensor_tensor(out=vg[:], in0=vg[:], in1=lng[:], op=ALU.mult), rd=[Tvg, Tlng], wr=[Tvg])
        P.op("dve", lambda e: e.tensor_tensor(out=vnb[:], in0=vg[:], in1=lnb[:], op=ALU.add), rd=[Tvg, Tlnb], wr=[Tvnb])
        yield
        pvb, Tpvb = proj_tok(1552)
        P.op("act", lambda e, pvb=pvb: e.activation(out=vbb[:], in_=pvb[:], func=AF.Copy), rd=[Tpvb], wr=[Tvbb])
        yield
        pm, Tpm = BK.next()
        for g in range(4):
            P.op("pe", lambda e, g=g, pm=pm: e.matmul(pm[:, g * 128:(g + 1) * 128], lhsT=wsT[:, g, :],
                                                      rhs=vnb[:, g * 128:(g + 1) * 128], start=True, stop=True),
                 rd=[TwsT, Tvnb], wr=[Tpm])
        for g in range(4):
            P.op("dve", lambda e, g=g, pm=pm: e.scalar_tensor_tensor(
                out=mix[:, g * 128:(g + 1) * 128], in0=pm[:, g * 128:(g + 1) * 128], scalar=bs[:, g:g + 1],
                in1=ug[:, g * 128:(g + 1) * 128], op0=ALU.add, op1=ALU.mult), rd=[Tpm, Tbs, Tug], wr=[Tmix])
        yield
        pg, Tpg = proj_feat(1536, 16, 1)
        P.op("act", lambda e, pg=pg: e.activation(out=glr[:], in_=pg[0:16, 0:128], func=AF.Copy), rd=[Tpg], wr=[Tglr])
        yield
        pgt, Tpgt = BK.next()
        for h in range(4):
            P.op("pe", lambda e, h=h, pgt=pgt: e.matmul(pgt[0:64, h * 128:(h + 1) * 128],
                                                        lhsT=gup[:, 0, h * 64:(h + 1) * 64], rhs=glr[:],
                                                        start=True, stop=True), rd=[Tgup, Tglr], wr=[Tpgt])
        for h in range(4):
            P.op("act", lambda e, h=h, pgt=pgt: e.activation(out=el[:, h * 128:(h + 1) * 128],
                                                             in_=pgt[0:64, h * 128:(h + 1) * 128], func=AF.Exp,
                                                             scale=-1.0, bias=ngb[:, h:h + 1]),
                 rd=[Tpgt, Tngb], wr=[Tel])
        P.op("act", lambda e: e.activation(out=el[:], in_=el[:], func=AF.Ln, bias=1.0), rd=[Tel], wr=[Tel])
        yield
        for h in range(4):
            P.op("dve", lambda e, h=h: e.tensor_tensor_scan(out=cs[:, h, :], data0=C.ones[0:64, :],
                                                            data1=el[:, h * 128:(h + 1) * 128], initial=0.0,
                                                            op0=ALU.mult, op1=ALU.add),
                 rd=[Tel, C.Tones], wr=[Tcs])
        P.op("dve", lambda e: e.tensor_scalar(out=ncl[:], in0=cs[:, :, 127], scalar1=-1.0 / 16, scalar2=None,
                                              op0=ALU.mult), rd=[Tcs], wr=[Tncl])
        P.op("act", lambda e: e.activation(out=egl[:], in_=ncl[:], func=AF.Exp), rd=[Tncl], wr=[Tegl])
        yield
        pq, Tpq = proj_feat(1024, 64, 4)
        yield
        csf = cs[:].rearrange("p h t -> p (h t)")
        P.op("act", lambda e: e.activation(out=e1[:], in_=csf, func=AF.Exp, scale=-1.0 / 16), rd=[Tcs], wr=[Te1])
        P.op("dve", lambda e, pq=pq: e.scalar_tensor_tensor(out=qt[:].rearrange("p h t -> p (h t)"), in0=pq[0:64, :],
                                                            scalar=0.125, in1=e1[:], op0=ALU.mult, op1=ALU.mult),
             rd=[Tpq, Te1], wr=[Tqt])
        yield
        pk, Tpk = proj_feat(1280, 64, 4)
        yield
        P.op("act", lambda e: e.activation(out=e1[:], in_=csf, func=AF.Exp, scale=1.0 / 16), rd=[Tcs], wr=[Te1])
        P.op("dve", lambda e, pk=pk: e.tensor_tensor(out=kt[:].rearrange("p h t -> p (h t)"), in0=pk[0:64, :],
                                                     in1=e1[:], op=ALU.mult), rd=[Tpk, Te1], wr=[Tkt])
        for h in range(4):
            P.op("act", lambda e, h=h: e.activation(out=e1[:, h * 128:(h + 1) * 128], in_=cs[:, h, :], func=AF.Exp,
                                                    scale=1.0 / 16, bias=ncl[:, h:h + 1]), rd=[Tcs, Tncl], wr=[Te1])
        P.op("dve", lambda e, pk=pk: e.tensor_tensor(out=kd32[:], in0=pk[0:64, :], in1=e1[:], op=ALU.mult),
             rd=[Tpk, Te1], wr=[Tkd32])
        yield
        transpose_to(P, C, BK, kd32, Tkd32, 512, kdec, Tkdec, npart=64)
        yield "MID"
        pa, Tpa = BK.next()
        for h in range(4):
            P.op("pe", lambda e, h=h, pa=pa: e.matmul(pa[:, h * 128:(h + 1) * 128], lhsT=kt[:, h, :], rhs=qt[:, h, :],
                                                      start=True, stop=True), rd=[Tkt, Tqt], wr=[Tpa])
        P.op("dve", lambda e, pa=pa: e.tensor_tensor(out=attT[:], in0=pa[:].rearrange("p (h t) -> p h t", h=4),
                                                     in1=mask[:].unsqueeze(1).to_broadcast([128, 4, 128]),
                                                     op=ALU.mult), rd=[Tpa, Tmask], wr=[TattT])
        yield
        pS, TpS = BK.next()
        for h in range(4):
            P.op("pe", lambda e, h=h, pS=pS: e.matmul(pS[0:64, h * 128:(h + 1) * 128], lhsT=kdec[:, h, :],
                                                      rhs=vbb[:, h * 128:(h + 1) * 128], start=True, stop=True),
                 rd=[Tkdec, Tvbb], wr=[TpS])
        yield
        po, Tpo = BK.next()
        for h in range(4):
            P.op("pe", lambda e, h=h, po=po: e.matmul(po[:, h * 128:(h + 1) * 128], lhsT=attT[:, h, :],
                                                      rhs=vbb[:, h * 128:(h + 1) * 128], start=True, stop=False),
                 rd=[TattT, Tvbb], wr=[Tpo])
            P.op("pe", lambda e, h=h, po=po: e.matmul(po[:, h * 128:(h + 1) * 128], lhsT=qt[:, h, :],
                                                      rhs=Sb[:, h, :], start=False, stop=True),
                 rd=[Tqt, TSb], wr=[Tpo])
        yield
        for h in range(4):
            P.op("dve", lambda e, h=h, pS=pS: e.scalar_tensor_tensor(
                out=S[:, h, :], in0=S[:, h, :], scalar=egl[:, h:h + 1], in1=pS[0:64, h * 128:(h + 1) * 128],
                op0=ALU.mult, op1=ALU.add), rd=[TS, Tegl, TpS], wr=[TS])
        P.op("pool", lambda e: e.tensor_copy(out=Sb[:], in_=S[:]), rd=[TS], wr=[TSb])
        yield
        for h in range(4):
            P.op("act", lambda e, h=h, po=po: e.activation(out=sg[:, h * 128:(h + 1) * 128],
                                                           in_=po[:, h * 128:(h + 1) * 128], func=AF.Square,
                                                           accum_out=ssq[:, h:h + 1]), rd=[Tpo], wr=[Tsg, Tssq])
        P.op("act", lambda e: e.activation(out=ssq[:], in_=ssq[:], func=AF.Sqrt, scale=1.0 / 128, bias=EPS),
             rd=[Tssq], wr=[Tssq])
        P.op("dve", lambda e: e.reciprocal(out=ssq[:], in_=ssq[:]), rd=[Tssq], wr=[Tssq])
        yield
        P.op("dve", lambda e, po=po: e.tensor_tensor(out=on[:].rearrange("p (h v) -> p h v", h=4),
                                                     in0=po[:].rearrange("p (h v) -> p h v", h=4),
                                                     in1=ssq[:].unsqueeze(2).to_broadcast([128, 4, 128]),
                                                     op=ALU.mult), rd=[Tpo, Tssq], wr=[Ton])
        P.op("dve", lambda e: e.tensor_tensor(out=on[:], in0=on[:], in1=hdg[:], op=ALU.mult), rd=[Ton, Thdg], wr=[Ton])
        yield
        pog, Tpog = proj_tok(2064)
        yield
        P.op("act", lambda e, pog=pog: e.activation(out=sg[:], in_=pog[:], func=AF.Sigmoid), rd=[Tpog], wr=[Tsg])
        P.op("dve", lambda e, pog=pog: e.tensor_tensor(out=sg[:], in0=sg[:], in1=pog[:], op=ALU.mult),
             rd=[Tsg, Tpog], wr=[Tsg])
        P.op("dve", lambda e: e.tensor_tensor(out=mix[:, 512:1024], in0=on[:], in1=sg[:], op=ALU.mult),
             rd=[Ton, Tsg], wr=[Tmix])
        yield
        transpose_to(P, C, BK, mix, Tmix, 1024, mixT, TmixT)
        yield
        for half in range(2):
            pw, Tpw = BK.next()
            for c in range(8):
                P.op("pe", lambda e, c=c, pw=pw, half=half: e.matmul(pw[:], lhsT=mixT[:, c, :],
                                                                   rhs=w_o[:, c, half * 512:(half + 1) * 512],
                                                                   start=(c == 0), stop=(c == 7)),
                     rd=[TmixT, Tw_o], wr=[Tpw])
            P.op("dve", lambda e, pw=pw, half=half: e.tensor_tensor(
                out=xo_[:, half * 512:(half + 1) * 512], in0=pw[:], in1=x[:, half * 512:(half + 1) * 512], op=ALU.add),
                 rd=[Tpw, Txb], wr=[Txo])
            yield
        P.dma("act", x_out_ap[b * 128:(b + 1) * 128, :], xo_[:], rd=[Txo], wr=[Tx_out], store=True)

    run_pipeline(block(b) for b in range(nblk))


def head_norm(P, name, src, Tsrc, g_rep, Tg_rep, dst, Tdst, tmp, Ttmp, ss16, Tss16, scale):
    P.op("dve", lambda e: e.tensor_tensor(out=tmp[:], in0=src[:], in1=src[:], op=ALU.mult), rd=[Tsrc], wr=[Ttmp])
    P.op("dve", lambda e: e.tensor_reduce(out=ss16[:], in_=tmp[:].rearrange("p (h d) -> p h d", h=16), axis=AX.X,
                                          op=ALU.add), rd=[Ttmp], wr=[Tss16])
    P.op("act", lambda e: e.activation(out=ss16[:], in_=ss16[:], func=AF.Sqrt, scale=1.0 / 64, bias=EPS),
         rd=[Tss16], wr=[Tss16])
    P.op("dve", lambda e: e.reciprocal(out=ss16[:], in_=ss16[:]), rd=[Tss16], wr=[Tss16])
    P.op("dve", lambda e: e.tensor_tensor(out=tmp[:].rearrange("p (h d) -> p h d", h=16),
                                          in0=src[:].rearrange("p (h d) -> p h d", h=16),
                                          in1=ss16[:].unsqueeze(2).to_broadcast([128, 16, 64]), op=ALU.mult),
         rd=[Tsrc, Tss16], wr=[Ttmp])
    P.op("dve", lambda e: e.scalar_tensor_tensor(out=dst[:].rearrange("p (h d) -> p h d", h=16),
                                                 in0=tmp[:].rearrange("p (h d) -> p h d", h=16), scalar=scale,
                                                 in1=g_rep[:].unsqueeze(1).to_broadcast([128, 16, 64]),
                                                 op0=ALU.mult, op1=ALU.mult), rd=[Ttmp, Tg_rep], wr=[Tdst])


def split3(P, name, src, Tsrc, outs, Touts, r32, Tr32):
    P.op("dve", lambda e: e.tensor_copy(out=outs[0][:], in_=src), rd=[Tsrc], wr=[Touts[0]])
    P.op("dve", lambda e: e.tensor_tensor(out=r32[:], in0=src, in1=outs[0][:], op=ALU.subtract),
         rd=[Tsrc, Touts[0]], wr=[Tr32])
    P.op("dve", lambda e: e.tensor_copy(out=outs[1][:], in_=r32[:]), rd=[Tr32], wr=[Touts[1]])
    P.op("dve", lambda e: e.tensor_tensor(out=outs[2][:], in0=r32[:], in1=outs[1][:], op=ALU.subtract),
         rd=[Tr32, Touts[1]], wr=[Touts[2]])


def l1_proj_stage(P, C, x_in, W, sel_ap, scratch):
    x_in_ap, Tx_in = x_in
    KTd, VAd, NCd, CRd, QTd, SGd, XOd = [scratch[k] for k in ("KT", "VA", "NC", "CR", "QT", "SG", "XO")]
    BK = Banks(C)
    w_in, Tw_in = load_weight_bf16(P, "l1p_w_in", W["w_in"], D, 4112)
    gexp, Tgexp = load_gexp(P, C, "l1p_gn", W["norm_mix"])

    def sbt(name, shape, dt=F32):
        return P.sb("l1p_" + name, shape, dt), T("l1p_" + name)

    maskb, Tmaskb = sbt("maskb", [128, 128], BF16)
    P.op("pool", lambda e: e.memset(maskb[:], 1.0), wr=[Tmaskb])
    P.op("pool", lambda e: e.affine_select(out=maskb[:], in_=maskb[:], pattern=[[1, 128]], compare_op=ALU.is_ge,
                                           fill=0.0, base=0, channel_multiplier=-1), rd=[Tmaskb], wr=[Tmaskb])
    onesb, Tonesb = sbt("onesb", [128, 128], BF16)
    P.op("pool", lambda e: e.memset(onesb[:], 1.0), wr=[Tonesb])
    e127, Te127 = sbt("e127", [128, 128], BF16)
    P.op("pool", lambda e: e.memset(e127[:], 1.0), wr=[Te127])
    P.op("pool", lambda e: e.affine_select(out=e127[:], in_=e127[:], pattern=[[0, 128]], compare_op=ALU.is_equal,
                                           fill=0.0, base=-64, channel_multiplier=1), rd=[Te127], wr=[Te127])
    sel, Tsel = sbt("sel", [128, 2])
    P.dma("sp", sel[:], sel_ap, wr=[Tsel])
    kg, Tkg = sbt("kg", [128, 64])
    qg, Tqg = sbt("qg", [128, 64])
    fbr, Tfbr = sbt("fbr", [128, 16])
    P.dma("sp", kg[:], W["k_g"].partition_broadcast(128), wr=[Tkg])
    P.dma("sp", qg[:], W["q_g"].partition_broadcast(128), wr=[Tqg])
    P.dma("sp", fbr[:], W["forget_bias"].partition_broadcast(128), wr=[Tfbr])
    A, TA = sbt("A", [128, 16])
    P.op("pool", lambda e: e.memset(A[:], 0.0), wr=[TA])
    xb = [sbt("xb%d" % i, [128, D]) for i in range(4)]
    cum = [sbt("cum%d" % i, [128, 16]) for i in range(4)]
    xown, Txown = sbt("xown", [128, D])
    cown, Tcown = sbt("cown", [128, 16])
    junk, Tjunk = sbt("junk", [128, D], BF16)
    KTb = [sbt("KTb%d" % i, [128, 8, 128], BF16) for i in range(2)]
    VAb = [sbt("VAb%d" % i, [128, 16, 65], BF16) for i in range(2)]
    for i in range(2):
        P.op("pool", lambda e, i=i: e.memset(VAb[i][0][:], 1.0), wr=[VAb[i][1]])
    SGb, TSGb = sbt("SGb", [128, D], BF16)
    l32, Tl32 = sbt("l32", [128, 16])
    r32, Tr32 = sbt("r32", [128, 16])
    ls = [sbt("ls%d" % i, [128, 16], BF16) for i in range(3)]
    As = [sbt("As%d" % i, [128, 16], BF16) for i in range(3)]
    cs3 = [sbt("cs3%d" % i, [128, 16], BF16) for i in range(3)]
    crb, Tcrb = sbt("crb", [128, 16])
    TKT, TVA, TNC, TCR, TQT, TSG, TXO = [scratch["T" + k] for k in ("KT", "VA", "NC", "CR", "QT", "SG", "XO")]

    def mkset(i):
        d = {}
        d["ss"] = sbt("ss_%d" % i, [128, 1])
        d["rstd"] = sbt("rstd_%d" % i, [128, 1])
        d["xn"] = sbt("xn_%d" % i, [128, D])
        d["hT"] = sbt("hT_%d" % i, [128, 8, 128], BF16)
        d["kf"] = sbt("kf_%d" % i, [128, D])
        d["tmp"] = sbt("tmp_%d" % i, [128, D])
        d["kn"] = sbt("kn_%d" % i, [128, D])
        d["ss16"] = sbt("ss16_%d" % i, [128, 16])
        d["BK"] = HalfBanks(C, 4 * i)
        return d

    sets = [mkset(0), mkset(1)]

    def block(b):
        d = sets[b % 2]
        BK = d["BK"]
        hT, ThT = d["hT"]
        scr = (d["ss"][0], d["ss"][1], d["rstd"][0], d["rstd"][1], junk, Tjunk, d["xn"][0], d["xn"][1], hT, ThT)
        (kf, Tkf), (tmp, Ttmp), (kn, Tkn), (ss16, Tss16) = [d[k] for k in ("kf", "tmp", "kn", "ss16")]

        def proj(col0, ncols=512):
            bk, Tbk = BK.next()
            for c in range(8):
                P.op("pe", lambda e, c=c, bk=bk: e.matmul(bk[:, 0:ncols], lhsT=hT[:, c, :],
                                                          rhs=w_in[:, c, col0:col0 + ncols],
                                                          start=(c == 0), stop=(c == 7)), rd=[ThT, Tw_in], wr=[Tbk])
            return bk, Tbk

        x, Txb = xb[b % 4]
        cm, Tcm = cum[b % 4]
        P.dma("act", x[:], x_in_ap[b * 128:(b + 1) * 128, :], rd=[Tx_in], wr=[Txb])
        norm_T(P, C, BK, "l1p", x[:], Txb, gexp, Tgexp, scr)
        yield
        for half in range(2):
            bk, Tbk = proj(1024 + half * 512)
            P.op("act", lambda e, bk=bk, half=half: e.activation(out=kf[:, half * 512:(half + 1) * 512], in_=bk[:],
                                                                func=AF.Copy), rd=[Tbk], wr=[Tkf])
            yield
        head_norm(P, "k", kf, Tkf, kg, Tkg, kn, Tkn, tmp, Ttmp, ss16, Tss16, 1.0)
        yield
        ktb, Tktb = KTb[b % 2]
        transpose_to(P, C, BK, kn, Tkn, 1024, ktb, Tktb)
        P.dma("sp", KTd.rearrange("h p t -> p h t")[:, :, b * 128:(b + 1) * 128], ktb[:], rd=[Tktb], wr=[TKT], store=True)
        yield
        vab, Tvab = VAb[b % 2]
        for half in range(2):
            bk, Tbk = proj(2048 + half * 512)
            P.op("act", lambda e, bk=bk, half=half, vab=vab: e.activation(
                out=vab[:, half * 8:(half + 1) * 8, 0:64], in_=bk[:].rearrange("p (h d) -> p h d", h=8),
                func=AF.Copy), rd=[Tbk], wr=[Tvab])
            yield
        P.dma("sp", VAd[b * 128:(b + 1) * 128, :], vab[:].rearrange("p h d -> p (h d)"), rd=[Tvab], wr=[TVA], store=True)
        bkf, Tbkf = proj(4096, 16)
        yield "MID"
        P.op("dve", lambda e, bkf=bkf: e.tensor_tensor(out=l32[:], in0=bkf[:, 0:16], in1=fbr[:], op=ALU.add),
             rd=[Tbkf, Tfbr], wr=[Tl32])
        P.op("act", lambda e: e.activation(out=l32[:], in_=l32[:], func=AF.Exp, scale=-1.0), rd=[Tl32], wr=[Tl32])
        P.op("act", lambda e: e.activation(out=l32[:], in_=l32[:], func=AF.Ln, bias=1.0), rd=[Tl32], wr=[Tl32])
        yield
        split3(P, "l", l32[:], Tl32, [t[0] for t in ls], [t[1] for t in ls], r32, Tr32)
        yield
        split3(P, "A", A[:], TA, [t[0] for t in As], [t[1] for t in As], r32, Tr32)
        yield
        bk, Tbk = BK.next()
        for i in range(3):
            P.op("pe", lambda e, i=i, bk=bk: e.matmul(bk[:, 0:16], lhsT=maskb[:], rhs=ls[i][0][:], start=(i == 0),
                                                      stop=False), rd=[Tmaskb, ls[i][1]], wr=[Tbk])
        for i in range(3):
            P.op("pe", lambda e, i=i, bk=bk: e.matmul(bk[:, 0:16], lhsT=onesb[:], rhs=As[i][0][:], start=False,
                                                      stop=(i == 2)), rd=[Tonesb, As[i][1]], wr=[Tbk])
        P.op("act", lambda e, bk=bk, cm=cm: e.activation(out=cm[:], in_=bk[:, 0:16], func=AF.Copy), rd=[Tbk], wr=[Tcm])
        P.op("dve", lambda e: e.tensor_tensor(out=A[:], in0=A[:], in1=l32[:], op=ALU.add), rd=[TA, Tl32], wr=[TA])
        P.dma("sp", NCd[b * 128:(b + 1) * 128, :], cm[:], rd=[Tcm], wr=[TNC], store=True)
        yield
        if b % 2 == 0:
            return
        j = b // 2
        xa, Txa = xb[(b - 1) % 4]
        xb_, Txb_ = xb[b % 4]
        ca, Tca = cum[(b - 1) % 4]
        cb, Tcb = cum[b % 4]
        P.op("dve", lambda e: e.tensor_scalar(out=xown[:], in0=xa[:], scalar1=sel[:, 0:1], scalar2=None,
                                              op0=ALU.mult), rd=[Txa, Tsel], wr=[Txown])
        P.op("dve", lambda e: e.scalar_tensor_tensor(out=xown[:], in0=xb_[:], scalar=sel[:, 1:2], in1=xown[:],
                                                     op0=ALU.mult, op1=ALU.add), rd=[Txb_, Tsel, Txown], wr=[Txown])
        P.dma("sp", XOd[j * 128:(j + 1) * 128, :], xown[:], rd=[Txown], wr=[TXO], store=True)
        yield
        P.op("dve", lambda e: e.tensor_scalar(out=cown[:], in0=ca[:], scalar1=sel[:, 0:1], scalar2=None,
                                              op0=ALU.mult), rd=[Tca, Tsel], wr=[Tcown])
        P.op("dve", lambda e: e.scalar_tensor_tensor(out=cown[:], in0=cb[:], scalar=sel[:, 1:2], in1=cown[:],
                                                     op0=ALU.mult, op1=ALU.add), rd=[Tcb, Tsel, Tcown], wr=[Tcown])
        split3(P, "c", cown[:], Tcown, [t[0] for t in cs3], [t[1] for t in cs3], r32, Tr32)
        yield
        bk, Tbk = BK.next()
        for i in range(3):
            P.op("pe", lambda e, i=i, bk=bk: e.matmul(bk[:, 0:16], lhsT=e127[:], rhs=cs3[i][0][:], start=(i == 0),
                                                      stop=(i == 2)), rd=[Te127, cs3[i][1]], wr=[Tbk])
        P.op("act", lambda e, bk=bk: e.activation(out=crb[:], in_=bk[:, 0:16], func=AF.Copy), rd=[Tbk], wr=[Tcrb])
        P.dma("sp", CRd[j], crb[:], rd=[Tcrb], wr=[TCR], store=True)
        yield
        norm_T(P, C, BK, "l1p", xown[:], Txown, gexp, Tgexp, scr)
        yield
        for half in range(2):
            bk, Tbk = proj(half * 512)
            P.op("act", lambda e, bk=bk, half=half: e.activation(out=kf[:, half * 512:(half + 1) * 512], in_=bk[:],
                                                                func=AF.Copy), rd=[Tbk], wr=[Tkf])
            yield
        head_norm(P, "q", kf, Tkf, qg, Tqg, kn, Tkn, tmp, Ttmp, ss16, Tss16, 0.125)
        yield
        qtb, Tqtb = KTb[b % 2]
        transpose_to(P, C, BK, kn, Tkn, 1024, qtb, Tqtb)
        P.dma("sp", QTd.rearrange("h p t -> p h t")[:, :, j * 128:(j + 1) * 128], qtb[:], rd=[Tqtb], wr=[TQT], store=True)
        yield
        for half in range(2):
            bk, Tbk = proj(3072 + half * 512)
            P.op("act", lambda e, bk=bk, half=half: e.activation(out=SGb[:, half * 512:(half + 1) * 512], in_=bk[:],
                                                                func=AF.Sigmoid), rd=[Tbk], wr=[TSGb])
            yield
        P.dma("sp", SGd[j * 128:(j + 1) * 128, :], SGb[:], rd=[TSGb], wr=[TSG], store=True)

    run_pipeline(block(b) for b in range(32))


def l1_attn_stage(P, C, x_out, W, msk_ap, scratch, sel_ap):
    x_out_ap, Tx_out = x_out
    KTd, VAd, NCd, CRd, QTd, SGd, XOd = [scratch[k] for k in ("KT", "VA", "NC", "CR", "QT", "SG", "XO")]
    TKT, TVA, TNC, TCR, TQT, TSG, TXO = [scratch["T" + k] for k in ("KT", "VA", "NC", "CR", "QT", "SG", "XO")]
    BK = Banks(C)
    w_o, Tw_o = load_weight_bf16(P, "l1a_w_o", W["w_o"], D, D)

    def sbt(name, shape, dt=F32):
        return P.sb("l1a_" + name, shape, dt), T("l1a_" + name)

    msk32, Tmsk32 = sbt("msk32", [128, 2, 128])
    P.dma("sp", msk32[:], msk_ap.rearrange("m s t -> s m t"), wr=[Tmsk32])
    mskb, Tmskb = sbt("mskb", [128, 2, 128], BF16)
    P.op("pool", lambda e: e.tensor_copy(out=mskb[:], in_=msk32[:]), rd=[Tmsk32], wr=[Tmskb])
    NC_, TNC_ = sbt("NC", [128, 32, 16])
    P.dma("sp", NC_[:], NCd.rearrange("(b p) h -> p b h", p=128), rd=[TNC], wr=[TNC_])
    CR_, TCR_ = sbt("CR", [128, 16, 16])
    P.dma("sp", CR_[:], CRd.rearrange("j p h -> p j h"), rd=[TCR], wr=[TCR_])
    jj = [(j, J) for j in range(16) for J in range(2 * j + 2)]
    bidx = {k: i for i, k in enumerate(jj)}
    bias, Tbias = sbt("bias", [128, len(jj), 16])
    for j in range(16):
        nJ = 2 * j + 2
        i0 = bidx[(j, 0)]
        P.op("dve", lambda e, j=j, nJ=nJ, i0=i0: e.tensor_tensor(
            out=bias[:, i0:i0 + nJ, :], in0=NC_[:, 0:nJ, :],
            in1=CR_[:, j:j + 1, :].to_broadcast([128, nJ, 16]), op=ALU.subtract), rd=[TNC_, TCR_], wr=[Tbias])
    sel, Tsel = sbt("sel", [128, 2])
    P.dma("sp", sel[:], sel_ap, wr=[Tsel])
    nbig, Tnbig = sbt("nbig", [128, 1])
    P.op("dve", lambda e: e.tensor_scalar(out=nbig[:], in0=sel[:, 0:1], scalar1=-30000.0, scalar2=None, op0=ALU.mult),
         rd=[Tsel], wr=[Tnbig])
    for j in range(16):
        bi_ = bidx[(j, 2 * j + 1)]
        P.op("dve", lambda e, bi_=bi_: e.tensor_scalar(out=bias[:, bi_, :], in0=bias[:, bi_, :], scalar1=nbig[:, 0:1],
                                                     scalar2=None, op0=ALU.add), rd=[Tbias, Tnbig], wr=[Tbias])
    nb_ = len(jj)
    bm, Tbm = sbt("bm", [128, nb_, 8])
    b4 = bias[:].rearrange("p n (hp two) -> p n hp two", two=2)
    P.op("dve", lambda e: e.tensor_tensor(out=bm[:], in0=b4[:, :, :, 0], in1=b4[:, :, :, 1], op=ALU.max),
         rd=[Tbias], wr=[Tbm])
    P.op("dve", lambda e: e.tensor_tensor(out=b4, in0=b4, in1=bm[:].unsqueeze(3).to_broadcast([128, nb_, 8, 2]),
                                          op=ALU.subtract), rd=[Tbias, Tbm], wr=[Tbias])
    P.op("act", lambda e: e.activation(out=bias[:], in_=bias[:], func=AF.Exp), rd=[Tbias], wr=[Tbias])
    vsb = [sbt("vs%d" % i, [128, 2, 65], BF16) for i in range(4)]
    QTP = [sbt("QTP%d" % i, [128, 16, 2, 128], BF16) for i in range(2)]
    for i in range(2):
        P.op("pool", lambda e, i=i: e.memset(QTP[i][0][:], 0.0), wr=[QTP[i][1]])
    oT, ToT = sbt("oT", [128, 8, 2048], BF16)
    KT = [sbt("KT%d" % i, [128, 4096], BF16) for i in range(2)]
    VA = [sbt("VA%d" % i, [128, 32, 130], BF16) for i in range(2)]
    SG = [sbt("SG%d" % i, [128, 16, 128], BF16) for i in range(2)]
    pT = [sbt("pT%d" % i, [128, 2, 128], BF16) for i in range(4)]
    rec, Trec = sbt("rec", [128, 2])
    o2s = [sbt("o2_%d" % i, [128, 128]) for i in range(2)]
    xo = [sbt("xo%d" % i, [128, D]) for i in range(2)]
    xr = [sbt("xr%d" % i, [128, D]) for i in range(2)]
    pcount = 0
    pocount = 0
    if 'a_noloop' in DBG:
        P.op("pool", lambda e: e.memset(oT[:], 0.0), wr=[ToT])
    def hp_loads(hp):
        kt, Tkt = KT[hp % 2]
        va, Tva = VA[hp % 2]
        sg, Tsg = SG[hp % 2]
        qtp, Tqtp = QTP[hp % 2]
        P.dma("sp", kt[:], KTd[hp], rd=[TKT], wr=[Tkt])
        for h2 in range(2):
            P.dma("sp", qtp[h2 * 64:(h2 + 1) * 64, :, h2, :],
                  QTd[hp][h2 * 64:(h2 + 1) * 64, :].rearrange("p (j t) -> p j t", t=128), rd=[TQT], wr=[Tqtp])
        P.dma("sp", va[:].rearrange("p b (h d) -> p b h d", h=2),
              VAd.rearrange("(b p) (h d) -> p b h d", p=128, d=65)[:, :, 2 * hp:2 * hp + 2, :], rd=[TVA], wr=[Tva])
        P.dma("sp", sg[:], SGd.rearrange("(j p) c -> p j c", p=128)[:, :, hp * 128:(hp + 1) * 128], rd=[TSG], wr=[Tsg])

    hp_loads(0)
    for hp in range(8):
        kt, Tkt = KT[hp % 2]
        va, Tva = VA[hp % 2]
        sg, Tsg = SG[hp % 2]
        qtp, Tqtp = QTP[hp % 2]
        if hp + 1 < 8:
            hp_loads(hp + 1)
        items = [(j, J) for j in range(16) for J in range(2 * j + 2)]
        LOOK = 2

        def emit_qk(i, kt=kt, Tkt=Tkt, qtp=qtp, Tqtp=Tqtp):
            j, J = items[i]
            ps, Tps = C.bank[i % 3], C.Tbank[i % 3]
            P.op("pe", lambda e, ps=ps, J=J, j=j: e.matmul(
                ps[:, 0:256], lhsT=kt[:, J * 128:(J + 1) * 128],
                rhs=qtp[:, j, :, :].rearrange("p a t -> p (a t)"), start=True, stop=True),
                 rd=[Tkt, Tqtp], wr=[Tps])

        def emit_tr(pend):
            j_, o2_, To2_ = pend
            bk, Tbk = C.bank[3], C.Tbank[3]
            P.op("pe", lambda e, bk=bk, o2_=o2_: e.transpose(out=bk[:, 0:128], in_=o2_[:], identity=C.ident[:]),
                 rd=[To2_, C.Tident], wr=[Tbk])
            P.op("act", lambda e, bk=bk, hp=hp, j_=j_: e.activation(out=oT[:, hp, j_ * 128:(j_ + 1) * 128],
                                                                    in_=bk[:, 0:128], func=AF.Copy), rd=[Tbk], wr=[ToT])

        for i in range(LOOK):
            emit_qk(i)
        pending = None
        for i, (j, J) in enumerate(items):
            nJ = 2 * j + 2
            if i + LOOK < len(items):
                emit_qk(i + LOOK)
            ps, Tps = C.bank[i % 3], C.Tbank[i % 3]
            p_, Tp_ = pT[i % 4]
            if J == 0:
                pob = [(C.bank[4 + 2 * (pocount % 2) + h2], C.Tbank[4 + 2 * (pocount % 2) + h2]) for h2 in range(2)]
                o2, To2 = o2s[pocount % 2]
                pocount += 1
            bi = bidx[(j, J)]
            P.op("act", lambda e, ps=ps, p_=p_, bi=bi, hp=hp: e.activation(
                out=p_[:].rearrange("p a t -> p (a t)"), in_=ps[:, 0:256], func=AF.Exp,
                bias=bm[:, bi, hp:hp + 1]), rd=[Tps, Tbm], wr=[Tp_])
            vs, Tvs = vsb[i % 4]
            P.op("dve", lambda e, vs=vs, va=va, J=J, bi=bi, hp=hp: e.tensor_tensor(
                out=vs[:], in0=va[:, J, :].rearrange("p (h d) -> p h d", h=2),
                in1=bias[:, bi, 2 * hp:2 * hp + 2].unsqueeze(2).to_broadcast([128, 2, 65]), op=ALU.mult),
                 rd=[Tva, Tbias], wr=[Tvs])
            if J >= 2 * j:
                m = J - 2 * j
                P.op("pool", lambda e, p_=p_, m=m: e.tensor_tensor(
                    out=p_[:], in0=p_[:], in1=mskb[:, m:m + 1, :].to_broadcast([128, 2, 128]), op=ALU.mult),
                     rd=[Tp_, Tmskb], wr=[Tp_])
            for h2 in range(2):
                po, Tpo = pob[h2]
                P.op("pe", lambda e, po=po, h2=h2, p_=p_, vs=vs, J=J, nJ=nJ: e.matmul(
                    po[:, 0:65], lhsT=p_[:, h2, :], rhs=vs[:, h2, :],
                    start=(J == 0), stop=(J == nJ - 1)), rd=[Tp_, Tvs], wr=[Tpo])
            if pending is not None and J == 1:
                emit_tr(pending)
                pending = None
            if J == nJ - 1:
                for h2 in range(2):
                    po, Tpo = pob[h2]
                    P.op("dve", lambda e, po=po, h2=h2: e.reciprocal(out=rec[:, h2:h2 + 1], in_=po[:, 64:65]),
                         rd=[Tpo], wr=[Trec])
                    P.op("dve", lambda e, po=po, h2=h2, sg=sg, j=j, o2=o2: e.scalar_tensor_tensor(
                        out=o2[:, h2 * 64:(h2 + 1) * 64], in0=po[:, 0:64], scalar=rec[:, h2:h2 + 1],
                        in1=sg[:, j, h2 * 64:(h2 + 1) * 64], op0=ALU.mult, op1=ALU.mult),
                         rd=[Tpo, Trec, Tsg], wr=[To2])
                pending = (j, o2, To2)
        emit_tr(pending)
    for j in range(16):
        xr_, Txr = xr[j % 2]
        xo_, Txo = xo[j % 2]
        P.dma("sp", xr_[:], XOd[j * 128:(j + 1) * 128, :], rd=[TXO], wr=[Txr])
        for half in range(2):
            pw, Tpw = BK.next()
            for c in range(8):
                P.op("pe", lambda e, c=c, pw=pw, half=half, j=j: e.matmul(
                    pw[:], lhsT=oT[:, c, j * 128:(j + 1) * 128], rhs=w_o[:, c, half * 512:(half + 1) * 512],
                    start=(c == 0), stop=(c == 7)), rd=[ToT, Tw_o], wr=[Tpw])
            P.op("dve", lambda e, pw=pw, half=half, xr_=xr_, xo_=xo_: e.tensor_tensor(
                out=xo_[:, half * 512:(half + 1) * 512], in0=pw[:], in1=xr_[:, half * 512:(half + 1) * 512], op=ALU.add),
                 rd=[Tpw, Txr], wr=[Txo])
        P.dma("act", x_out_ap[j * 128:(j + 1) * 128, :], xo_[:], rd=[Txo], wr=[Tx_out], store=True)


W_SHAPES = {
    "even_norm_mix": [D], "even_w_in": [D, 2576], "even_gate_up": [16, 256], "even_gate_bias": [256],
    "even_w_s": [4, 128, 128], "even_b_s": [4, 128], "even_ln_g": [512], "even_ln_b": [512],
    "even_head_g": [4, 128], "even_w_o": [D, D], "even_norm_ffn": [D], "even_ffn_w1": [1, D, 2816],
    "even_ffn_w3": [1, D, 2816], "even_ffn_w2": [1, 2816, D], "odd_norm_mix": [D], "odd_w_in": [D, 4112],
    "odd_forget_bias": [16], "odd_q_g": [64], "odd_k_g": [64], "odd_w_o": [D, D], "odd_norm_ffn": [D],
    "odd_router": [D, 8], "odd_exp_w1": [8, D, 3584], "odd_exp_w3": [8, D, 3584], "odd_exp_w2": [8, 3584, D],
    "final_norm": [D],
}
SEQ = 4096


def build_program(stop=None):
    nc = bass.Bass("TRN2", target_bir_lowering=False)
    x = nc.dram_tensor("x", [SEQ, D], F32, kind="ExternalInput").ap()
    Wd = {k: nc.dram_tensor(k, s, F32, kind="ExternalInput").ap() for k, s in W_SHAPES.items()}
    sel = nc.dram_tensor("sel", [128, 2], F32, kind="ExternalInput").ap()
    msk = nc.dram_tensor("msk", [2, 128, 128], F32, kind="ExternalInput").ap()
    y = nc.dram_tensor("y", [SEQ if stop in ("l0m", "l0f") else SEQ // 2, D], F32, kind="ExternalOutput").ap()
    xmid = y if stop == "l0m" else nc.dram_tensor("xmid", [SEQ, D], F32).ap()
    x1 = y if stop == "l0f" else nc.dram_tensor("x1", [SEQ, D], F32).ap()
    x2 = y if stop == "l1a" else nc.dram_tensor("x2", [SEQ // 2, D], F32).ap()
    scratch = {
        "KT": nc.dram_tensor("KTd", [8, 128, SEQ], BF16).ap(),
        "VA": nc.dram_tensor("VAd", [SEQ, 16 * 65], BF16).ap(),
        "NC": nc.dram_tensor("NCd", [SEQ, 16], F32).ap(),
        "CR": nc.dram_tensor("CRd", [16, 128, 16], F32).ap(),
        "QT": nc.dram_tensor("QTd", [8, 128, SEQ // 2], BF16).ap(),
        "SG": nc.dram_tensor("SGd", [SEQ // 2, D], BF16).ap(),
        "XO": nc.dram_tensor("XOd", [SEQ // 2, D], F32).ap(),
    }
    for k in list(scratch.keys()):
        scratch["T" + k] = T(k)
    with ExitStack() as es:
        P = Prog(nc, es)
        C = Ctx(P)
        Tx, Txmid, Tx1, Tx2, Ty = T("x"), T("xmid"), T("x1"), T("x2"), T("y")
        P.emit()
        _skip = ""
        with P.stage():
            l0_mixer_stage(P, C, (x, Tx), (xmid, Txmid), SEQ if "l0m" not in _skip else 256, {
                "norm_mix": Wd["even_norm_mix"], "w_in": Wd["even_w_in"], "gate_up": Wd["even_gate_up"],
                "gate_bias": Wd["even_gate_bias"], "w_s": Wd["even_w_s"], "b_s": Wd["even_b_s"],
                "ln_g": Wd["even_ln_g"], "ln_b": Wd["even_ln_b"], "head_g": Wd["even_head_g"],
                "w_o": Wd["even_w_o"]})
            P.wait_all("sp", [Txmid])
        if stop == "l0m":
            return nc
        with P.stage():
            swiglu_stage(P, C, "f0", (xmid, Txmid), (x1, Tx1), SEQ, Wd["even_norm_ffn"], Wd["even_ffn_w1"],
                         Wd["even_ffn_w3"], Wd["even_ffn_w2"], 2816)
            P.wait_all("sp", [Tx1])
        if stop == "l0f":
            return nc
        with P.stage():
            l1_proj_stage(P, C, (x1, Tx1), {"norm_mix": Wd["odd_norm_mix"], "w_in": Wd["odd_w_in"],
                                            "forget_bias": Wd["odd_forget_bias"], "q_g": Wd["odd_q_g"],
                                            "k_g": Wd["odd_k_g"]}, sel, scratch)
            P.wait_all("sp", [scratch["T" + k] for k in ("KT", "VA", "NC", "CR", "QT", "SG", "XO")])
        with P.stage():
            l1_attn_stage(P, C, (x2, Tx2), {"w_o": Wd["odd_w_o"]}, msk, scratch, sel)
            P.wait_all("sp", [Tx2])
        if stop == "l1a":
            return nc
        with P.stage():
            swiglu_stage(P, C, "moe", (x2, Tx2), (y, Ty), SEQ // 2, Wd["odd_norm_ffn"], Wd["odd_exp_w1"],
                         Wd["odd_exp_w3"], Wd["odd_exp_w2"], 3584, router=Wd["odd_router"], nexp=8,
                         g_final=Wd["final_norm"])
            P.wait_all("sp", [Ty])
    return nc


def kernel(**inputs):
    x = np.ascontiguousarray(np.asarray(inputs["x"], dtype=np.float32))
    wmap = {}
    for k, shp in W_SHAPES.items():
        a = np.asarray(inputs[k], dtype=np.float32)
        if k in ("even_ffn_w1", "even_ffn_w3", "even_ffn_w2", "odd_exp_w1", "odd_exp_w3", "odd_exp_w2"):
            a = a.reshape(shp)
        elif k != "final_norm":
            a = a[0]
        wmap[k] = np.ascontiguousarray(a.reshape(shp))
    tril = np.triu(np.ones((128, 128), np.float32))
    msks = [np.stack([tril, np.zeros_like(tril)]), np.stack([np.ones_like(tril), tril])]
    sels = [np.tile(np.array([[1.0, 0.0]], np.float32), (128, 1)), np.tile(np.array([[0.0, 1.0]], np.float32), (128, 1))]
    nc = build_program()
    in_maps = []
    for core in range(8):
        b, par = core // 2, core % 2
        m = {"x": x[b], "sel": sels[par], "msk": msks[par]}
        m.update(wmap)
        in_maps.append(m)
    res = run_bass_kernel_spmd(nc, in_maps, core_ids=list(range(8)))
    out = np.empty((4, SEQ, D), np.float32)
    for core in range(8):
        b, par = core // 2, core % 2
        yv = np.asarray(res.results[core]["y"]).reshape(16, 128, D)
        out[b].reshape(16, 2, 128, D)[:, par] = yv
    return out


SCR_SPECS = {"KT": ([8, 128, SEQ], BF16), "VA": ([SEQ, 16 * 65], BF16), "NC": ([SEQ, 16], F32),
             "CR": ([16, 128, 16], F32), "QT": ([8, 128, SEQ // 2], BF16), "SG": ([SEQ // 2, D], BF16),
             "XO": ([SEQ // 2, D], F32)}
STAGE_W = {
    "l0m": ["even_norm_mix", "even_w_in", "even_gate_up", "even_gate_bias", "even_w_s", "even_b_s", "even_ln_g",
            "even_ln_b", "even_head_g", "even_w_o"],
    "l0f": ["even_norm_ffn", "even_ffn_w1", "even_ffn_w3", "even_ffn_w2"],
    "l1p": ["odd_norm_mix", "odd_w_in", "odd_forget_bias", "odd_q_g", "odd_k_g"],
    "l1a": ["odd_w_o"],
    "moe": ["odd_norm_ffn", "odd_router", "odd_exp_w1", "odd_exp_w3", "odd_exp_w2", "final_norm"],
}


def build_single(stage):
    nc = bass.Bass("TRN2", target_bir_lowering=False)
    Wd = {k: nc.dram_tensor(k, W_SHAPES[k], F32, kind="ExternalInput").ap() for k in STAGE_W[stage]}
    n_in = SEQ if stage in ("l0m", "l0f", "l1p") else SEQ // 2
    n_out = SEQ if stage in ("l0m", "l0f") else SEQ // 2
    xin, y, scratch = None, None, {}
    if stage != "l1a":
        xin = nc.dram_tensor("xin", [n_in, D], F32, kind="ExternalInput").ap()
    if stage != "l1p":
        y = nc.dram_tensor("y", [n_out, D], F32, kind="ExternalOutput").ap()
    if stage in ("l1p", "l1a"):
        kind = "ExternalOutput" if stage == "l1p" else "ExternalInput"
        for k, (shp, dt) in SCR_SPECS.items():
            scratch[k] = nc.dram_tensor("s_" + k, shp, dt, kind=kind).ap()
            scratch["T" + k] = T(k)
    if stage in ("l1p", "l1a"):
        sel = nc.dram_tensor("sel", [128, 2], F32, kind="ExternalInput").ap()
    if stage == "l1a":
        msk = nc.dram_tensor("msk", [2, 128, 128], F32, kind="ExternalInput").ap()
    with ExitStack() as es:
        P = Prog(nc, es)
        C = Ctx(P)
        Tx, Ty = T("xin"), T("y")
        P.emit()
        with P.stage():
            if stage == "l0m":
                l0_mixer_stage(P, C, (xin, Tx), (y, Ty), SEQ, {k[5:]: Wd[k] for k in STAGE_W[stage]})
                P.wait_all("sp", [Ty])
            elif stage == "l0f":
                swiglu_stage(P, C, "f0", (xin, Tx), (y, Ty), SEQ, Wd["even_norm_ffn"], Wd["even_ffn_w1"],
                             Wd["even_ffn_w3"], Wd["even_ffn_w2"], 2816)
                P.wait_all("sp", [Ty])
            elif stage == "l1p":
                l1_proj_stage(P, C, (xin, Tx), {k[4:]: Wd[k] for k in STAGE_W[stage]}, sel, scratch)
                P.wait_all("sp", [scratch["T" + k] for k in SCR_SPECS])
            elif stage == "l1a":
                l1_attn_stage(P, C, (y, Ty), {"w_o": Wd["odd_w_o"]}, msk, scratch, sel)
                P.wait_all("sp", [Ty])
            elif stage == "moe":
                swiglu_stage(P, C, "moe", (xin, Tx), (y, Ty), SEQ // 2, Wd["odd_norm_ffn"], Wd["odd_exp_w1"],
                             Wd["odd_exp_w3"], Wd["odd_exp_w2"], 3584, router=Wd["odd_router"], nexp=8,
                             g_final=Wd["final_norm"])
                P.wait_all("sp", [Ty])
    return nc


def kernel_unfused(**inputs):
    x = np.ascontiguousarray(np.asarray(inputs["x"], dtype=np.float32))
    wmap = {k: np.ascontiguousarray(np.asarray(inputs[k], dtype=np.float32).reshape(shp)) for k, shp in W_SHAPES.items()}
    tril = np.triu(np.ones((128, 128), np.float32))
    msks = [np.stack([tril, np.zeros_like(tril)]), np.stack([np.ones_like(tril), tril])]
    sels = [np.tile(np.array([[1.0, 0.0]], np.float32), (128, 1)), np.tile(np.array([[0.0, 1.0]], np.float32), (128, 1))]
    cores = list(range(8))
    cur = [{"xin": x[c // 2]} for c in cores]
    for stage in ("l0m", "l0f", "l1p", "l1a", "moe"):
        nc = build_single(stage)
        in_maps = []
        for c in cores:
            m = dict(cur[c])
            for k in STAGE_W[stage]:
                m[k] = wmap[k]
            if stage in ("l1p", "l1a"):
                m["sel"] = sels[c % 2]
            if stage == "l1a":
                m["msk"] = msks[c % 2]
            in_maps.append(m)
        res = run_bass_kernel_spmd(nc, in_maps, core_ids=cores)
        if stage == "l1p":
            cur = [{"s_" + k: np.asarray(res.results[c]["s_" + k]) for k in SCR_SPECS} for c in cores]
        else:
            cur = [{"xin": np.asarray(res.results[c]["y"])} for c in cores]
    out = np.empty((4, SEQ, D), np.float32)
    for c in cores:
        b, par = c // 2, c % 2
        out[b].reshape(16, 2, 128, D)[:, par] = cur[c]["xin"].reshape(16, 128, D)
    return out
```
